# Optimizing a Trainium2 kernel written in Bass

```python
import jax
import jax.numpy as jnp
from jax import lax
import numpy as np

D_MODEL = 2048
BATCH = 1
SEQ = 16384
DEPTH = 2

GRID_W = 64
CTX_LEN = 256
NORM_EPS = 1e-6

RWKV_WIDTH = D_MODEL // 2
RWKV_HEAD = 64
RWKV_HEADS = RWKV_WIDTH // RWKV_HEAD
DECAY_LORA = 64
ICLR_LORA = 64
GATE_LORA = 160
LNX_EPS = 64e-5
RWKV_COLS = (RWKV_WIDTH, RWKV_WIDTH, RWKV_WIDTH, DECAY_LORA, DECAY_LORA, ICLR_LORA, ICLR_LORA, GATE_LORA)
RWKV_IN = 3 * RWKV_WIDTH + 2 * DECAY_LORA + 2 * ICLR_LORA + GATE_LORA

RET_WIDTH = D_MODEL - RWKV_WIDTH
RET_HEAD = 128
RET_HEADS = RET_WIDTH // RET_HEAD
RET_CHUNK = 128
ROPE_BASE = 10000.0
RET_IN = 4 * RET_WIDTH
P_IN = RWKV_IN + RET_IN

N_EXPERTS = 32
N_GROUPS = 4
EXPERTS_PER_GROUP = N_EXPERTS // N_GROUPS
TOP_K = 2
D_EXPERT = 1024
MOE_BLOCK = 128

kernel_name = "hybrid_rwkv7_retention_moe_diffusion"


def rms_norm(x, g):
    xf = x.astype(jnp.float32)
    y = xf * lax.rsqrt(jnp.mean(xf * xf, axis=-1, keepdims=True) + NORM_EPS)
    return (y * g.astype(jnp.float32)).astype(x.dtype)


def split_cols(t, sizes):
    return jnp.split(t, [int(s) for s in np.cumsum(sizes)[:-1]], axis=-1)


def shift_seq(p, mu_prev, mu_next):
    prev = jnp.pad(p[:, :-1], ((0, 0), (1, 0), (0, 0)))
    nxt = jnp.pad(p[:, 1:], ((0, 0), (0, 1), (0, 0)))
    return p + mu_prev * (prev - p) + mu_next * (nxt - p)


def shift_grid(p, mu, rows):
    b, n, ch = p.shape
    g = p.reshape(b, rows, GRID_W, ch)
    left = jnp.pad(g[:, :, :-1], ((0, 0), (0, 0), (1, 0), (0, 0)))
    right = jnp.pad(g[:, :, 1:], ((0, 0), (0, 0), (0, 1), (0, 0)))
    up = jnp.pad(g[:, :-1], ((0, 0), (1, 0), (0, 0), (0, 0)))
    down = jnp.pad(g[:, 1:], ((0, 0), (0, 1), (0, 0), (0, 0)))
    out = g + mu[0] * (left - g) + mu[1] * (right - g) + mu[2] * (up - g) + mu[3] * (down - g)
    return out.reshape(b, n, ch)


def rwkv7_inputs(u, w0, w2, a0, a2, g2, k_k, k_a):
    b, n, _ = u.shape
    r, k, v, wd_f, wd_b, ad_f, ad_b, gd = split_cols(u.astype(jnp.float32), RWKV_COLS)

    def heads(t):
        return t.reshape(b, n, RWKV_HEADS, RWKV_HEAD)

    kk = heads(k * k_k)
    kk = kk / jnp.maximum(jnp.linalg.norm(kk, axis=-1, keepdims=True), 1e-12)
    per_dir = []
    for d, (wd, ad) in enumerate(((wd_f, ad_f), (wd_b, ad_b))):
        w_log = -jax.nn.softplus(-(w0[d] + jnp.tanh(wd) @ w2[d])) - 0.5
        a = jax.nn.sigmoid(a0[d] + ad @ a2[d])
        decay = jnp.exp(-jnp.exp(w_log))
        per_dir.append((heads(decay), heads(k * (1.0 + (a - 1.0) * k_a)), heads(a)))
    g = jax.nn.sigmoid(gd) @ g2
    return heads(r), heads(v), kk, g, per_dir


def rwkv7_scan(r, w, k, v, kk, a, s0, reverse):
    def step(s, inp):
        r_t, w_t, k_t, v_t, kk_t, a_t = inp
        sa = jnp.einsum('bhvk,bhk->bhv', s, kk_t)
        s = (s * w_t[:, :, None, :] - sa[..., None] * (kk_t * a_t)[:, :, None, :]
             + v_t[..., None] * k_t[:, :, None, :])
        return s, jnp.einsum('bhvk,bhk->bhv', s, r_t)

    xs = tuple(jnp.swapaxes(t, 0, 1) for t in (r, w, k, v, kk, a))
    s_fin, ys = lax.scan(step, s0, xs, reverse=reverse)
    return jnp.swapaxes(ys, 0, 1), s_fin


def rwkv7_output(y, r, v, g, per_dir, r_k, lnx_w, lnx_b):
    b, n = y.shape[:2]
    mu = jnp.mean(y, axis=-1, keepdims=True)
    var = jnp.mean(jnp.square(y - mu), axis=-1, keepdims=True)
    yn = (y - mu) * lax.rsqrt(var + LNX_EPS)
    k_f, k_b = per_dir[0][1], per_dir[1][1]
    bonus = (jnp.sum(r * k_f * r_k, axis=-1, keepdims=True)
             + jnp.sum(r * k_b * r_k, axis=-1, keepdims=True)) * v
    return (yn.reshape(b, n, RWKV_WIDTH) * lnx_w + lnx_b + bonus.reshape(b, n, RWKV_WIDTH)) * g


def rwkv7_mixer(u_c, u_x, w0, w2, a0, a2, g2, k_k, k_a, r_k, lnx_w, lnx_b, with_ctx):
    r_c, v_c, kk_c, g_c, dirs_c = rwkv7_inputs(u_c, w0, w2, a0, a2, g2, k_k, k_a)
    r_x, v_x, kk_x, g_x, dirs_x = rwkv7_inputs(u_x, w0, w2, a0, a2, g2, k_k, k_a)
    s0 = jnp.zeros((u_x.shape[0], RWKV_HEADS, RWKV_HEAD, RWKV_HEAD), jnp.float32)
    ys_c, ys_x = [], []
    for d in range(2):
        rev = d == 1
        dec_c, kd_c, a_c = dirs_c[d]
        y_c, s_c = rwkv7_scan(r_c, dec_c, kd_c, v_c, kk_c, a_c, s0, rev)
        dec_x, kd_x, a_x = dirs_x[d]
        y_x, _ = rwkv7_scan(r_x, dec_x, kd_x, v_x, kk_x, a_x, s_c, rev)
        ys_c.append(y_c)
        ys_x.append(y_x)
    out_x = rwkv7_output(ys_x[0] + ys_x[1], r_x, v_x, g_x, dirs_x, r_k, lnx_w, lnx_b)
    out_c = rwkv7_output(ys_c[0] + ys_c[1], r_c, v_c, g_c, dirs_c, r_k, lnx_w, lnx_b) if with_ctx else None
    return out_c, out_x


def rotary_2d(t):
    n = t.shape[1]
    pos = jnp.arange(n)
    half = RET_HEAD // 2
    inv = ROPE_BASE ** (-jnp.arange(0, half, 2, dtype=jnp.float32) / half)

    def rot(xp, p):
        ang = p.astype(jnp.float32)[:, None] * inv
        cos = jnp.cos(ang)[None, :, None, :]
        sin = jnp.sin(ang)[None, :, None, :]
        x1, x2 = xp[..., :half // 2], xp[..., half // 2:]
        return jnp.concatenate([x1 * cos - x2 * sin, x1 * sin + x2 * cos], axis=-1)

    return jnp.concatenate([rot(t[..., :half], pos // GRID_W), rot(t[..., half:], pos % GRID_W)], axis=-1)


def retention_scan(q, k, v, log_gamma, s0, include_diag):
    b, n, h, _ = q.shape
    nc = n // RET_CHUNK

    def chunks(t):
        return t.reshape(b, nc, RET_CHUNK, h, t.shape[-1]).transpose(1, 0, 3, 2, 4)

    idx = jnp.arange(RET_CHUNK, dtype=jnp.float32)
    diff = idx[:, None] - idx[None, :]
    keep = diff >= 0 if include_diag else diff > 0
    dmat = jnp.where(keep[None], jnp.exp(jnp.maximum(diff, 0.0)[None] * log_gamma[:, None, None]), 0.0)
    xi = jnp.exp((idx + 1.0)[None, :] * log_gamma[:, None])[..., None]
    zeta = jnp.exp((RET_CHUNK - 1.0 - idx)[None, :] * log_gamma[:, None])[..., None]
    g_chunk = jnp.exp(RET_CHUNK * log_gamma)[:, None, None]

    def step(s, inp):
        qc, kc, vc = inp
        scores = jnp.einsum('bhnd,bhmd->bhnm', qc, kc) * dmat
        y = jnp.einsum('bhnm,bhmv->bhnv', scores, vc) + jnp.einsum('bhnd,bhdv->bhnv', qc * xi, s)
        s = s * g_chunk + jnp.einsum('bhmd,bhmv->bhdv', kc * zeta, vc)
        return s, y

    s_fin, ys = lax.scan(step, s0, (chunks(q), chunks(k), chunks(v)))
    return ys.transpose(1, 0, 3, 2, 4).reshape(b, n, h, v.shape[-1]), s_fin


def retention_inputs(p, rotate):
    b, n, _ = p.shape
    q, k, v, g = jnp.split(p.astype(jnp.float32), 4, axis=-1)

    def heads(t):
        return t.reshape(b, n, RET_HEADS, RET_HEAD)

    q, k, v = heads(q), heads(k) * (RET_HEAD ** -0.5), heads(v)
    if rotate:
        q, k = rotary_2d(q), rotary_2d(k)
    return q, k, v, g


def retention_output(y, g, gn_w):
    b, n = y.shape[:2]
    yn = y * lax.rsqrt(jnp.mean(y * y, axis=-1, keepdims=True) + NORM_EPS)
    return yn.reshape(b, n, RET_WIDTH) * gn_w * jax.nn.silu(g)


def retention_mixer(p_c, p_x, log2_decay, gn_w, with_ctx):
    qc, kc, vc, gc = retention_inputs(p_c, False)
    qx, kx, vx, gx = retention_inputs(p_x, True)
    log_gamma = jnp.log1p(-jnp.exp2(-log2_decay.astype(jnp.float32)))
    s0 = jnp.zeros((p_x.shape[0], RET_HEADS, RET_HEAD, RET_HEAD), jnp.float32)

    def flip(t):
        return jnp.flip(t, axis=1)

    yc_f, sc_f = retention_scan(qc, kc, vc, log_gamma[0], s0, True)
    yx_f, _ = retention_scan(qx, kx, vx, log_gamma[0], sc_f, True)
    yc_b, sc_b = retention_scan(flip(qc), flip(kc), flip(vc), log_gamma[1], s0, False)
    yx_b, _ = retention_scan(flip(qx), flip(kx), flip(vx), log_gamma[1], sc_b, False)
    out_x = retention_output(yx_f + flip(yx_b), gx, gn_w)
    out_c = retention_output(yc_f + flip(yc_b), gc, gn_w) if with_ctx else None
    return out_c, out_x


def route(h, router_w, router_b):
    t = h.shape[0]
    scores = jax.nn.sigmoid((h @ router_w).astype(jnp.float32))
    biased = scores + router_b.astype(jnp.float32)
    grp = biased.reshape(t, N_GROUPS, EXPERTS_PER_GROUP)
    grp_score = jnp.sum(lax.top_k(grp, 2)[0], axis=-1)
    g_sel = jnp.argmax(grp_score, axis=-1)
    sel_idx = jnp.broadcast_to(g_sel[:, None, None], (t, 1, EXPERTS_PER_GROUP))
    in_grp = jnp.take_along_axis(grp, sel_idx, axis=1)[:, 0]
    _, local = lax.top_k(in_grp, TOP_K)
    expert = g_sel[:, None] * EXPERTS_PER_GROUP + local
    w = jnp.take_along_axis(scores, expert, axis=1)
    return expert, w / jnp.sum(w, axis=-1, keepdims=True)


def moe(h, router_w, router_b, w1, w2, layer):
    t, d = h.shape
    expert, gate = route(h, router_w, router_b)
    m = t * TOP_K
    e_flat = expert.reshape(-1)
    tok = jnp.repeat(jnp.arange(t, dtype=jnp.int32), TOP_K)
    order = jnp.argsort(e_flat)
    e_s = e_flat[order]
    counts = jnp.bincount(e_flat, length=N_EXPERTS)
    padded = (counts + MOE_BLOCK - 1) // MOE_BLOCK * MOE_BLOCK
    start = jnp.cumsum(counts) - counts
    pend = jnp.cumsum(padded)
    pstart = pend - padded
    dest = pstart[e_s] + jnp.arange(m, dtype=jnp.int32) - start[e_s]
    m_pad = -(-m // MOE_BLOCK) * MOE_BLOCK + N_EXPERTS * MOE_BLOCK
    n_blocks = m_pad // MOE_BLOCK
    row_tok = jnp.zeros((m_pad,), jnp.int32).at[dest].set(tok[order])
    row_w = jnp.zeros((m_pad,), h.dtype).at[dest].set(gate.reshape(-1)[order].astype(h.dtype))
    block_start = jnp.arange(n_blocks, dtype=jnp.int32) * MOE_BLOCK
    block_expert = jnp.minimum(jnp.searchsorted(pend, block_start, side='right'), N_EXPERTS - 1)
    xb = h[row_tok].reshape(n_blocks, MOE_BLOCK, d)

    def expert_block(args):
        xblk, e = args
        gu = xblk @ w1[layer, e]
        g_, u_ = jnp.split(gu, 2, axis=-1)
        return (jax.nn.silu(g_) * u_) @ w2[layer, e]

    yb = lax.map(expert_block, (xb, block_expert)).reshape(m_pad, d)
    return jnp.zeros_like(h).at[row_tok].add(yb * row_w[:, None])


def setup_inputs(seed: int = 0) -> dict:
    key = jax.random.key(seed)
    ks = jax.random.split(key, 28)
    f32 = jnp.float32

    def nrm(k, shape, s):
        return s * jax.random.normal(k, shape, f32)

    D = D_MODEL
    return {
        "x": nrm(ks[0], (BATCH, SEQ, D), 1.0),
        "c": nrm(ks[1], (BATCH, D), 1.0),
        "ctx": nrm(ks[2], (BATCH, CTX_LEN, D), 1.0),
        "c_ctx": nrm(ks[3], (D,), 1.0),
        "ada_w": nrm(ks[4], (DEPTH, D, 6 * D), 0.3 * D ** -0.5),
        "ada_b": nrm(ks[5], (DEPTH, 6 * D), 0.02),
        "norm1_g": 1.0 + nrm(ks[6], (DEPTH, D), 0.05),
        "norm2_g": 1.0 + nrm(ks[7], (DEPTH, D), 0.05),
        "w_in": nrm(ks[8], (DEPTH, D, P_IN), D ** -0.5),
        "shift_mu": jax.random.uniform(ks[9], (DEPTH, 4, RWKV_IN), f32, 0.0, 0.5),
        "rwkv_w0": jax.random.uniform(ks[10], (DEPTH, 2, RWKV_WIDTH), f32, -6.0, -1.0),
        "rwkv_w2": nrm(ks[11], (DEPTH, 2, DECAY_LORA, RWKV_WIDTH), 0.5 * DECAY_LORA ** -0.5),
        "rwkv_a0": nrm(ks[12], (DEPTH, 2, RWKV_WIDTH), 0.5),
        "rwkv_a2": nrm(ks[13], (DEPTH, 2, ICLR_LORA, RWKV_WIDTH), 0.5 * ICLR_LORA ** -0.5),
        "rwkv_g2": nrm(ks[14], (DEPTH, GATE_LORA, RWKV_WIDTH), GATE_LORA ** -0.5),
        "rwkv_k_k": 0.85 + nrm(ks[15], (DEPTH, RWKV_WIDTH), 0.05),
        "rwkv_k_a": 1.0 + nrm(ks[16], (DEPTH, RWKV_WIDTH), 0.05),
        "rwkv_r_k": nrm(ks[17], (DEPTH, RWKV_HEADS, RWKV_HEAD), 0.1),
        "rwkv_lnx_w": 1.0 + nrm(ks[18], (DEPTH, RWKV_WIDTH), 0.05),
        "rwkv_lnx_b": nrm(ks[19], (DEPTH, RWKV_WIDTH), 0.02),
        "ret_log2_decay": ((5.0 + jnp.arange(RET_HEADS, dtype=f32))[None, None, :]
                           + jnp.array([0.0, 0.5], f32)[None, :, None]
                           + nrm(ks[20], (DEPTH, 2, RET_HEADS), 0.1)),
        "ret_gn_w": 1.0 + nrm(ks[21], (DEPTH, RET_WIDTH), 0.05),
        "w_out": nrm(ks[22], (DEPTH, D, D), D ** -0.5),
        "router_w": nrm(ks[23], (D, N_EXPERTS), D ** -0.5),
        "router_b": nrm(ks[24], (N_EXPERTS,), 0.01),
        "moe_w1": nrm(ks[25], (DEPTH, N_EXPERTS, D, 2 * D_EXPERT), D ** -0.5),
        "moe_w2": nrm(ks[26], (DEPTH, N_EXPERTS, D_EXPERT, D), D_EXPERT ** -0.5),
        "final_g": 1.0 + nrm(ks[27], (D,), 0.05),
    }


def reference(x, c, ctx, c_ctx, ada_w, ada_b, norm1_g, norm2_g, w_in, shift_mu,
              rwkv_w0, rwkv_w2, rwkv_a0, rwkv_a2, rwkv_g2, rwkv_k_k, rwkv_k_a, rwkv_r_k,
              rwkv_lnx_w, rwkv_lnx_b, ret_log2_decay, ret_gn_w, w_out, router_w, router_b,
              moe_w1, moe_w2, final_g):
    b, n, d = x.shape
    rows = n // GRID_W
    n_ctx = ctx.shape[1]
    cx = ctx
    for l in range(DEPTH):
        last = l == DEPTH - 1
        mod_x = jax.nn.silu(c) @ ada_w[l] + ada_b[l]
        mod_c = jax.nn.silu(c_ctx) @ ada_w[l] + ada_b[l]
        sh1, sc1, gt1, sh2, sc2, gt2 = jnp.split(mod_x[:, None, :], 6, axis=-1)
        csh1, csc1, cgt1, csh2, csc2, cgt2 = jnp.split(mod_c, 6, axis=-1)

        h_x = rms_norm(x, norm1_g[l]) * (1.0 + sc1) + sh1
        h_c = rms_norm(cx, norm1_g[l]) * (1.0 + csc1) + csh1
        p = jnp.concatenate([h_c, h_x], axis=1) @ w_in[l]
        p_c, p_x = p[:, :n_ctx], p[:, n_ctx:]
        u_c = shift_seq(p_c[..., :RWKV_IN], shift_mu[l, 0], shift_mu[l, 1])
        u_x = shift_grid(p_x[..., :RWKV_IN], shift_mu[l], rows)
        ya_c, ya_x = rwkv7_mixer(u_c, u_x, rwkv_w0[l], rwkv_w2[l], rwkv_a0[l], rwkv_a2[l], rwkv_g2[l],
                                 rwkv_k_k[l], rwkv_k_a[l], rwkv_r_k[l], rwkv_lnx_w[l], rwkv_lnx_b[l],
                                 not last)
        yb_c, yb_x = retention_mixer(p_c[..., RWKV_IN:], p_x[..., RWKV_IN:], ret_log2_decay[l],
                                     ret_gn_w[l], not last)

        if last:
            y = jnp.concatenate([ya_x, yb_x], axis=-1).astype(x.dtype) @ w_out[l]
            x = x + gt1 * y
            h2 = rms_norm(x, norm2_g[l]) * (1.0 + sc2) + sh2
            f = moe(h2.reshape(-1, d), router_w, router_b, moe_w1, moe_w2, l).reshape(b, n, d)
            x = x + gt2 * f
        else:
            y_all = jnp.concatenate([jnp.concatenate([ya_c, yb_c], axis=-1),
                                     jnp.concatenate([ya_x, yb_x], axis=-1)], axis=1)
            y = y_all.astype(x.dtype) @ w_out[l]
            cx = cx + cgt1 * y[:, :n_ctx]
            x = x + gt1 * y[:, n_ctx:]
            h2 = jnp.concatenate([rms_norm(cx, norm2_g[l]) * (1.0 + csc2) + csh2,
                                  rms_norm(x, norm2_g[l]) * (1.0 + sc2) + sh2], axis=1)
            f = moe(h2.reshape(-1, d), router_w, router_b, moe_w1, moe_w2, l).reshape(b, n_ctx + n, d)
            cx = cx + cgt2 * f[:, :n_ctx]
            x = x + gt2 * f[:, n_ctx:]
    return rms_norm(x, final_g)
```

```python
import contextlib
import numpy as np
import concourse.bass as bass
import concourse.mybir as mybir
from concourse.bass_utils import run_bass_kernel_spmd

F32 = mybir.dt.float32
F32R = mybir.dt.float32r
AF = mybir.ActivationFunctionType
ALU = mybir.AluOpType
AX = mybir.AxisListType

ENGS = ("pe", "act", "dve", "pool", "sp")
DMAQ = ("sp", "act", "pool")
NDSEM = 6


class _Op:
    __slots__ = ("eng", "fn", "dma", "deps", "sig", "dsem", "dval", "consumed", "idx")

    def __init__(self, eng, fn, dma):
        self.eng = eng
        self.fn = fn
        self.dma = dma
        self.deps = ()
        self.sig = 0
        self.dsem = None
        self.dval = 0
        self.consumed = False
        self.idx = 0


class Sched:
    def __init__(self, nc, stack):
        self.nc = nc
        self.ops = {e: [] for e in ENGS}
        self.res = {}
        self.sem = {e: stack.enter_context(nc.semaphore("sem_" + e)) for e in ENGS}
        self.dsem = {e: [stack.enter_context(nc.semaphore("dsem_%s%d" % (e, i)))
                         for i in range(NDSEM)] for e in DMAQ}
        self.duse = {e: [None] * NDSEM for e in DMAQ}
        self.dcnt = {e: 0 for e in DMAQ}
        self.nops = 0

    def add(self, eng, fn, reads=(), writes=(), dma=False):
        op = _Op(eng, fn, dma)
        op.idx = self.nops
        self.nops += 1
        deps = {}
        res = self.res
        if "PHASE" not in writes:
            reads = tuple(reads) + ("PHASE",)
        for r in reads:
            st = res.get(r)
            if st is None:
                st = res[r] = [None, {}]
            if st[0] is not None:
                deps[id(st[0])] = st[0]
        for w in writes:
            st = res.get(w)
            if st is None:
                st = res[w] = [None, {}]
            if st[0] is not None:
                deps[id(st[0])] = st[0]
            for rd in st[1].values():
                deps[id(rd)] = rd
        if dma:
            k = self.dcnt[eng] % NDSEM
            self.dcnt[eng] += 1
            prev = self.duse[eng][k]
            if prev is not None:
                deps[id(prev)] = prev
            op.dsem = (eng, k)
            op.dval = (prev.dval if prev is not None else 0) + 16
            self.duse[eng][k] = op
        for r in reads:
            st = res[r]
            key = (("d",) + op.dsem) if dma else eng
            st[1][key] = op
        for w in writes:
            res[w] = [op, {}]
        deps.pop(id(op), None)
        dl = []
        for d in deps.values():
            if (not d.dma) and d.eng == eng and eng == "pe":
                continue
            d.consumed = True
            dl.append(d)
        op.deps = dl
        self.ops[eng].append(op)
        return op

    def pe(self, fn, reads=(), writes=()):
        return self.add("pe", fn, reads, writes)

    def act(self, fn, reads=(), writes=()):
        return self.add("act", fn, reads, writes)

    def dve(self, fn, reads=(), writes=()):
        return self.add("dve", fn, reads, writes)

    def pool(self, fn, reads=(), writes=()):
        return self.add("pool", fn, reads, writes)

    def barrier(self, tile):
        return self.add("dve", lambda e: e.memset(tile, 0.0), (), ("PHASE",))

    def dma(self, q, out, in_, reads=(), writes=()):
        return self.add(q, lambda e: e.dma_start(out=out, in_=in_), reads, writes, dma=True)

    def emit(self):
        nc = self.nc
        eobj = {"pe": nc.tensor, "act": nc.scalar, "dve": nc.vector, "pool": nc.gpsimd, "sp": nc.sync}
        for e in ENGS:
            n = 0
            for op in self.ops[e]:
                if op.consumed and not op.dma:
                    n += 1
                    op.sig = n
        sem = self.sem
        dsem = self.dsem
        duse = self.duse
        allops = self.ops

        def run(e, eng):
            known = {}
            for op in allops[e]:
                need = {}
                for d in op.deps:
                    if d.dma:
                        key = ("d",) + d.dsem
                        val = d.dval
                        s = dsem[d.dsem[0]][d.dsem[1]]
                    else:
                        key = d.eng
                        val = d.sig
                        s = sem[d.eng]
                    if known.get(key, 0) >= val:
                        continue
                    if key not in need or need[key][0] < val:
                        need[key] = (val, s)
                for key, (val, s) in need.items():
                    known[key] = val
                    eng.wait_ge(s, val)
                ins = op.fn(eng)
                if op.dma:
                    ins.then_inc(dsem[op.dsem[0]][op.dsem[1]], 16)
                elif op.consumed:
                    ins.then_inc(sem[e], 1)
            if e in DMAQ:
                for k in range(NDSEM):
                    last = duse[e][k]
                    if last is not None and known.get(("d", e, k), 0) < last.dval:
                        eng.wait_ge(dsem[e][k], last.dval)

        with nc.Block() as block:
            @block.sync
            def _(eng):
                run("sp", eng)

            @block.tensor
            def _(eng):
                run("pe", eng)

            @block.scalar
            def _(eng):
                run("act", eng)

            @block.vector
            def _(eng):
                run("dve", eng)

            @block.gpsimd
            def _(eng):
                run("pool", eng)

import math

NTOK = 16640
NCTX = 256
NCOLB = 13
KAPPA = math.exp(-0.5)
LNX_EPS = 64e-5
RET_SCALE = 128 ** -0.5
C_DIFF, C_NP1, C_N128, C_MLT, C_MLE, C_MGT, C_MGE, C_ID, C_BONES = [128 * i for i in range(9)]
C_HSEL = 128 * 9
C_COLM = C_HSEL + 2
C_COL127 = C_COLM + 1
C_ONES = C_COL127 + 1
CW = C_ONES + 128


def make_cst():
    m = np.arange(128, dtype=np.float32)[:, None]
    n = np.arange(128, dtype=np.float32)[None, :]
    c = np.zeros((128, CW), np.float32)
    c[:, C_DIFF:C_DIFF + 128] = n - m
    c[:, C_NP1:C_NP1 + 128] = n + 1 + 0 * m
    c[:, C_N128:C_N128 + 128] = 128 - n + 0 * m
    c[:, C_MLT:C_MLT + 128] = (m < n)
    c[:, C_MLE:C_MLE + 128] = (m <= n)
    c[:, C_MGT:C_MGT + 128] = (m > n)
    c[:, C_MGE:C_MGE + 128] = (m >= n)
    c[:, C_ID:C_ID + 128] = (m == n)
    c[:, C_BONES:C_BONES + 128] = ((m // 64) == (n // 64))
    c[:, C_HSEL] = (m[:, 0] < 64)
    c[:, C_HSEL + 1] = (m[:, 0] >= 64)
    c[:, C_COLM] = m[:, 0]
    c[:, C_COL127] = 127 - m[:, 0]
    c[:, C_ONES:C_ONES + 128] = 1.0
    return c


def build_A():
    nc = bass.Bass("TRN2", target_bir_lowering=False)

    def din(name, shape):
        return nc.dram_tensor(name, shape, F32, kind="ExternalInput").ap()

    xT = din("xT", [2048, NTOK])
    W = din("W", [2048, 1568])
    mvec = din("mvec", [128, 5, 16])
    mu = din("mu", [128, 7, 4])
    pvec = din("pvec", [128, 8])
    w2s = din("w2s", [128, 128])
    a2s = din("a2s", [128, 128])
    g2a = din("g2a", [128, 128])
    g2b = din("g2b", [32, 128])
    rowv = din("rowv", [3, 128])
    dec = din("dec", [2])
    cosT = din("cosT", [128, 16384])
    sinT = din("sinT", [128, 16384])
    cst_d = din("cst", [128, CW])
    out = nc.dram_tensor("out", [NTOK, 256], F32, kind="ExternalOutput").ap()
    pT = nc.dram_tensor("pT", [NCOLB, 128, NTOK], F32).ap()
    yfw = nc.dram_tensor("yfw", [NTOK, 260], F32).ap()

    with contextlib.ExitStack() as st:
        S = Sched(nc, st)
        U = st.enter_context(nc.sbuf_tensor("U", [128, 44000], F32))
        cst = st.enter_context(nc.sbuf_tensor("cstt", [128, CW], F32))
        pers = st.enter_context(nc.sbuf_tensor("pers", [128, 2048], F32))
        dummy = st.enter_context(nc.sbuf_tensor("dummya", [128, 8], F32))
        ps = [st.enter_context(nc.psum_tensor("ps%d" % i, [128, 512], F32)) for i in range(8)]
        off = [0]

        def carve(n):
            a = U[:, off[0]:off[0] + n]
            off[0] += n
            assert off[0] <= 44000, off[0]
            return a

        poff = [0]

        def pcarve(n):
            a = pers[:, poff[0]:poff[0] + n]
            poff[0] += n
            assert poff[0] <= 2040
            return a

        S.dma("sp", cst[:], cst_d[:, :], writes=["cst"])
        epsv = pers[:, 2040:2042]
        S.dve(lambda e: e.memset(pers[:, 2040:2041], 1e-6), writes=["epsv"])
        S.dve(lambda e: e.memset(pers[:, 2041:2042], LNX_EPS), writes=["epsv"])
        ones = cst[:, C_ONES:C_ONES + 128]
        ident = cst[:, C_ID:C_ID + 128]

        mv = pcarve(80).rearrange("p (v c) -> p v c", v=5)
        S.dma("sp", mv, mvec[:, :, :], writes=["mv"])
        mus = pcarve(28).rearrange("p (b k) -> p b k", b=7)
        S.dma("sp", mus, mu[:, :, :], writes=["mus"])
        pv = pcarve(8)
        S.dma("sp", pv, pvec[:, :], writes=["pv"])
        w2t = pcarve(128); a2t = pcarve(128); g2at = pcarve(128); g2bt = pcarve(128)
        S.dma("sp", w2t, w2s[:, :], writes=["w2t"])
        S.dma("sp", a2t, a2s[:, :], writes=["a2t"])
        S.dma("sp", g2at, g2a[:, :], writes=["g2at"])
        S.dma("sp", g2bt[0:32, :], g2b[:, :], writes=["g2bt"])
        lnw = pcarve(128); lnb = pcarve(128); gnw = pcarve(128)
        S.dma("sp", lnw, rowv[0].partition_broadcast(128), writes=["rowp"])
        S.dma("sp", lnb, rowv[1].partition_broadcast(128), writes=["rowp"])
        S.dma("sp", gnw, rowv[2].partition_broadcast(128), writes=["rowp"])
        dect = pcarve(2)
        S.dma("sp", dect, dec.partition_broadcast(128), writes=["dect"])
        sx = pcarve(16); sc_ = pcarve(16)
        c0 = pcarve(7)
        c0c = pcarve(7)
        bias = pcarve(16)
        lg = pcarve(2); nlg = pcarve(2); gC = pcarve(2); zeta = pcarve(2)
        state = pcarve(128)
        Sret = pcarve(128)
        dmT = [pcarve(128), pcarve(128)]
        xibc = [pcarve(128), pcarve(128)]

        S.dve(lambda e: e.scalar_tensor_tensor(out=sx, in0=mv[:, 1, :], scalar=1.0, in1=mv[:, 4, :], op0=ALU.add, op1=ALU.mult),
              reads=["mv"], writes=["sx"])
        S.dve(lambda e: e.scalar_tensor_tensor(out=sc_, in0=mv[:, 3, :], scalar=1.0, in1=mv[:, 4, :], op0=ALU.add, op1=ALU.mult),
              reads=["mv"], writes=["sc_"])
        S.dve(lambda e: e.tensor_reduce(out=c0, in_=mus, axis=AX.X, op=ALU.add), reads=["mus"], writes=["c0"])
        S.dve(lambda e: e.tensor_scalar(c0, c0, -1.0, 1.0, ALU.mult, ALU.add), reads=["c0"], writes=["c0"])
        S.dve(lambda e: e.tensor_reduce(out=c0c, in_=mus[:, :, 0:2], axis=AX.X, op=ALU.add), reads=["mus"], writes=["c0c"])
        S.dve(lambda e: e.tensor_scalar(c0c, c0c, -1.0, 1.0, ALU.mult, ALU.add), reads=["c0c"], writes=["c0c"])
        S.act(lambda e: e.activation(out=lg, in_=dect, func=AF.Exp, scale=-math.log(2.0)), reads=["dect"], writes=["lg"])
        S.act(lambda e: e.activation(out=lg, in_=lg, func=AF.Ln, scale=-1.0, bias=1.0), reads=["lg"], writes=["lg"])
        S.dve(lambda e: e.tensor_scalar(nlg, lg, -1.0, None, ALU.mult), reads=["lg"], writes=["nlg"])
        S.act(lambda e: e.activation(out=gC, in_=lg, func=AF.Exp, scale=128.0), reads=["lg"], writes=["gC"])
        S.act(lambda e: e.activation(out=dmT[0], in_=cst[:, C_DIFF:C_DIFF + 128], func=AF.Exp, scale=lg[:, 0:1]),
              reads=["cst", "lg"], writes=["dmT"])
        S.dve(lambda e: e.scalar_tensor_tensor(out=dmT[0], in0=dmT[0], scalar=RET_SCALE, in1=cst[:, C_MLE:C_MLE + 128],
                                               op0=ALU.mult, op1=ALU.mult), reads=["dmT", "cst"], writes=["dmT"])
        S.act(lambda e: e.activation(out=dmT[1], in_=cst[:, C_DIFF:C_DIFF + 128], func=AF.Exp, scale=nlg[:, 1:2]),
              reads=["cst", "nlg"], writes=["dmT"])
        S.dve(lambda e: e.scalar_tensor_tensor(out=dmT[1], in0=dmT[1], scalar=RET_SCALE, in1=cst[:, C_MGT:C_MGT + 128],
                                               op0=ALU.mult, op1=ALU.mult), reads=["dmT", "cst"], writes=["dmT"])
        S.act(lambda e: e.activation(out=xibc[0], in_=cst[:, C_NP1:C_NP1 + 128], func=AF.Exp, scale=lg[:, 0:1]),
              reads=["cst", "lg"], writes=["xibc"])
        S.act(lambda e: e.activation(out=xibc[1], in_=cst[:, C_N128:C_N128 + 128], func=AF.Exp, scale=lg[:, 1:2]),
              reads=["cst", "lg"], writes=["xibc"])
        S.act(lambda e: e.activation(out=zeta[:, 0:1], in_=cst[:, C_COL127:C_COL127 + 1], func=AF.Exp, scale=lg[:, 0:1]),
              reads=["cst", "lg"], writes=["zeta"])
        S.act(lambda e: e.activation(out=zeta[:, 1:2], in_=cst[:, C_COLM:C_COLM + 1], func=AF.Exp, scale=lg[:, 1:2]),
              reads=["cst", "lg"], writes=["zeta"])
        S.dve(lambda e: e.tensor_scalar(zeta, zeta, RET_SCALE, None, ALU.mult), reads=["zeta"], writes=["zeta"])

        Wp = carve(16 * 1568).rearrange("p (c n) -> p c n", c=16)
        xb = [carve(16 * 256).rearrange("p (c t) -> p c t", c=16) for _ in range(2)]
        sq = carve(16 * 256).rearrange("p (c t) -> p c t", c=16)
        rsb = carve(256)
        tmpb = [carve(256) for _ in range(2)]
        pob = [carve(256) for _ in range(2)]
        sh1r = carve(32)
        ones_r = carve(128)
        S.act(lambda e: e.copy(ones_r, ones), reads=["cst"], writes=["ones_r"])
        colw = [128] * 6 + [32] + [128] * 6
        colo = [0]
        for wdt in colw:
            colo.append(colo[-1] + wdt)

        blocks = [(0, 256, True)] + [(256 + 256 * i, 256, False) for i in range(64)]
        kk_ = 0
        cnt = 0
        for variant in ("c", "x"):
            vi = 2 if variant == "c" else 0
            sv = sc_ if variant == "c" else sx
            S.dma("act", Wp, W.rearrange("(c p) n -> p c n", p=128), writes=["Wp"])
            S.act(lambda e, vi=vi: e.copy(sh1r[:, 0:16], mv[:, vi, :]), reads=["mv"], writes=["sh1r"])
            for b in range(NCOLB):
                wdt = colw[b]
                pq = ps[7][:wdt, b:b + 1]
                for c in range(16):
                    S.pe(lambda e, c=c, pq=pq, b=b, wdt=wdt: e.matmul(pq, Wp[:, c, colo[b]:colo[b] + wdt], sh1r[:, c:c + 1],
                                                                        start=(c == 0), stop=(c == 15)),
                         reads=["Wp", "sh1r"], writes=["ps7"])
            S.dve(lambda e: e.tensor_copy(bias[:, 0:6], ps[7][:, 0:6]), reads=["ps7"], writes=["bias"])
            S.dve(lambda e: e.tensor_copy(bias[:32, 6:7], ps[7][:32, 6:7]), reads=["ps7"], writes=["bias"])
            S.dve(lambda e: e.tensor_copy(bias[:, 7:13], ps[7][:, 7:13]), reads=["ps7"], writes=["bias"])
            for c in range(16):
                S.dve(lambda e, c=c, sv=sv: e.tensor_scalar(Wp[:, c, :], Wp[:, c, :], sv[:, c:c + 1], None, ALU.mult),
                      reads=["Wp", "sx", "sc_"], writes=["Wp"])
            for (t0, tn, isctx) in blocks:
                if isctx != (variant == "c"):
                    continue
                xbb = xb[kk_ % 2]
                rx = ("xb", kk_ % 2)
                kk_ += 1
                S.dma("act", xbb, xT[:, t0:t0 + tn].rearrange("(c p) t -> p c t", p=128), writes=[rx])
                S.act(lambda e, xbb=xbb: e.activation(out=sq, in_=xbb, func=AF.Square), reads=[rx], writes=["sq"])
                for c in range(16):
                    S.pe(lambda e, c=c: e.matmul(ps[6][:, 0:256], ones_r, sq[:, c, :], start=(c == 0), stop=(c == 15)),
                         reads=["sq", "ones_r"], writes=["ps6"])
                S.act(lambda e: e.activation(out=rsb, in_=ps[6][:, 0:256], func=AF.Sqrt, scale=1.0 / 2048, bias=epsv[:, 0:1]),
                      reads=["ps6", "epsv"], writes=["rsb"])
                S.dve(lambda e: e.reciprocal(rsb, rsb), reads=["rsb"], writes=["rsb"])
                for b in range(NCOLB):
                    wdt = colw[b]
                    pb = cnt % 4
                    cnt += 1
                    pq = ps[pb][:wdt, 0:256]
                    for c in range(16):
                        S.pe(lambda e, c=c, pq=pq, b=b, wdt=wdt, xbb=xbb: e.matmul(
                            pq, Wp[:, c, colo[b]:colo[b] + wdt], xbb[:, c, :], start=(c == 0), stop=(c == 15)),
                            reads=["Wp", rx], writes=[("psp", pb)])
                    tb, ob_ = tmpb[cnt % 2], pob[cnt % 2]
                    S.dve(lambda e, tb=tb, pq=pq, wdt=wdt: e.tensor_tensor(tb[:wdt], pq, rsb[:wdt], ALU.mult),
                          reads=[("psp", pb), "rsb"], writes=[("tmpb", cnt % 2)])
                    S.act(lambda e, tb=tb, ob_=ob_, wdt=wdt, b=b: e.activation(out=ob_[:wdt], in_=tb[:wdt], func=AF.Identity,
                                                                               bias=bias[:wdt, b:b + 1]),
                          reads=[("tmpb", cnt % 2), "bias"], writes=[("pob", cnt % 2)])
                    S.dma("sp", pT[b, :wdt, t0:t0 + tn], ob_[:wdt], reads=[("pob", cnt % 2)], writes=["pT"])
        S.barrier(dummy[:])

        off[0] = 0
        NT_ = [0]

        def T(par=2):
            r = []
            for _ in range(par):
                NT_[0] += 1
                r.append((carve(128), "t%d" % NT_[0]))
            return r

        names = ["H", "R", "u", "cosb", "sinb", "kkraw", "sq", "rn", "kk", "th", "sig", "cs", "E", "Ex", "gin", "gex", "ginv",
                 "a", "t1", "kd", "b", "bt", "at", "kt", "rt", "vtok", "atok", "ktok", "zT", "sgd", "gtok",
                 "qr", "kr", "qx", "vr", "krz", "PT", "ytok", "yret", "yfwb", "o1", "o2", "gsil", "gtot", "stmp"]
        tl = {}
        for nme in names:
            if nme == "H":
                tl[nme] = []
                for _ in range(2):
                    NT_[0] += 1
                    tl[nme].append((carve(7 * 256).rearrange("p (b t) -> p b t", b=7), "t%d" % NT_[0]))
            elif nme in ("R", "u"):
                nb = 6 if nme == "R" else 7
                tl[nme] = []
                for _ in range(2):
                    NT_[0] += 1
                    tl[nme].append((carve(nb * 128).rearrange("p (b t) -> p b t", b=nb), "t%d" % NT_[0]))
            elif nme in ("atok", "ktok"):
                tl[nme] = []
                for _ in range(2):
                    NT_[0] += 1
                    tl[nme].append((carve(256).rearrange("p (h k) -> p h k", h=2), "t%d" % NT_[0]))
            elif nme == "yfwb":
                tl[nme] = []
                for _ in range(2):
                    NT_[0] += 1
                    tl[nme].append((carve(260), "t%d" % NT_[0]))
            else:
                tl[nme] = T(2)
        hn = ["N", "A", "BK", "RA", "RK", "Np", "Ap", "P", "Q", "Np2", "Ap2", "P2", "Q2", "RHS", "Us"]
        th_ = {nme: [T(2), T(2)] for nme in hn}
        gCr = carve(4)
        tot = carve(4)
        sm = carve(64)
        bsv = carve(4)
        for nme in ("atok", "ktok"):
            for (ap, rn) in tl[nme]:
                S.pool(lambda e, ap=ap: e.memset(ap, 0.0), writes=[rn])

        pqc = [0]

        def PQ():
            i = pqc[0] % 6
            pqc[0] += 1
            return ps[i][:, 0:128], ("pq", i)

        def mm(o, ro, lhsT, rl, rhs, rr, start=True, stop=True):
            S.pe(lambda e: e.matmul(o, lhsT, rhs, start=start, stop=stop), reads=list(rl) + list(rr), writes=[ro])

        def tr(o, ro, in_, ri):
            S.pe(lambda e: e.transpose(o, in_, ident), reads=[ri, "cst"], writes=[ro])

        def TT(eng, o, ro, a, ra, b, rb, op):
            S.add(eng, lambda e: e.tensor_tensor(o, a, b, op), reads=[ra, rb], writes=[ro])

        def TS(eng, o, ro, a, ra, s1, s2, op0, op1=None, extra=()):
            if op1 is None:
                S.add(eng, lambda e: e.tensor_scalar(o, a, s1, None, op0), reads=[ra] + list(extra), writes=[ro])
            else:
                S.add(eng, lambda e: e.tensor_scalar(o, a, s1, s2, op0, op1), reads=[ra] + list(extra), writes=[ro])

        def STT(eng, o, ro, a, ra, sc, b, rb, op0, op1, extra=()):
            S.add(eng, lambda e: e.scalar_tensor_tensor(out=o, in0=a, scalar=sc, in1=b, op0=op0, op1=op1),
                  reads=[ra, rb] + list(extra), writes=[ro])

        def ACT(o, ro, a, ra, func, bias=None, scale=None, extra=(), accum=None):
            kw = {}
            if bias is not None:
                kw["bias"] = bias
            if scale is not None:
                kw["scale"] = scale
            if accum is not None:
                kw["accum_out"] = accum
            S.act(lambda e: e.activation(out=o, in_=a, func=func, **kw), reads=[ra] + list(extra), writes=[ro])

        chunks_f = [(0, "c", 0), (128, "c", 1)] + [(256 + 128 * i, "x", i) for i in range(128)]
        chunks_b = [(128, "c", 1), (0, "c", 0)] + [(256 + 128 * i, "x", i) for i in reversed(range(128))]
        CD = cst

        it = 0
        for d, chunks in ((0, chunks_f), (1, chunks_b)):
            S.dve(lambda e: e.memset(state, 0.0), writes=["state"])
            S.dve(lambda e: e.memset(Sret, 0.0), writes=["Sret"])
            Mst = CD[:, C_MLT:C_MLT + 128] if d == 0 else CD[:, C_MGT:C_MGT + 128]
            MstT = CD[:, C_MGT:C_MGT + 128] if d == 0 else CD[:, C_MLT:C_MLT + 128]
            Min = CD[:, C_MLE:C_MLE + 128] if d == 0 else CD[:, C_MGE:C_MGE + 128]
            for (t0, kind, ci) in chunks:
                par = it % 2
                it += 1

                def g(nme):
                    return tl[nme][par]
                H, rH = g("H")
                R_, rR = g("R")
                u, ru = g("u")
                if kind == "x":
                    lo_ok = ci > 0
                    hi_ok = ci < 127
                else:
                    lo_ok = ci > 0
                    hi_ok = ci < 1
                if not (lo_ok and hi_ok):
                    S.pool(lambda e, H=H: e.memset(H, 0.0), writes=[rH])
                a0 = t0 - (64 if lo_ok else 0)
                a1 = t0 + 128 + (64 if hi_ok else 0)
                h0 = 64 - (t0 - a0)
                S.dma("sp", H[:, 0:6, h0:h0 + (a1 - a0)], pT[0:6, :, a0:a1].rearrange("b p t -> p b t"), writes=[rH])
                S.dma("sp", H[0:32, 6, h0:h0 + (a1 - a0)], pT[6, 0:32, a0:a1], writes=[rH])
                S.dma("act", R_, pT[7:13, :, t0:t0 + 128].rearrange("b p t -> p b t"), writes=[rR])
                stm, rstm = g("stmp")
                for b in range(7):
                    np_ = colw[b]
                    Hc = H[:np_, b, 64:192]
                    ub = u[:np_, b, :]
                    tb_ = stm[:np_, :]

                    def sh(dst, src, tdst, mcol, b=b, np_=np_):
                        S.pool(lambda e: e.tensor_scalar(tdst, src, mus[:np_, b, mcol:mcol + 1], None, ALU.mult),
                               reads=[rH, "mus"], writes=[rstm])
                        S.pool(lambda e: e.tensor_tensor(dst, dst, tdst, ALU.add), reads=[ru, rstm], writes=[ru])
                    if kind == "x":
                        S.pool(lambda e, ub=ub, Hc=Hc, b=b, np_=np_: e.tensor_scalar(ub, Hc, c0[:np_, b:b + 1], None, ALU.mult),
                               reads=[rH, "c0"], writes=[ru])
                        u3 = ub.rearrange("p (r c) -> p r c", r=2)
                        H3 = Hc.rearrange("p (r c) -> p r c", r=2)
                        t3 = tb_.rearrange("p (r c) -> p r c", r=2)
                        sh(u3[:, :, 1:64], H3[:, :, 0:63], t3[:, :, 1:64], 0)
                        sh(u3[:, :, 0:63], H3[:, :, 1:64], t3[:, :, 0:63], 1)
                        sh(ub, H[:np_, b, 0:128], tb_, 2)
                        sh(ub, H[:np_, b, 128:256], tb_, 3)
                    else:
                        S.pool(lambda e, ub=ub, Hc=Hc, b=b, np_=np_: e.tensor_scalar(ub, Hc, c0c[:np_, b:b + 1], None, ALU.mult),
                               reads=[rH, "c0c"], writes=[ru])
                        sh(ub, H[:np_, b, 63:191], tb_, 0)
                        sh(ub, H[:np_, b, 65:193], tb_, 1)
                rT, kT, vT = u[:, 0, :], u[:, 1, :], u[:, 2, :]
                hs = slice(d * 64, d * 64 + 64)
                kkraw, r1 = g("kkraw"); sqt, r2 = g("sq"); rn, r3 = g("rn"); kk, r4 = g("kk")
                TS("dve", kkraw, r1, kT, ru, pv[:, 4:5], None, ALU.mult, extra=["pv"])
                ACT(sqt, r2, kkraw, r1, AF.Square)
                pq, rq = PQ()
                mm(pq, rq, CD[:, C_BONES:C_BONES + 128], ["cst"], sqt, [r2])
                ACT(rn, r3, pq, rq, AF.Sqrt)
                TS("dve", rn, r3, rn, r3, 1e-12, None, ALU.max)
                S.dve(lambda e, rn=rn: e.reciprocal(rn, rn), reads=[r3], writes=[r3])
                TT("dve", kk, r4, kkraw, r1, rn, r3, ALU.mult)
                tht, r5 = g("th"); sig, r6 = g("sig"); cs, r7 = g("cs"); E, r8 = g("E"); Ex, r9 = g("Ex")
                gin, r10 = g("gin"); gex, r11 = g("gex"); ginv, r12 = g("ginv")
                ACT(tht[hs, :], r5, u[hs, 3, :], ru, AF.Tanh)
                pq, rq = PQ()
                mm(pq, rq, w2t[hs, :], ["w2t"], tht[hs, :], [r5])
                ACT(sig, r6, pq, rq, AF.Sigmoid, bias=pv[:, d:d + 1], extra=["pv"])
                S.dve(lambda e, cs=cs, sig=sig: e.tensor_tensor_scan(cs, ones, sig, 0.0, ALU.mult, ALU.add),
                      reads=[r6, "cst"], writes=[r7])
                totp = tot[:, par:par + 1]
                gCp = gCr[:, par:par + 1]
                if d == 0:
                    Et, rE = cs, r7
                    TT("dve", Ex, r9, cs, r7, sig, r6, ALU.subtract)
                    Ext, rEx = Ex, r9
                else:
                    TS("dve", Ex, r9, cs, r7, -1.0, cs[:, 127:128], ALU.mult, ALU.add)
                    TT("dve", E, r8, Ex, r9, sig, r6, ALU.add)
                    Et, rE, Ext, rEx = E, r8, Ex, r9
                ACT(gin, r10, Et, rE, AF.Exp, scale=-KAPPA)
                ACT(gex, r11, Ext, rEx, AF.Exp, scale=-KAPPA)
                ACT(ginv, r12, Et, rE, AF.Exp, scale=KAPPA)
                ACT(gCp, ("gC", par), cs[:, 127:128], r7, AF.Exp, scale=-KAPPA)
                at_, r13 = g("a"); t1, r14 = g("t1"); kd, r15 = g("kd"); bb, r16 = g("b")
                pq, rq = PQ()
                mm(pq, rq, a2t[hs, :], ["a2t"], u[hs, 4, :], [ru])
                ACT(at_, r13, pq, rq, AF.Sigmoid, bias=pv[:, 2 + d:3 + d], extra=["pv"])
                TS("dve", t1, r14, at_, r13, -1.0, pv[:, 5:6], ALU.add, ALU.mult, extra=["pv"])
                STT("dve", kd, r15, t1, r14, 1.0, kT, ru, ALU.add, ALU.mult)
                TT("dve", bb, r16, kk, r4, at_, r13, ALU.mult)
                bt, r17 = g("bt"); al, r18 = g("at"); kt, r19 = g("kt"); rt, r20 = g("rt")
                TT("dve", bt, r17, kk, r4, gex, r11, ALU.mult)
                STT("dve", al, r18, bb, r16, -1.0, ginv, r12, ALU.mult, ALU.mult)
                TT("dve", kt, r19, kd, r15, ginv, r12, ALU.mult)
                TT("dve", rt, r20, rT, ru, gin, r10, ALU.mult)
                vtok, r21 = g("vtok"); atok, r22 = g("atok"); ktok, r23 = g("ktok")
                pq, rq = PQ(); tr(pq, rq, vT, ru)
                S.act(lambda e, vtok=vtok, pq=pq: e.copy(vtok, pq), reads=[rq], writes=[r21])
                pq, rq = PQ(); tr(pq, rq, al, r18)
                S.act(lambda e, atok=atok, pq=pq: e.copy(atok[:, 0, 0:64], pq[:, 0:64]), reads=[rq], writes=[r22])
                S.dve(lambda e, atok=atok, pq=pq: e.tensor_copy(atok[:, 1, 64:128], pq[:, 64:128]), reads=[rq], writes=[r22])
                pq, rq = PQ(); tr(pq, rq, kt, r19)
                S.act(lambda e, ktok=ktok, pq=pq: e.copy(ktok[:, 0, 0:64], pq[:, 0:64]), reads=[rq], writes=[r23])
                S.dve(lambda e, ktok=ktok, pq=pq: e.tensor_copy(ktok[:, 1, 64:128], pq[:, 64:128]), reads=[rq], writes=[r23])
                zT, r24 = g("zT")
                STT("dve", zT, r24, rT, ru, pv[:, 6:7], kd, r15, ALU.mult, ALU.mult, extra=["pv"])
                pqb_, rqb_ = PQ()
                mm(pqb_[:, 0:2], rqb_, zT, [r24], CD[:, C_HSEL:C_HSEL + 2], ["cst"])
                pqb = bsv[:, 2 * par:2 * par + 2]
                rqb = ("bsv", par)
                S.act(lambda e, pqb=pqb, pqb_=pqb_: e.copy(pqb, pqb_[:, 0:2]), reads=[rqb_], writes=[rqb])
                yfb, r25 = g("yfwb")
                ytok, r26 = g("ytok")
                for h in range(2):
                    hp = slice(h * 64, h * 64 + 64)

                    def gh(nme):
                        return th_[nme][h][par]
                    N_, n1 = gh("N"); A_, n2 = gh("A"); BK, n3 = gh("BK"); RA, n4 = gh("RA"); RK, n5 = gh("RK")
                    pq, rq = PQ(); mm(pq, rq, al[hp, :], [r18], bt[hp, :], [r17])
                    TT("dve", N_, n1, pq, rq, Mst, "cst", ALU.mult)
                    pq, rq = PQ(); mm(pq, rq, bt[hp, :], [r17], al[hp, :], [r18])
                    TT("dve", A_, n2, pq, rq, MstT, "cst", ALU.mult)
                    pq, rq = PQ(); mm(pq, rq, kt[hp, :], [r19], bt[hp, :], [r17])
                    TT("dve", BK, n3, pq, rq, Mst, "cst", ALU.mult)
                    pq, rq = PQ(); mm(pq, rq, al[hp, :], [r18], rt[hp, :], [r20])
                    TT("dve", RA, n4, pq, rq, Min, "cst", ALU.mult)
                    pq, rq = PQ(); mm(pq, rq, kt[hp, :], [r19], rt[hp, :], [r20])
                    TT("dve", RK, n5, pq, rq, Min, "cst", ALU.mult)
                    Pc, nP = gh("P"); Qc, nQ = gh("Q")
                    TT("pool", Pc, nP, N_, n1, ident, "cst", ALU.add)
                    TT("pool", Qc, nQ, A_, n2, ident, "cst", ALU.add)
                    Npc, nNp, Apc, nAp = N_, n1, A_, n2
                    alt = [(gh("Np"), gh("Ap"), gh("P2"), gh("Q2")), (gh("Np2"), gh("Ap2"), gh("P"), gh("Q"))]
                    for l in range(1, 7):
                        (Npn, nNpn), (Apn, nApn), (Pn, nPn), (Qn, nQn) = alt[(l - 1) % 2]
                        pq, rq = PQ(); mm(pq, rq, Apc, [nAp], Npc, [nNp])
                        S.act(lambda e, Npn=Npn, pq=pq: e.copy(Npn, pq), reads=[rq], writes=[nNpn])
                        if l < 6:
                            pq, rq = PQ(); mm(pq, rq, Npc, [nNp], Apc, [nAp])
                            S.act(lambda e, Apn=Apn, pq=pq: e.copy(Apn, pq), reads=[rq], writes=[nApn])
                        pq, rq = PQ(); mm(pq, rq, Qc, [nQ], Npn, [nNpn])
                        TT("dve", Pn, nPn, pq, rq, Pc, nP, ALU.add)
                        if l < 6:
                            pq, rq = PQ(); mm(pq, rq, Pc, [nP], Apn, [nApn])
                            TT("dve", Qn, nQn, pq, rq, Qc, nQ, ALU.add)
                        Npc, nNp, Apc, nAp = Npn, nNpn, Apn, nApn
                        Pc, nP = Pn, nPn
                        if l < 6:
                            Qc, nQ = Qn, nQn
                    RHS, n6 = gh("RHS"); Us, n7 = gh("Us")
                    vh = vtok[:, hp]
                    pq, rq = PQ()
                    mm(pq[:, 0:64], rq, bt[hp, :], [r17], state[hp, 0:64], ["state"], start=True, stop=False)
                    mm(pq[:, 0:64], rq, BK, [n3], vh, [r21], start=False, stop=True)
                    S.act(lambda e, RHS=RHS, pq=pq: e.copy(RHS[:, 0:64], pq[:, 0:64]), reads=[rq], writes=[n6])
                    pq, rq = PQ()
                    mm(pq[:, 0:64], rq, Pc, [nP], RHS[:, 0:64], [n6])
                    S.act(lambda e, Us=Us, pq=pq: e.copy(Us[:, 0:64], pq[:, 0:64]), reads=[rq], writes=[n7])
                    pq, rq = PQ()
                    mm(pq[:, 0:64], rq, rt[hp, :], [r20], state[hp, 0:64], ["state"], start=True, stop=False)
                    mm(pq[:, 0:64], rq, RA, [n4], Us[:, 0:64], [n7], start=False, stop=False)
                    mm(pq[:, 0:64], rq, RK, [n5], vh, [r21], start=False, stop=True)
                    S.dve(lambda e, ytok=ytok, pq=pq, hp=hp: e.tensor_copy(ytok[:, hp], pq[:, 0:64]), reads=[rq], writes=[r26])
                    if h == 0:
                        pqs, rqs = ps[6 + par][:, 0:128], ("pqs", par)
                    mm(pqs[:, 0:64], rqs, atok[:, h, :], [r22], Us[:, 0:64], [n7], start=(h == 0), stop=False)
                    mm(pqs[:, 0:64], rqs, ktok[:, h, :], [r23], vh, [r21], start=False, stop=(h == 1))
                TS("dve", state[:, 0:64], "state", state[:, 0:64], "state", gCp, None, ALU.mult, extra=[("gC", par)])
                STT("dve", state[:, 0:64], "state", pqs[:, 0:64], rqs, gCp, state[:, 0:64], "state", ALU.mult, ALU.add,
                    extra=[("gC", par)])

                qT, kT2, vT2, gT, qsw, ksw = (R_[:, i, :] for i in range(6))
                qr, q1 = g("qr"); kr, q2 = g("kr"); qx, q3 = g("qx"); vr, q4 = g("vr"); krz, q5 = g("krz"); PT, q6 = g("PT")
                if kind == "x":
                    cb, q7 = g("cosb"); sb, q8 = g("sinb")
                    xp = t0 - NCTX
                    S.dma("sp", cb, cosT[:, xp:xp + 128], writes=[q7])
                    S.dma("sp", sb, sinT[:, xp:xp + 128], writes=[q8])
                    TT("pool", qr, q1, qT, rR, cb, q7, ALU.mult)
                    TT("pool", qx, q3, qsw, rR, sb, q8, ALU.mult)
                    TT("pool", qr, q1, qr, q1, qx, q3, ALU.add)
                    TT("pool", kr, q2, kT2, rR, cb, q7, ALU.mult)
                    TT("pool", qx, q3, ksw, rR, sb, q8, ALU.mult)
                    TT("pool", kr, q2, kr, q2, qx, q3, ALU.add)
                    qrr, krr = qr, kr
                    rqr, rkr = q1, q2
                else:
                    qrr, krr, rqr, rkr = qT, kT2, rR, rR
                TT("dve", qx, q3, qrr, rqr, xibc[d], "xibc", ALU.mult)
                pq, rq = PQ(); tr(pq, rq, vT2, rR)
                S.act(lambda e, vr=vr, pq=pq: e.copy(vr, pq), reads=[rq], writes=[q4])
                pq, rq = PQ(); tr(pq, rq, krr, rkr)
                TS("dve", krz, q5, pq, rq, zeta[:, d:d + 1], None, ALU.mult, extra=["zeta"])
                pq, rq = PQ(); mm(pq, rq, krr, [rkr], qrr, [rqr])
                TT("dve", PT, q6, pq, rq, dmT[d], "dmT", ALU.mult)
                yret, q9 = g("yret")
                pq, rq = PQ()
                mm(pq, rq, PT, [q6], vr, [q4], start=True, stop=False)
                mm(pq, rq, qx, [q3], Sret, ["Sret"], start=False, stop=True)
                S.act(lambda e, yret=yret, pq=pq: e.copy(yret, pq), reads=[rq], writes=[q9])
                pq, rq = PQ()
                mm(pq, rq, krz, [q5], vr, [q4])
                STT("dve", Sret, "Sret", Sret, "Sret", gC[:, d:d + 1], pq, rq, ALU.mult, ALU.add, extra=["gC"])

                if d == 0:
                    S.dve(lambda e, yfb=yfb, ytok=ytok: e.tensor_copy(yfb[:, 0:128], ytok), reads=[r26], writes=[r25])
                    S.dve(lambda e, yfb=yfb, yret=yret: e.tensor_copy(yfb[:, 128:256], yret), reads=[q9], writes=[r25])
                    S.dve(lambda e, yfb=yfb, pqb=pqb: e.tensor_copy(yfb[:, 256:258], pqb[:, 0:2]), reads=[rqb], writes=[r25])
                    S.dma("sp", yfw[t0:t0 + 128, :], yfb, reads=[r25], writes=["yfw"])
                else:
                    S.dma("act", yfb, yfw[t0:t0 + 128, :], writes=[r25])
                    o1, o1r = g("o1"); o2, o2r = g("o2"); gtok, gtr = g("gtok"); sgd, sgr = g("sgd"); gsil, gsr = g("gsil")
                    gtot, gtotr = g("gtot")
                    TT("dve", ytok, r26, ytok, r26, yfb[:, 0:128], r25, ALU.add)
                    y3 = ytok.rearrange("p (h v) -> p h v", h=2)
                    smr = ("sm", par)
                    so = par * 32
                    mean = sm[:, so:so + 2]; var = sm[:, so + 2:so + 4]; bsum = sm[:, so + 4:so + 6]
                    S.dve(lambda e, mean=mean, y3=y3: e.tensor_reduce(out=mean, in_=y3, axis=AX.X, op=ALU.add), reads=[r26], writes=[smr])
                    S.dve(lambda e, mean=mean: e.tensor_scalar(mean, mean, 1.0 / 64, None, ALU.mult), reads=[smr], writes=[smr])
                    o13 = o1.rearrange("p (h v) -> p h v", h=2)
                    o23 = o2.rearrange("p (h v) -> p h v", h=2)
                    S.dve(lambda e, o13=o13, y3=y3, mean=mean: e.tensor_tensor(o13, y3, mean.unsqueeze(2).broadcast_to([128, 2, 64]), ALU.subtract),
                          reads=[r26, smr], writes=[o1r])
                    TT("dve", o2, o2r, o1, o1r, o1, o1r, ALU.mult)
                    S.dve(lambda e, var=var, o23=o23: e.tensor_reduce(out=var, in_=o23, axis=AX.X, op=ALU.add), reads=[o2r], writes=[smr])
                    S.act(lambda e, var=var: e.activation(out=var, in_=var, func=AF.Sqrt, scale=1.0 / 64, bias=epsv[:, 1:2]), reads=[smr, "epsv"], writes=[smr])
                    S.dve(lambda e, var=var: e.reciprocal(var, var), reads=[smr], writes=[smr])
                    S.dve(lambda e, o13=o13, var=var: e.tensor_tensor(o13, o13, var.unsqueeze(2).broadcast_to([128, 2, 64]), ALU.mult),
                          reads=[o1r, smr], writes=[o1r])
                    TT("dve", o1, o1r, o1, o1r, lnw, "rowp", ALU.mult)
                    TT("dve", o1, o1r, o1, o1r, lnb, "rowp", ALU.add)
                    S.dve(lambda e, bsum=bsum, pqb=pqb, yfb=yfb: e.tensor_tensor(bsum, pqb[:, 0:2], yfb[:, 256:258], ALU.add),
                          reads=[rqb, r25], writes=[smr])
                    v3 = vtok.rearrange("p (h v) -> p h v", h=2)
                    S.dve(lambda e, o23=o23, v3=v3, bsum=bsum: e.tensor_tensor(o23, v3, bsum.unsqueeze(2).broadcast_to([128, 2, 64]), ALU.mult),
                          reads=[r21, smr], writes=[o2r])
                    TT("dve", o1, o1r, o1, o1r, o2, o2r, ALU.add)
                    ACT(sgd, sgr, u[:, 5, :], ru, AF.Sigmoid)
                    ACT(gsil[0:32, :], gsr, u[0:32, 6, :], ru, AF.Sigmoid)
                    pq, rq = PQ()
                    mm(pq, rq, sgd, [sgr], g2at, ["g2at"], start=True, stop=False)
                    mm(pq, rq, gsil[0:32, :], [gsr], g2bt[0:32, :], ["g2bt"], start=False, stop=True)
                    ob_, obr = g("gtok")
                    S.dve(lambda e, gtot=gtot, o1=o1, pq=pq: e.tensor_tensor(gtot, o1, pq, ALU.mult), reads=[o1r, rq], writes=[gtotr])
                    S.dma("sp", out[t0:t0 + 128, 0:128], gtot, reads=[gtotr], writes=["out"])
                    TT("dve", yret, q9, yret, q9, yfb[:, 128:256], r25, ALU.add)
                    ss = sm[:, so + 8:so + 9]
                    S.dve(lambda e, ss=ss: e.memset(ss, 0.0), writes=[smr])
                    ACT(o2, o2r, yret, q9, AF.Square, accum=ss, extra=[smr])
                    S.act(lambda e, ss=ss: e.activation(out=ss, in_=ss, func=AF.Sqrt, scale=1.0 / 128, bias=epsv[:, 0:1]), reads=[smr, o2r, "epsv"], writes=[smr])
                    S.dve(lambda e, ss=ss: e.reciprocal(ss, ss), reads=[smr], writes=[smr])
                    STT("dve", o2, o2r, yret, q9, ss, gnw, "rowp", ALU.mult, ALU.mult, extra=[smr])
                    pq, rq = PQ(); tr(pq, rq, gT, rR)
                    S.act(lambda e, ob_=ob_, pq=pq: e.activation(out=ob_, in_=pq, func=AF.Silu), reads=[rq], writes=[obr])
                    TT("dve", o2, o2r, o2, o2r, ob_, obr, ALU.mult)
                    S.dma("sp", out[t0:t0 + 128, 128:256], o2, reads=[o2r], writes=["out"])
            S.barrier(dummy[:])
        S.emit()
    return nc


D = 2048
NTB = 2080
NE = 32
TILES_B = [(0, 32)] + [(32 + 128 * i, 128) for i in range(16)]
EPS = 1e-6


def build_B(n_exp=NE):
    nc = bass.Bass("TRN2", target_bir_lowering=False)

    def din(name, shape):
        return nc.dram_tensor(name, shape, F32, kind="ExternalInput").ap()

    ymT = din("ymT", [D, NTB])
    xres = din("xres", [NTB, D])
    w_out = din("w_out", [D, D])
    w1 = din("w1", [NE, D, 2048])
    w2 = din("w2", [NE, 1024, D])
    rw = din("rw", [D, 32])
    rb = din("rb", [32])
    vecs = din("vecs", [10, D])
    ident_d = din("ident", [128, 128])
    out = nc.dram_tensor("out", [NTB, D], F32, kind="ExternalOutput").ap()
    outn = nc.dram_tensor("outn", [NTB, D], F32, kind="ExternalOutput").ap()
    xn = nc.dram_tensor("xn", [NTB, D], F32).ap()
    h2Td = nc.dram_tensor("h2Td", [D, NTB], F32).ap()

    with contextlib.ExitStack() as st:
        S = Sched(nc, st)
        UR = st.enter_context(nc.sbuf_tensor("UR", [128, 29440], F32R))
        UF = st.enter_context(nc.sbuf_tensor("UF", [128, 21024], F32))
        gates = st.enter_context(nc.sbuf_tensor("gates", [128, 17, 32], F32))
        small = st.enter_context(nc.sbuf_tensor("small", [128, 512], F32))
        ident = st.enter_context(nc.sbuf_tensor("identt", [128, 128], F32))
        dummy = st.enter_context(nc.sbuf_tensor("dummyb", [128, 8], F32))
        ps = [st.enter_context(nc.psum_tensor("ps%d" % i, [128, 512], F32)) for i in range(8)]

        off = [0]
        offr = [0]

        def carve(n):
            a = UF[:, off[0]:off[0] + n]
            off[0] += n
            assert off[0] <= 21024, off[0]
            return a

        def carver(n):
            a = UR[:, offr[0]:offr[0] + n]
            offr[0] += n
            assert offr[0] <= 29440, offr[0]
            return a

        S.dma("sp", ident[:], ident_d[:, :], writes=["ident"])
        epsb = small[:, 500:501]
        S.dve(lambda e: e.memset(small[:, 500:501], EPS), writes=["epsb"])

        wo = [carver(16 * 512).rearrange("p (c n) -> p c n", c=16) for _ in range(2)]
        ym = [carver(16 * 128).rearrange("p (c t) -> p c t", c=16) for _ in range(2)]
        xg = [carve(512) for _ in range(2)]
        ob = [carve(512) for _ in range(2)]
        bc = {k: carve(2048) for k in ("gt1x", "gt1c", "gscx", "gscc", "sh2x", "sh2c")}
        xt1 = carve(2048)
        xt = [xt1, xt1]
        h2 = carve(2048)
        tmpv = h2
        h2T1 = carve(16 * 128).rearrange("p (c t) -> p c t", c=16)
        h2T = [h2T1, h2T1]
        rws = carve(16 * 32).rearrange("p (c e) -> p c e", c=16)
        rbb = carve(32)

        vidx = {"gt1x": 0, "gt1c": 1, "sh2x": 2, "sh2c": 3, "sc2x": 4, "sc2c": 5, "gt2x": 6, "gt2c": 7, "n2g": 8, "fg": 9}
        for k in ("gt1x", "gt1c", "sh2x", "sh2c"):
            S.dma("sp", bc[k], vecs[vidx[k]].partition_broadcast(128), writes=["bc_" + k])
        S.dma("sp", tmpv, vecs[8].partition_broadcast(128), writes=["h2"])
        for k, sck in (("gscx", "sc2x"), ("gscc", "sc2c")):
            S.dma("sp", bc[k], vecs[vidx[sck]].partition_broadcast(128), writes=["bc_" + k])
            S.dve(lambda e, k=k: e.scalar_tensor_tensor(out=bc[k], in0=bc[k], scalar=1.0, in1=tmpv,
                                                        op0=ALU.add, op1=ALU.mult),
                  reads=["bc_" + k, "h2"], writes=["bc_" + k])
        S.dma("sp", rws, rw.rearrange("(c p) e -> p c e", p=128), writes=["rws"])
        S.dma("sp", rbb, rb.partition_broadcast(128), writes=["rbb"])

        k = 0
        for g in range(4):
            gs = slice(g * 512, (g + 1) * 512)
            wob = wo[g % 2]
            S.dma("pool", wob, w_out[:, gs].rearrange("(c p) n -> p c n", p=128), writes=[("wo", g % 2)])
            for ti, (r0, nt) in enumerate(TILES_B):
                kb = k % 2
                ymb, xgb, obb, psb = ym[kb], xg[kb], ob[kb], ps[kb]
                S.dma("pool", ymb[:, :, :nt], ymT[:, r0:r0 + nt].rearrange("(c p) t -> p c t", p=128),
                      writes=[("ym", kb)])
                S.dma("sp", xgb[:nt], xres[r0:r0 + nt, gs], writes=[("xg", kb)])
                for c in range(16):
                    S.pe(lambda e, c=c, ymb=ymb, wob=wob, psb=psb, nt=nt: e.matmul(
                        psb[:nt, :], ymb[:, c, :nt], wob[:, c, :], start=(c == 0), stop=(c == 15)),
                        reads=[("ym", kb), ("wo", g % 2)], writes=[("ps", kb)])
                gtb = bc["gt1c"] if ti == 0 else bc["gt1x"]
                S.dve(lambda e, obb=obb, psb=psb, gtb=gtb, nt=nt, gs=gs: e.tensor_tensor(
                    obb[:nt], psb[:nt, :], gtb[:nt, gs], ALU.mult),
                    reads=[("ps", kb), "bc_gt1c", "bc_gt1x"], writes=[("ob", kb)])
                S.dve(lambda e, obb=obb, xgb=xgb, nt=nt: e.tensor_tensor(obb[:nt], obb[:nt], xgb[:nt], ALU.add),
                      reads=[("ob", kb), ("xg", kb)], writes=[("ob", kb)])
                S.dma("act", xn[r0:r0 + nt, gs], obb[:nt], reads=[("ob", kb)], writes=[("xn", ti)])
                k += 1

        for ti, (r0, nt) in enumerate(TILES_B):
            kb = 0
            xtb, h2Tb = xt[kb], h2T[kb]
            sfx = "c" if ti == 0 else "x"
            S.dma("sp", xtb[:nt], xn[r0:r0 + nt, :], reads=[("xn", ti)], writes=[("xt", kb)])
            ss = small[:, 0:1]
            rstd = small[:, 1:2]
            S.dve(lambda e: e.memset(small[:, 0:2], 0.0), writes=["ss", "rstd"])
            S.act(lambda e, xtb=xtb, nt=nt: e.activation(out=h2[:nt], in_=xtb[:nt], func=AF.Square,
                                                         accum_out=small[:nt, 0:1]),
                  reads=[("xt", kb), "ss"], writes=["h2", "ss"])
            S.act(lambda e, nt=nt: e.activation(out=small[:nt, 1:2], in_=small[:nt, 0:1], func=AF.Sqrt, scale=1.0 / D, bias=epsb[:nt, 0:1]),
                  reads=["ss", "epsb"], writes=["rstd"])
            S.dve(lambda e, nt=nt: e.reciprocal(small[:nt, 1:2], small[:nt, 1:2]),
                  reads=["rstd"], writes=["rstd"])
            S.dve(lambda e, xtb=xtb, nt=nt, sfx=sfx: e.scalar_tensor_tensor(
                out=h2[:nt], in0=xtb[:nt], scalar=small[:nt, 1:2], in1=bc["gsc" + sfx][:nt],
                op0=ALU.mult, op1=ALU.mult),
                reads=[("xt", kb), "rstd", "bc_gscx", "bc_gscc"], writes=["h2"])
            S.dve(lambda e, nt=nt, sfx=sfx: e.tensor_tensor(h2[:nt], h2[:nt], bc["sh2" + sfx][:nt], ALU.add),
                  reads=["h2", "bc_sh2x", "bc_sh2c"], writes=["h2"])
            for c in range(16):
                q = 2 + c // 4
                S.pe(lambda e, c=c, q=q, nt=nt: e.transpose(
                    ps[q][:, (c % 4) * 128:(c % 4) * 128 + nt], h2[:nt, c * 128:(c + 1) * 128], ident[:nt, :nt]),
                    reads=["h2", "ident"], writes=[("ps", q)])
            for q in range(4):
                src = ps[2 + q][:, :].rearrange("p (c t) -> p c t", c=4)[:, :, :nt]
                dst = h2Tb[:, 4 * q:4 * q + 4, :nt]
                if q % 2 == 0:
                    S.act(lambda e, src=src, dst=dst: e.copy(dst, src), reads=[("ps", 2 + q)], writes=[("h2T", kb)])
                else:
                    S.dve(lambda e, src=src, dst=dst: e.tensor_copy(dst, src), reads=[("ps", 2 + q)],
                          writes=[("h2T", kb)])
            for c in range(16):
                S.pe(lambda e, c=c, h2Tb=h2Tb, nt=nt: e.matmul(
                    ps[6][:nt, 0:32], h2Tb[:, c, :nt], rws[:, c, :], start=(c == 0), stop=(c == 15)),
                    reads=[("h2T", kb), "rws"], writes=[("ps", 6)])
            S.dma("act", h2Td[:, r0:r0 + nt].rearrange("(c p) t -> p c t", p=128), h2Tb[:, :, :nt],
                  reads=[("h2T", kb)], writes=[("h2Td", ti)])
            sc = small[:, 32:64]
            bi = small[:, 64:96]
            eq = small[:, 96:128]
            mk = small[:, 128:160]
            sel = small[:, 160:192]
            ww = small[:, 192:224]
            m1 = small[:, 224:228]
            m2 = small[:, 228:232]
            gsum = small[:, 232:236]
            goh = small[:, 236:240]
            gm = small[:, 240:241]
            wsum = small[:, 241:242]

            def v3(a, nt):
                return a[:nt].rearrange("p (g e) -> p g e", g=4)

            def b3(a, nt):
                return a[:nt].unsqueeze(2).broadcast_to([nt, 4, 8])

            R = ["rt"]
            S.act(lambda e, nt=nt: e.activation(out=sc[:nt], in_=ps[6][:nt, 0:32], func=AF.Sigmoid),
                  reads=[("ps", 6)], writes=R)
            S.dve(lambda e, nt=nt: e.tensor_tensor(bi[:nt], sc[:nt], rbb[:nt], ALU.add), reads=R + ["rbb"], writes=R)
            S.dve(lambda e, nt=nt: e.tensor_reduce(out=m1[:nt], in_=v3(bi, nt), axis=AX.X, op=ALU.max), reads=R, writes=R)
            S.dve(lambda e, nt=nt: e.tensor_tensor(v3(eq, nt), v3(bi, nt), b3(m1, nt), ALU.is_equal), reads=R, writes=R)
            S.dve(lambda e, nt=nt: e.scalar_tensor_tensor(out=mk[:nt], in0=eq[:nt], scalar=-1e9, in1=bi[:nt],
                                                          op0=ALU.mult, op1=ALU.add), reads=R, writes=R)
            S.dve(lambda e, nt=nt: e.tensor_reduce(out=m2[:nt], in_=v3(mk, nt), axis=AX.X, op=ALU.max), reads=R, writes=R)
            S.dve(lambda e, nt=nt: e.tensor_tensor(gsum[:nt], m1[:nt], m2[:nt], ALU.add), reads=R, writes=R)
            S.dve(lambda e, nt=nt: e.tensor_reduce(out=gm[:nt], in_=gsum[:nt], axis=AX.X, op=ALU.max), reads=R, writes=R)
            S.dve(lambda e, nt=nt: e.tensor_scalar(goh[:nt], gsum[:nt], gm[:nt, 0:1], None, ALU.is_equal), reads=R, writes=R)
            S.dve(lambda e, nt=nt: e.tensor_tensor(v3(sel, nt), v3(bi, nt), b3(m2, nt), ALU.is_ge), reads=R, writes=R)
            S.dve(lambda e, nt=nt: e.tensor_tensor(v3(sel, nt), v3(sel, nt), b3(goh, nt), ALU.mult), reads=R, writes=R)
            S.dve(lambda e, nt=nt: e.tensor_tensor(ww[:nt], sc[:nt], sel[:nt], ALU.mult), reads=R, writes=R)
            S.dve(lambda e, nt=nt: e.tensor_reduce(out=wsum[:nt], in_=ww[:nt], axis=AX.X, op=ALU.add), reads=R, writes=R)
            S.dve(lambda e, nt=nt: e.reciprocal(wsum[:nt], wsum[:nt]), reads=R, writes=R)
            S.dve(lambda e, nt=nt, ti=ti: e.tensor_scalar(gates[:nt, ti, :], ww[:nt], wsum[:nt, 0:1], None, ALU.mult),
                  reads=R, writes=["gates"])

        S.barrier(dummy[:])

        off[0] = 0
        offr[0] = 0
        h2Tp = carver(16 * 544).rearrange("p (c t) -> p c t", c=16)
        acc = carve(5 * 2048).rearrange("p (j n) -> p j n", j=5)
        w1g = [carver(16 * 128).rearrange("p (c f) -> p c f", c=16) for _ in range(2)]
        w1u = [carver(16 * 128).rearrange("p (c f) -> p c f", c=16) for _ in range(2)]
        w2g = [carver(8 * 512).rearrange("p (f n) -> p f n", f=8) for _ in range(2)]
        actT = carver(8 * 544).rearrange("p (f t) -> p f t", f=8)
        xt2 = carve(2048)
        bc2 = {k: carve(2048) for k in ("gt2x", "gt2c", "fg")}
        S.dma("sp", bc2["gt2x"], vecs[6].partition_broadcast(128), writes=["bc_gt2x"])
        S.dma("sp", bc2["gt2c"], vecs[7].partition_broadcast(128), writes=["bc_gt2c"])
        S.dma("sp", bc2["fg"], vecs[9].partition_broadcast(128), writes=["bc_fg"])

        pi = 0
        w2c = 0
        yc = 0
        for p in range(4):
            ptiles = list(range(0, 5)) if p == 0 else list(range(4 * p + 1, 4 * p + 5))
            t0 = TILES_B[ptiles[0]][0]
            T = sum(TILES_B[t][1] for t in ptiles)
            chunks = [(0, 512), (512, 32)] if p == 0 else [(0, 512)]
            S.dma("pool", h2Tp[:, :, :T], h2Td[:, t0:t0 + T].rearrange("(c p) t -> p c t", p=128),
                  reads=[("h2Td", t) for t in ptiles], writes=["h2Tp"])
            S.pool(lambda e: e.memset(acc, 0.0), writes=[("acc", j) for j in range(5)])
            for ex in range(n_exp):
                w2bufs = {}
                for fb in range(8):
                    b = pi % 2
                    w1gb, w1ub = w1g[b], w1u[b]
                    S.dma("pool", w1gb, w1[ex, :, fb * 128:(fb + 1) * 128].rearrange("(c p) f -> p c f", p=128),
                          writes=[("w1g", b)])
                    S.dma("pool", w1ub, w1[ex, :, 1024 + fb * 128:1024 + (fb + 1) * 128].rearrange("(c p) f -> p c f", p=128),
                          writes=[("w1u", b)])
                    for ci, (c0, cn) in enumerate(chunks):
                        if ci == 0:
                            psg, psu = ps[(pi % 2) * 2][:, 0:cn], ps[(pi % 2) * 2 + 1][:, 0:cn]
                            rg, ru = ("ps", (pi % 2) * 2), ("ps", (pi % 2) * 2 + 1)
                        else:
                            o = (pi % 2) * 64
                            psg, psu = ps[4][:, o:o + 32], ps[4][:, o + 32:o + 64]
                            rg, ru = ("ps", 4), ("ps", 4)
                        for c in range(16):
                            S.pe(lambda e, c=c, psg=psg, w1gb=w1gb, c0=c0, cn=cn: e.matmul(
                                psg, w1gb[:, c, :], h2Tp[:, c, c0:c0 + cn], start=(c == 0), stop=(c == 15)),
                                reads=[("w1g", b), "h2Tp"], writes=[rg])
                        for c in range(16):
                            S.pe(lambda e, c=c, psu=psu, w1ub=w1ub, c0=c0, cn=cn: e.matmul(
                                psu, w1ub[:, c, :], h2Tp[:, c, c0:c0 + cn], start=(c == 0), stop=(c == 15)),
                                reads=[("w1u", b), "h2Tp"], writes=[ru])
                        dst = actT[:, fb, c0:c0 + cn]
                        S.act(lambda e, dst=dst, psg=psg: e.activation(out=dst, in_=psg, func=AF.Silu),
                              reads=[rg], writes=[("actT", fb, ci)])
                        S.dve(lambda e, dst=dst, psu=psu: e.tensor_tensor(dst, dst, psu, ALU.mult),
                              reads=[ru, ("actT", fb, ci)], writes=[("actT", fb, ci)])
                    pi += 1
                for grp in range(4):
                    wb = w2c % 2
                    w2c += 1
                    w2b = w2g[wb]
                    S.dma("pool", w2b, w2[ex, :, grp * 512:(grp + 1) * 512].rearrange("(f p) n -> p f n", p=128),
                          writes=[("w2g", wb)])
                    tt0 = 0
                    for j, t in enumerate(ptiles):
                        nt = TILES_B[t][1]
                        psy = ps[5 + yc % 2]
                        ry = ("ps", 5 + yc % 2)
                        yc += 1
                        for fb in range(8):
                            S.pe(lambda e, fb=fb, psy=psy, nt=nt, tt0=tt0, w2b=w2b: e.matmul(
                                psy[:nt, :], actT[:, fb, tt0:tt0 + nt], w2b[:, fb, :], start=(fb == 0), stop=(fb == 7)),
                                reads=[("actT", fb, 0), ("actT", fb, 1), ("w2g", wb)], writes=[ry])
                        gsl = slice(grp * 512, (grp + 1) * 512)
                        S.dve(lambda e, psy=psy, nt=nt, j=j, t=t, ex=ex, gsl=gsl: e.scalar_tensor_tensor(
                            out=acc[:nt, j, gsl], in0=psy[:nt, :], scalar=gates[:nt, t, ex:ex + 1],
                            in1=acc[:nt, j, gsl], op0=ALU.mult, op1=ALU.add),
                            reads=[ry, "gates", ("acc", j)], writes=[("acc", j)])
                        tt0 += nt
            for j, t in enumerate(ptiles):
                r0, nt = TILES_B[t]
                sfx = "c" if t == 0 else "x"
                S.dma("sp", xt2[:nt], xn[r0:r0 + nt, :], reads=[("xn", t)], writes=["xt2"])
                S.dve(lambda e, nt=nt, j=j, sfx=sfx: e.tensor_tensor(acc[:nt, j, :], acc[:nt, j, :], bc2["gt2" + sfx][:nt], ALU.mult),
                      reads=[("acc", j), "bc_gt2x", "bc_gt2c"], writes=[("acc", j)])
                S.dve(lambda e, nt=nt, j=j: e.tensor_tensor(acc[:nt, j, :], acc[:nt, j, :], xt2[:nt], ALU.add),
                      reads=[("acc", j), "xt2"], writes=[("acc", j)])
                S.dma("sp", out[r0:r0 + nt, :], acc[:nt, j, :], reads=[("acc", j)], writes=[("out", t)])
                S.dve(lambda e: e.memset(small[:, 0:2], 0.0), writes=["ss", "rstd"])
                S.act(lambda e, nt=nt, j=j: e.activation(out=xt2[:nt], in_=acc[:nt, j, :], func=AF.Square,
                                                         accum_out=small[:nt, 0:1]),
                      reads=[("acc", j), "ss"], writes=["xt2", "ss"])
                S.act(lambda e, nt=nt: e.activation(out=small[:nt, 1:2], in_=small[:nt, 0:1], func=AF.Sqrt, scale=1.0 / D, bias=epsb[:nt, 0:1]),
                      reads=["ss", "epsb"], writes=["rstd"])
                S.dve(lambda e, nt=nt: e.reciprocal(small[:nt, 1:2], small[:nt, 1:2]),
                      reads=["rstd"], writes=["rstd"])
                S.dve(lambda e, nt=nt, j=j: e.scalar_tensor_tensor(
                    out=xt2[:nt], in0=acc[:nt, j, :], scalar=small[:nt, 1:2], in1=bc2["fg"][:nt],
                    op0=ALU.mult, op1=ALU.mult),
                    reads=[("acc", j), "rstd", "bc_fg"], writes=["xt2"])
                S.dma("sp", outn[r0:r0 + nt, :], xt2[:nt], reads=["xt2"], writes=[("outn", t)])
        S.emit()
    return nc


def build_M():
    nc = bass.Bass("TRN2", target_bir_lowering=False)
    aw = nc.dram_tensor("aw", [2, 2048, 1536], F32, kind="ExternalInput").ap()
    ab = nc.dram_tensor("ab", [2, 1536], F32, kind="ExternalInput").ap()
    cc = nc.dram_tensor("cc", [128, 16, 2], F32, kind="ExternalInput").ap()
    mo = nc.dram_tensor("mo", [2, 2, 1536], F32, kind="ExternalOutput").ap()
    with contextlib.ExitStack() as st:
        S = Sched(nc, st)
        cct = st.enter_context(nc.sbuf_tensor("cct", [128, 16, 2], F32))
        sct = st.enter_context(nc.sbuf_tensor("sct", [128, 16, 2], F32))
        awt = [st.enter_context(nc.sbuf_tensor("awt%d" % i, [128, 16, 512], F32)) for i in range(2)]
        bt = [st.enter_context(nc.sbuf_tensor("bt%d" % i, [2, 512], F32)) for i in range(2)]
        ot = [st.enter_context(nc.sbuf_tensor("ot%d" % i, [2, 512], F32)) for i in range(2)]
        ps = [st.enter_context(nc.psum_tensor("psm%d" % i, [128, 512], F32)) for i in range(2)]
        S.dma("sp", cct[:], cc[:, :, :], writes=["cct"])
        S.act(lambda e: e.activation(out=sct[:], in_=cct[:], func=AF.Silu), reads=["cct"], writes=["sct"])
        k = 0
        for l in range(2):
            for g in range(3):
                b = k % 2
                k += 1
                gs = slice(g * 512, (g + 1) * 512)
                S.dma("sp" if b == 0 else "act", awt[b][:], aw[l, :, gs].rearrange("(c p) n -> p c n", p=128), writes=[("awt", b)])
                S.dma("sp", bt[b][:], ab[l, gs].partition_broadcast(2), writes=[("bt", b)])
                for c in range(16):
                    S.pe(lambda e, c=c, b=b: e.matmul(ps[b][0:2, :], sct[:, c, :], awt[b][:, c, :], start=(c == 0), stop=(c == 15)),
                         reads=["sct", ("awt", b)], writes=[("ps", b)])
                S.dve(lambda e, b=b: e.tensor_tensor(ot[b][:], ps[b][0:2, :], bt[b][:], ALU.add),
                      reads=[("ps", b), ("bt", b)], writes=[("ot", b)])
                S.dma("sp", mo[l, :, gs], ot[b][:], reads=[("ot", b)], writes=["mo"])
        S.emit()
    return nc

import numpy as np

RWKV_IN = 3488


def rope_tables():
    half = 64
    inv = (10000.0 ** (-np.arange(0, half, 2, dtype=np.float32) / half)).astype(np.float32)
    t = np.arange(16384)
    posr = (t // 64).astype(np.float32)
    posc = (t % 64).astype(np.float32)
    cosT = np.zeros((128, 16384), np.float32)
    sinT = np.zeros((128, 16384), np.float32)
    for hb, pos in ((0, posr), (64, posc)):
        ang = (pos[None, :] * inv[:, None]).astype(np.float32)
        c, s = np.cos(ang).astype(np.float32), np.sin(ang).astype(np.float32)
        cosT[hb:hb + 32] = c
        cosT[hb + 32:hb + 64] = c
        sinT[hb:hb + 32] = -s
        sinT[hb + 32:hb + 64] = s
    return cosT, sinT


SWAP = np.concatenate([np.arange(32, 64), np.arange(0, 32), np.arange(96, 128), np.arange(64, 96)])


def col_blocks(c):
    oc = np.arange(c * 128, c * 128 + 128)
    blocks = [oc, 1024 + oc, 2048 + oc, np.arange(3072, 3200), np.arange(3200, 3328), np.arange(3328, 3456),
              np.arange(3456, 3488), RWKV_IN + oc, RWKV_IN + 1024 + oc, RWKV_IN + 2048 + oc, RWKV_IN + 3072 + oc,
              RWKV_IN + oc[SWAP], RWKV_IN + 1024 + oc[SWAP]]
    return blocks


def fp(v):
    return np.ascontiguousarray(v.reshape(16, 128).T)


def prep_A(inp, l, c, xT, mod_x, mod_c, tables, cst):
    blocks = col_blocks(c)
    cols = np.concatenate(blocks)
    sh1, sc1 = mod_x[0:2048], mod_x[2048:4096]
    csh1, csc1 = mod_c[0:2048], mod_c[2048:4096]
    mvec = np.stack([fp(sh1), fp(sc1), fp(csh1), fp(csc1), fp(inp["norm1_g"][l])], axis=1).astype(np.float32)
    mu = np.zeros((128, 7, 4), np.float32)
    for b in range(7):
        cb = blocks[b]
        mu[:len(cb), b, :] = inp["shift_mu"][l][:, cb].T
    oc = np.arange(c * 128, c * 128 + 128)
    pvec = np.zeros((128, 8), np.float32)
    pvec[:, 0] = inp["rwkv_w0"][l, 0, oc]
    pvec[:, 1] = inp["rwkv_w0"][l, 1, oc]
    pvec[:, 2] = inp["rwkv_a0"][l, 0, oc]
    pvec[:, 3] = inp["rwkv_a0"][l, 1, oc]
    pvec[:, 4] = inp["rwkv_k_k"][l, oc]
    pvec[:, 5] = inp["rwkv_k_a"][l, oc]
    pvec[:, 6] = inp["rwkv_r_k"][l].reshape(-1)[oc]
    w2s = np.concatenate([inp["rwkv_w2"][l, 0][:, oc], inp["rwkv_w2"][l, 1][:, oc]], 0)
    a2s = np.concatenate([inp["rwkv_a2"][l, 0][:, oc], inp["rwkv_a2"][l, 1][:, oc]], 0)
    g2 = inp["rwkv_g2"][l]
    rowv = np.stack([inp["rwkv_lnx_w"][l, oc], inp["rwkv_lnx_b"][l, oc], inp["ret_gn_w"][l, oc]])
    return {
        "xT": xT, "W": np.ascontiguousarray(inp["w_in"][l][:, cols]), "mvec": mvec, "mu": mu, "pvec": pvec,
        "w2s": np.ascontiguousarray(w2s), "a2s": np.ascontiguousarray(a2s),
        "g2a": np.ascontiguousarray(g2[0:128, oc]), "g2b": np.ascontiguousarray(g2[128:160, oc]),
        "rowv": np.ascontiguousarray(rowv.astype(np.float32)),
        "dec": np.ascontiguousarray(inp["ret_log2_decay"][l, :, c]),
        "cosT": tables[0], "sinT": tables[1], "cst": cst,
    }

def kernel(**inp):
    inp = {k: np.asarray(v) for k, v in inp.items()}
    x = inp["x"][0]
    ctx = inp["ctx"][0]
    cores = list(range(8))
    ncM = build_M()
    cc = np.ascontiguousarray(np.stack([fp(inp["c"][0]), fp(inp["c_ctx"])], axis=2).astype(np.float32))
    ims = []
    for c in cores:
        cs = slice(c * 1536, (c + 1) * 1536)
        ims.append({"aw": np.ascontiguousarray(inp["ada_w"][:, :, cs]),
                    "ab": np.ascontiguousarray(inp["ada_b"][:, cs]), "cc": cc})
    res = run_bass_kernel_spmd(ncM, ims, core_ids=cores)
    mod = np.concatenate([r["mo"] for r in res.results], axis=2)
    del ims
    ncA = build_A()
    ncB = build_B()
    xin = np.concatenate([ctx, x], 0).astype(np.float32)
    tables = rope_tables()
    cst = make_cst()
    ident = np.eye(128, dtype=np.float32)
    rows_of = [np.concatenate([np.arange(32 * c, 32 * c + 32), 256 + np.arange(2048 * c, 2048 * c + 2048)]) for c in cores]
    for l in range(2):
        mod_x, mod_c = mod[l, 0], mod[l, 1]
        xT = np.ascontiguousarray(xin.T)
        imsA = [prep_A(inp, l, c, xT, mod_x, mod_c, tables, cst) for c in cores]
        resA = run_bass_kernel_spmd(ncA, imsA, core_ids=cores)
        del imsA, xT
        ymix = np.empty((NTOK, 2048), np.float32)
        for c in cores:
            o = resA.results[c]["out"]
            ymix[:, c * 128:(c + 1) * 128] = o[:, :128]
            ymix[:, 1024 + c * 128:1024 + (c + 1) * 128] = o[:, 128:]
        del resA
        sh1, sc1, gt1, sh2, sc2, gt2 = np.split(mod_x, 6)
        csh1, csc1, cgt1, csh2, csc2, cgt2 = np.split(mod_c, 6)
        vecs = np.ascontiguousarray(np.stack([gt1, cgt1, sh2, csh2, sc2, csc2, gt2, cgt2,
                                              inp["norm2_g"][l], inp["final_g"]]).astype(np.float32))
        imsB = []
        for c in cores:
            rows = rows_of[c]
            imsB.append({"ymT": np.ascontiguousarray(ymix[rows].T), "xres": np.ascontiguousarray(xin[rows]),
                         "w_out": inp["w_out"][l], "w1": inp["moe_w1"][l], "w2": inp["moe_w2"][l],
                         "rw": inp["router_w"], "rb": inp["router_b"], "vecs": vecs, "ident": ident})
        resB = run_bass_kernel_spmd(ncB, imsB, core_ids=cores)
        del imsB, ymix
        key = "outn" if l == 1 else "out"
        xnew = np.empty_like(xin)
        for c in cores:
            xnew[rows_of[c]] = resB.results[c][key]
        del resB
        xin = xnew
    return np.ascontiguousarray(xin[256:].reshape(1, 16384, 2048).astype(np.float32))
```

```python
import contextlib
import numpy as np
import concourse.bass as bass
import concourse.mybir as mybir
from concourse.bass_utils import run_bass_kernel_spmd

F32 = mybir.dt.float32
F32R = mybir.dt.float32r
AF = mybir.ActivationFunctionType
ALU = mybir.AluOpType
AX = mybir.AxisListType

ENGS = ("pe", "act", "dve", "pool", "sp")
DMAQ = ("sp", "act", "pool")
NDSEM = 6


class _Op:
    __slots__ = ("eng", "fn", "dma", "deps", "sig", "dsem", "dval", "consumed", "idx")

    def __init__(self, eng, fn, dma):
        self.eng = eng
        self.fn = fn
        self.dma = dma
        self.deps = ()
        self.sig = 0
        self.dsem = None
        self.dval = 0
        self.consumed = False
        self.idx = 0


class Sched:
    def __init__(self, nc, stack):
        self.nc = nc
        self.ops = {e: [] for e in ENGS}
        self.res = {}
        self.sem = {e: stack.enter_context(nc.semaphore("sem_" + e)) for e in ENGS}
        self.dsem = {e: [stack.enter_context(nc.semaphore("dsem_%s%d" % (e, i)))
                         for i in range(NDSEM)] for e in DMAQ}
        self.duse = {e: [None] * NDSEM for e in DMAQ}
        self.dcnt = {e: 0 for e in DMAQ}
        self.nops = 0
        self.rec = None
        self.cur = None

    def stage(self, name):
        self.cur = name

    def begin(self, names):
        self.rec = {n: [] for n in names}

    def end(self):
        r = self.rec
        self.rec = None
        return r

    def replay(self, lst):
        for t in lst:
            self.add(t[0], t[1], t[2], t[3], t[4])

    def add(self, eng, fn, reads=(), writes=(), dma=False, hold=False):
        if self.rec is not None:
            self.rec[self.cur].append((eng, fn, tuple(reads), tuple(writes), dma, hold))
            return None
        op = _Op(eng, fn, dma)
        op.idx = self.nops
        self.nops += 1
        deps = {}
        res = self.res
        if "PHASE" not in writes:
            reads = tuple(reads) + ("PHASE",)
        for r in reads:
            st = res.get(r)
            if st is None:
                st = res[r] = [None, {}]
            if st[0] is not None:
                deps[id(st[0])] = st[0]
        for w in writes:
            st = res.get(w)
            if st is None:
                st = res[w] = [None, {}]
            if st[0] is not None:
                deps[id(st[0])] = st[0]
            for rd in st[1].values():
                deps[id(rd)] = rd
        if dma:
            k = self.dcnt[eng] % NDSEM
            self.dcnt[eng] += 1
            prev = self.duse[eng][k]
            if prev is not None:
                deps[id(prev)] = prev
            op.dsem = (eng, k)
            op.dval = (prev.dval if prev is not None else 0) + 16
            self.duse[eng][k] = op
        for r in reads:
            st = res[r]
            key = (("d",) + op.dsem) if dma else eng
            st[1][key] = op
        for w in writes:
            res[w] = [op, {}]
        deps.pop(id(op), None)
        dl = []
        for d in deps.values():
            if (not d.dma) and d.eng == eng and eng == "pe":
                continue
            d.consumed = True
            dl.append(d)
        op.deps = dl
        self.ops[eng].append(op)
        return op

    def pe(self, fn, reads=(), writes=(), hold=False):
        return self.add("pe", fn, reads, writes, hold=hold)

    def act(self, fn, reads=(), writes=()):
        return self.add("act", fn, reads, writes)

    def dve(self, fn, reads=(), writes=()):
        return self.add("dve", fn, reads, writes)

    def pool(self, fn, reads=(), writes=()):
        return self.add("pool", fn, reads, writes)

    def barrier(self, tile):
        return self.add("dve", lambda e: e.memset(tile, 0.0), (), ("PHASE",))

    def dma(self, q, out, in_, reads=(), writes=()):
        return self.add(q, lambda e: e.dma_start(out=out, in_=in_), reads, writes, dma=True)

    def emit(self):
        nc = self.nc
        eobj = {"pe": nc.tensor, "act": nc.scalar, "dve": nc.vector, "pool": nc.gpsimd, "sp": nc.sync}
        for e in ENGS:
            n = 0
            for op in self.ops[e]:
                if op.consumed and not op.dma:
                    n += 1
                    op.sig = n
        sem = self.sem
        dsem = self.dsem
        duse = self.duse
        allops = self.ops

        def run(e, eng):
            known = {}
            for op in allops[e]:
                need = {}
                for d in op.deps:
                    if d.dma:
                        key = ("d",) + d.dsem
                        val = d.dval
                        s = dsem[d.dsem[0]][d.dsem[1]]
                    else:
                        key = d.eng
                        val = d.sig
                        s = sem[d.eng]
                    if known.get(key, 0) >= val:
                        continue
                    if key not in need or need[key][0] < val:
                        need[key] = (val, s)
                for key, (val, s) in need.items():
                    known[key] = val
                    eng.wait_ge(s, val)
                ins = op.fn(eng)
                if op.dma:
                    ins.then_inc(dsem[op.dsem[0]][op.dsem[1]], 16)
                elif op.consumed:
                    ins.then_inc(sem[e], 1)
            if e in DMAQ:
                for k in range(NDSEM):
                    last = duse[e][k]
                    if last is not None and known.get(("d", e, k), 0) < last.dval:
                        eng.wait_ge(dsem[e][k], last.dval)

        with nc.Block() as block:
            @block.sync
            def _(eng):
                run("sp", eng)

            @block.tensor
            def _(eng):
                run("pe", eng)

            @block.scalar
            def _(eng):
                run("act", eng)

            @block.vector
            def _(eng):
                run("dve", eng)

            @block.gpsimd
            def _(eng):
                run("pool", eng)


def zipper(lists):
    lists = [l for l in lists if l]
    out = []
    idx = [0] * len(lists)
    n = [len(l) for l in lists]
    while True:
        best = -1
        bf = 2.0
        for i in range(len(lists)):
            if idx[i] < n[i]:
                f = idx[i] / n[i]
                if f < bf:
                    bf = f
                    best = i
        if best < 0:
            break
        l = lists[best]
        while True:
            t = l[idx[best]]
            out.append(t)
            idx[best] += 1
            if not t[5] or idx[best] >= n[best]:
                break
    return out

import math

NTOK = 16640
NCTX = 256
NCOLB = 13
KAPPA = math.exp(-0.5)
LNX_EPS = 64e-5
RET_SCALE = 128 ** -0.5
C_DIFF, C_NP1, C_N128, C_MLT, C_MLE, C_MGT, C_MGE, C_ID, C_BONES = [128 * i for i in range(9)]
C_HSEL = 128 * 9
C_COLM = C_HSEL + 2
C_COL127 = C_COLM + 1
C_ONES = C_COL127 + 1
CW = C_ONES + 128


def make_cst():
    m = np.arange(128, dtype=np.float32)[:, None]
    n = np.arange(128, dtype=np.float32)[None, :]
    c = np.zeros((128, CW), np.float32)
    c[:, C_DIFF:C_DIFF + 128] = n - m
    c[:, C_NP1:C_NP1 + 128] = n + 1 + 0 * m
    c[:, C_N128:C_N128 + 128] = 128 - n + 0 * m
    c[:, C_MLT:C_MLT + 128] = (m < n)
    c[:, C_MLE:C_MLE + 128] = (m <= n)
    c[:, C_MGT:C_MGT + 128] = (m > n)
    c[:, C_MGE:C_MGE + 128] = (m >= n)
    c[:, C_ID:C_ID + 128] = (m == n)
    c[:, C_BONES:C_BONES + 128] = ((m // 64) == (n // 64))
    c[:, C_HSEL] = (m[:, 0] < 64)
    c[:, C_HSEL + 1] = (m[:, 0] >= 64)
    c[:, C_COLM] = m[:, 0]
    c[:, C_COL127] = 127 - m[:, 0]
    c[:, C_ONES:C_ONES + 128] = 1.0
    return c


def build_A():
    nc = bass.Bass("TRN2", target_bir_lowering=False)

    def din(name, shape):
        return nc.dram_tensor(name, shape, F32, kind="ExternalInput").ap()

    xT = din("xT", [2048, NTOK])
    W = din("W", [2048, 1568])
    mvec = din("mvec", [128, 5, 16])
    mu = din("mu", [128, 7, 4])
    pvec = din("pvec", [128, 8])
    w2s = din("w2s", [128, 128])
    a2s = din("a2s", [128, 128])
    g2a = din("g2a", [128, 128])
    g2b = din("g2b", [32, 128])
    rowv = din("rowv", [3, 128])
    dec = din("dec", [2])
    cosT = din("cosT", [128, 16384])
    sinT = din("sinT", [128, 16384])
    cst_d = din("cst", [128, CW])
    out = nc.dram_tensor("out", [NTOK, 256], F32, kind="ExternalOutput").ap()
    pT = nc.dram_tensor("pT", [NCOLB, 128, NTOK], F32).ap()
    yfw = nc.dram_tensor("yfw", [NTOK, 260], F32).ap()

    with contextlib.ExitStack() as st:
        S = Sched(nc, st)
        U = st.enter_context(nc.sbuf_tensor("U", [128, 28400], F32))
        UR2 = st.enter_context(nc.sbuf_tensor("UR2", [128, 21008], F32R))
        cst = st.enter_context(nc.sbuf_tensor("cstt", [128, CW], F32))
        pers = st.enter_context(nc.sbuf_tensor("pers", [128, 2048], F32))
        dummy = st.enter_context(nc.sbuf_tensor("dummya", [128, 8], F32))
        ps = [st.enter_context(nc.psum_tensor("ps%d" % i, [128, 512], F32)) for i in range(8)]
        off = [0]

        def carve(n):
            a = U[:, off[0]:off[0] + n]
            off[0] += n
            assert off[0] <= 28400, off[0]
            return a

        poff = [0]

        def pcarve(n):
            a = pers[:, poff[0]:poff[0] + n]
            poff[0] += n
            assert poff[0] <= 2040
            return a

        S.dma("sp", cst[:], cst_d[:, :], writes=["cst"])
        epsv = pers[:, 2040:2042]
        S.dve(lambda e: e.memset(pers[:, 2040:2041], 1e-6), writes=["epsv"])
        S.dve(lambda e: e.memset(pers[:, 2041:2042], LNX_EPS), writes=["epsv"])
        ones = cst[:, C_ONES:C_ONES + 128]
        ident = cst[:, C_ID:C_ID + 128]

        mv = pcarve(80).rearrange("p (v c) -> p v c", v=5)
        S.dma("sp", mv, mvec[:, :, :], writes=["mv"])
        mus = pcarve(28).rearrange("p (b k) -> p b k", b=7)
        S.dma("sp", mus, mu[:, :, :], writes=["mus"])
        pv = pcarve(8)
        S.dma("sp", pv, pvec[:, :], writes=["pv"])
        w2t = pcarve(128); a2t = pcarve(128); g2at = pcarve(128); g2bt = pcarve(128)
        S.dma("sp", w2t, w2s[:, :], writes=["w2t"])
        S.dma("sp", a2t, a2s[:, :], writes=["a2t"])
        S.dma("sp", g2at, g2a[:, :], writes=["g2at"])
        S.dma("sp", g2bt[0:32, :], g2b[:, :], writes=["g2bt"])
        lnw = pcarve(128); lnb = pcarve(128); gnw = pcarve(128)
        S.dma("sp", lnw, rowv[0].partition_broadcast(128), writes=["rowp"])
        S.dma("sp", lnb, rowv[1].partition_broadcast(128), writes=["rowp"])
        S.dma("sp", gnw, rowv[2].partition_broadcast(128), writes=["rowp"])
        dect = pcarve(2)
        S.dma("sp", dect, dec.partition_broadcast(128), writes=["dect"])
        sx = pcarve(16); sc_ = pcarve(16)
        c0 = pcarve(7)
        c0c = pcarve(7)
        bias = pcarve(16)
        lg = pcarve(2); nlg = pcarve(2); gC = pcarve(2); zeta = pcarve(2)
        state = pcarve(128)
        Sret = pcarve(128)
        dmT = [pcarve(128), pcarve(128)]
        xibc = [pcarve(128), pcarve(128)]

        S.dve(lambda e: e.scalar_tensor_tensor(out=sx, in0=mv[:, 1, :], scalar=1.0, in1=mv[:, 4, :], op0=ALU.add, op1=ALU.mult),
              reads=["mv"], writes=["sx"])
        S.dve(lambda e: e.scalar_tensor_tensor(out=sc_, in0=mv[:, 3, :], scalar=1.0, in1=mv[:, 4, :], op0=ALU.add, op1=ALU.mult),
              reads=["mv"], writes=["sc_"])
        S.dve(lambda e: e.tensor_reduce(out=c0, in_=mus, axis=AX.X, op=ALU.add), reads=["mus"], writes=["c0"])
        S.dve(lambda e: e.tensor_scalar(c0, c0, -1.0, 1.0, ALU.mult, ALU.add), reads=["c0"], writes=["c0"])
        S.dve(lambda e: e.tensor_reduce(out=c0c, in_=mus[:, :, 0:2], axis=AX.X, op=ALU.add), reads=["mus"], writes=["c0c"])
        S.dve(lambda e: e.tensor_scalar(c0c, c0c, -1.0, 1.0, ALU.mult, ALU.add), reads=["c0c"], writes=["c0c"])
        S.act(lambda e: e.activation(out=lg, in_=dect, func=AF.Exp, scale=-math.log(2.0)), reads=["dect"], writes=["lg"])
        S.act(lambda e: e.activation(out=lg, in_=lg, func=AF.Ln, scale=-1.0, bias=1.0), reads=["lg"], writes=["lg"])
        S.dve(lambda e: e.tensor_scalar(nlg, lg, -1.0, None, ALU.mult), reads=["lg"], writes=["nlg"])
        S.act(lambda e: e.activation(out=gC, in_=lg, func=AF.Exp, scale=128.0), reads=["lg"], writes=["gC"])
        S.act(lambda e: e.activation(out=dmT[0], in_=cst[:, C_DIFF:C_DIFF + 128], func=AF.Exp, scale=lg[:, 0:1]),
              reads=["cst", "lg"], writes=["dmT"])
        S.dve(lambda e: e.scalar_tensor_tensor(out=dmT[0], in0=dmT[0], scalar=RET_SCALE, in1=cst[:, C_MLE:C_MLE + 128],
                                               op0=ALU.mult, op1=ALU.mult), reads=["dmT", "cst"], writes=["dmT"])
        S.act(lambda e: e.activation(out=dmT[1], in_=cst[:, C_DIFF:C_DIFF + 128], func=AF.Exp, scale=nlg[:, 1:2]),
              reads=["cst", "nlg"], writes=["dmT"])
        S.dve(lambda e: e.scalar_tensor_tensor(out=dmT[1], in0=dmT[1], scalar=RET_SCALE, in1=cst[:, C_MGT:C_MGT + 128],
                                               op0=ALU.mult, op1=ALU.mult), reads=["dmT", "cst"], writes=["dmT"])
        S.act(lambda e: e.activation(out=xibc[0], in_=cst[:, C_NP1:C_NP1 + 128], func=AF.Exp, scale=lg[:, 0:1]),
              reads=["cst", "lg"], writes=["xibc"])
        S.act(lambda e: e.activation(out=xibc[1], in_=cst[:, C_N128:C_N128 + 128], func=AF.Exp, scale=lg[:, 1:2]),
              reads=["cst", "lg"], writes=["xibc"])
        S.act(lambda e: e.activation(out=zeta[:, 0:1], in_=cst[:, C_COL127:C_COL127 + 1], func=AF.Exp, scale=lg[:, 0:1]),
              reads=["cst", "lg"], writes=["zeta"])
        S.act(lambda e: e.activation(out=zeta[:, 1:2], in_=cst[:, C_COLM:C_COLM + 1], func=AF.Exp, scale=lg[:, 1:2]),
              reads=["cst", "lg"], writes=["zeta"])
        S.dve(lambda e: e.tensor_scalar(zeta, zeta, RET_SCALE, None, ALU.mult), reads=["zeta"], writes=["zeta"])

        roff = [0]

        def rcarve(n):
            a = UR2[:, roff[0]:roff[0] + n]
            roff[0] += n
            assert roff[0] <= 21008, roff[0]
            return a
        Wh = rcarve(16 * 800).rearrange("p (c n) -> p c n", c=16)
        xb = [rcarve(16 * 256).rearrange("p (c t) -> p c t", c=16) for _ in range(2)]
        sh1r = rcarve(16)
        sq = carve(16 * 256).rearrange("p (c t) -> p c t", c=16)
        rsb = carve(256)
        tmpb = [carve(256) for _ in range(2)]
        pob = [carve(256) for _ in range(2)]
        colw = [128] * 6 + [32] + [128] * 6
        colo = [0]
        for wdt in colw:
            colo.append(colo[-1] + wdt)
        halves = [(list(range(0, 7)), 0, 800), (list(range(7, 13)), 800, 768)]

        blocks = [(0, 256, True)] + [(256 + 256 * i, 256, False) for i in range(64)]
        kk_ = 0
        cnt = 0
        for variant in ("c", "x"):
            vi = 2 if variant == "c" else 0
            sv = sc_ if variant == "c" else sx
            for (hblocks, hoff, hw) in halves:
                S.dma("pool", Wh[:, :, 0:hw], W[:, hoff:hoff + hw].rearrange("(c p) n -> p c n", p=128), writes=["Wp"])
                for b in hblocks:
                    wdt = colw[b]
                    pq = ps[7][:wdt, b:b + 1]
                    lo = colo[b] - hoff
                    for c in range(16):
                        S.pe(lambda e, c=c, pq=pq, lo=lo, wdt=wdt, vi=vi: e.matmul(pq, Wh[:, c, lo:lo + wdt].bitcast(F32), mv[:, vi, c:c + 1],
                                                                                     start=(c == 0), stop=(c == 15)),
                             reads=["Wp", "mv"], writes=["ps7"])
                    S.dve(lambda e, b=b, wdt=wdt: e.tensor_copy(bias[:wdt, b:b + 1], ps[7][:wdt, b:b + 1]), reads=["ps7"], writes=["bias"])
                for c in range(16):
                    S.dve(lambda e, c=c, sv=sv, hw=hw: e.tensor_scalar(Wh[:, c, 0:hw], Wh[:, c, 0:hw], sv[:, c:c + 1], None, ALU.mult),
                          reads=["Wp", "sx", "sc_"], writes=["Wp"])
                for (t0, tn, isctx) in blocks:
                    if isctx != (variant == "c"):
                        continue
                    xbb = xb[kk_ % 2]
                    rx = ("xb", kk_ % 2)
                    kk_ += 1
                    S.dma("pool", xbb, xT[:, t0:t0 + tn].rearrange("(c p) t -> p c t", p=128), writes=[rx])
                    S.act(lambda e, xbb=xbb: e.activation(out=sq, in_=xbb, func=AF.Square), reads=[rx], writes=["sq"])
                    for c in range(16):
                        S.pe(lambda e, c=c: e.matmul(ps[6][:, 0:256], ones, sq[:, c, :], start=(c == 0), stop=(c == 15)),
                             reads=["sq", "cst"], writes=["ps6"])
                    S.act(lambda e: e.activation(out=rsb, in_=ps[6][:, 0:256], func=AF.Sqrt, scale=1.0 / 2048, bias=epsv[:, 0:1]),
                          reads=["ps6", "epsv"], writes=["rsb"])
                    S.dve(lambda e: e.reciprocal(rsb, rsb), reads=["rsb"], writes=["rsb"])
                    for b in hblocks:
                        wdt = colw[b]
                        lo = colo[b] - hoff
                        pb = cnt % 4
                        cnt += 1
                        pq = ps[pb][:wdt, 0:256]
                        for c in range(16):
                            S.pe(lambda e, c=c, pq=pq, lo=lo, wdt=wdt, xbb=xbb: e.matmul(
                                pq, Wh[:, c, lo:lo + wdt], xbb[:, c, :], start=(c == 0), stop=(c == 15)),
                                reads=["Wp", rx], writes=[("psp", pb)])
                        tb, ob_ = tmpb[cnt % 2], pob[cnt % 2]
                        S.dve(lambda e, tb=tb, pq=pq, wdt=wdt: e.tensor_tensor(tb[:wdt], pq, rsb[:wdt], ALU.mult),
                              reads=[("psp", pb), "rsb"], writes=[("tmpb", cnt % 2)])
                        S.act(lambda e, tb=tb, ob_=ob_, wdt=wdt, b=b: e.activation(out=ob_[:wdt], in_=tb[:wdt], func=AF.Identity,
                                                                                   bias=bias[:wdt, b:b + 1]),
                              reads=[("tmpb", cnt % 2), "bias"], writes=[("pob", cnt % 2)])
                        S.dma("sp", pT[b, :wdt, t0:t0 + tn], ob_[:wdt], reads=[("pob", cnt % 2)], writes=["pT"])
        S.barrier(dummy[:])

        off[0] = 0
        NT_ = [0]

        def T(par=2):
            r = []
            for _ in range(par):
                NT_[0] += 1
                r.append((carve(128), "t%d" % NT_[0]))
            return r

        names = ["H", "R", "u", "cosb", "sinb", "kkraw", "sq", "rn", "kk", "th", "sig", "cs", "E", "Ex", "gin", "gex", "ginv",
                 "a", "t1", "kd", "b", "bt", "at", "kt", "rt", "vtok", "atok", "ktok", "zT", "sgd", "gtok",
                 "qr", "kr", "qx", "vr", "krz", "PT", "ytok", "yret", "yfwb", "o1", "o2", "gsil", "gtot", "stmp", "gsl"]
        tl = {}
        for nme in names:
            if nme == "H":
                tl[nme] = []
                for _ in range(2):
                    NT_[0] += 1
                    tl[nme].append((carve(7 * 256).rearrange("p (b t) -> p b t", b=7), "t%d" % NT_[0]))
            elif nme in ("R", "u"):
                nb = 6 if nme == "R" else 7
                tl[nme] = []
                for _ in range(2):
                    NT_[0] += 1
                    tl[nme].append((carve(nb * 128).rearrange("p (b t) -> p b t", b=nb), "t%d" % NT_[0]))
            elif nme in ("atok", "ktok"):
                tl[nme] = []
                for _ in range(2):
                    NT_[0] += 1
                    tl[nme].append((carve(256).rearrange("p (h k) -> p h k", h=2), "t%d" % NT_[0]))
            elif nme == "yfwb":
                tl[nme] = []
                for _ in range(2):
                    NT_[0] += 1
                    tl[nme].append((carve(260), "t%d" % NT_[0]))
            else:
                tl[nme] = T(2)
        hn = ["N", "A", "BK", "RA", "RK", "Np", "Ap", "P", "Q", "Np2", "Ap2", "P2", "Q2", "RHS", "Us"]
        th_ = {nme: [T(2), T(2)] for nme in hn}
        gCr = carve(4)
        tot = carve(4)
        sm = carve(64)
        bsv = carve(4)
        for nme in ("atok", "ktok"):
            for (ap, rn) in tl[nme]:
                S.pool(lambda e, ap=ap: e.memset(ap, 0.0), writes=[rn])

        pools = {"P0": [4], "PR": [4], "H0": [0, 1], "H1": [2, 3], "S": [5, 7]}
        pqc = {k: 0 for k in pools}

        def PQ():
            pl = pools[S.cur]
            i = pl[pqc[S.cur] % len(pl)]
            pqc[S.cur] += 1
            return ps[i][:, 0:128], ("pq", i)

        def mm(o, ro, lhsT, rl, rhs, rr, start=True, stop=True):
            S.pe(lambda e: e.matmul(o, lhsT, rhs, start=start, stop=stop), reads=list(rl) + list(rr), writes=[ro],
                 hold=(not stop))

        def tr(o, ro, in_, ri):
            S.pe(lambda e: e.transpose(o, in_, ident), reads=[ri, "cst"], writes=[ro])

        def TT(eng, o, ro, a, ra, b, rb, op):
            S.add(eng, lambda e: e.tensor_tensor(o, a, b, op), reads=[ra, rb], writes=[ro])

        def TS(eng, o, ro, a, ra, s1, s2, op0, op1=None, extra=()):
            if op1 is None:
                S.add(eng, lambda e: e.tensor_scalar(o, a, s1, None, op0), reads=[ra] + list(extra), writes=[ro])
            else:
                S.add(eng, lambda e: e.tensor_scalar(o, a, s1, s2, op0, op1), reads=[ra] + list(extra), writes=[ro])

        def STT(eng, o, ro, a, ra, sc, b, rb, op0, op1, extra=()):
            S.add(eng, lambda e: e.scalar_tensor_tensor(out=o, in0=a, scalar=sc, in1=b, op0=op0, op1=op1),
                  reads=[ra, rb] + list(extra), writes=[ro])

        def ACT(o, ro, a, ra, func, bias=None, scale=None, extra=(), accum=None):
            kw = {}
            if bias is not None:
                kw["bias"] = bias
            if scale is not None:
                kw["scale"] = scale
            if accum is not None:
                kw["accum_out"] = accum
            S.act(lambda e: e.activation(out=o, in_=a, func=func, **kw), reads=[ra] + list(extra), writes=[ro])

        chunks_f = [(0, "c", 0), (128, "c", 1)] + [(256 + 128 * i, "x", i) for i in range(128)]
        chunks_b = [(128, "c", 1), (0, "c", 0)] + [(256 + 128 * i, "x", i) for i in reversed(range(128))]
        CD = cst

        it = 0
        for d, chunks in ((0, chunks_f), (1, chunks_b)):
            S.dve(lambda e: e.memset(state, 0.0), writes=["state"])
            S.dve(lambda e: e.memset(Sret, 0.0), writes=["Sret"])
            Mst = CD[:, C_MLT:C_MLT + 128] if d == 0 else CD[:, C_MGT:C_MGT + 128]
            MstT = CD[:, C_MGT:C_MGT + 128] if d == 0 else CD[:, C_MLT:C_MLT + 128]
            Min = CD[:, C_MLE:C_MLE + 128] if d == 0 else CD[:, C_MGE:C_MGE + 128]
            prevS = None
            for (t0, kind, ci) in chunks:
                par = it % 2
                it += 1
                S.begin(["P0", "H0", "H1", "PR", "S"])
                S.stage("P0")

                def g(nme):
                    return tl[nme][par]
                H, rH = g("H")
                R_, rR = g("R")
                u, ru = g("u")
                if kind == "x":
                    lo_ok = ci > 0
                    hi_ok = ci < 127
                else:
                    lo_ok = ci > 0
                    hi_ok = ci < 1
                if not (lo_ok and hi_ok):
                    S.pool(lambda e, H=H: e.memset(H, 0.0), writes=[rH])
                a0 = t0 - (64 if lo_ok else 0)
                a1 = t0 + 128 + (64 if hi_ok else 0)
                h0 = 64 - (t0 - a0)
                S.dma("sp", H[:, 0:6, h0:h0 + (a1 - a0)], pT[0:6, :, a0:a1].rearrange("b p t -> p b t"), writes=[rH])
                S.dma("sp", H[0:32, 6, h0:h0 + (a1 - a0)], pT[6, 0:32, a0:a1], writes=[rH])
                S.dma("act", R_, pT[7:13, :, t0:t0 + 128].rearrange("b p t -> p b t"), writes=[rR])
                def rub(b):
                    return (ru, b)
                steps = 5 if kind == "x" else 3
                for step in range(steps):
                    for b in range(7):
                        np_ = colw[b]
                        Hc = H[:np_, b, 64:192]
                        ub = u[:np_, b, :]
                        if step == 0:
                            cc_ = c0 if kind == "x" else c0c
                            S.dve(lambda e, ub=ub, Hc=Hc, b=b, np_=np_, cc_=cc_: e.tensor_scalar(ub, Hc, cc_[:np_, b:b + 1], None, ALU.mult),
                                  reads=[rH, "c0", "c0c"], writes=[rub(b)])
                            continue
                        if kind == "x":
                            u3 = ub.rearrange("p (r c) -> p r c", r=2)
                            H3 = Hc.rearrange("p (r c) -> p r c", r=2)
                            dst, src = [(u3[:, :, 1:64], H3[:, :, 0:63]), (u3[:, :, 0:63], H3[:, :, 1:64]),
                                        (ub, H[:np_, b, 0:128]), (ub, H[:np_, b, 128:256])][step - 1]
                        else:
                            dst, src = [(ub, H[:np_, b, 63:191]), (ub, H[:np_, b, 65:193])][step - 1]
                        mcol = step - 1
                        S.dve(lambda e, dst=dst, src=src, b=b, np_=np_, mcol=mcol: e.scalar_tensor_tensor(
                            out=dst, in0=src, scalar=mus[:np_, b, mcol:mcol + 1], in1=dst, op0=ALU.mult, op1=ALU.add),
                            reads=[rH, rub(b), "mus"], writes=[rub(b)])
                rT, kT, vT = u[:, 0, :], u[:, 1, :], u[:, 2, :]
                hs = slice(d * 64, d * 64 + 64)
                kkraw, r1 = g("kkraw"); sqt, r2 = g("sq"); rn, r3 = g("rn"); kk, r4 = g("kk")
                TS("dve", kkraw, r1, kT, rub(1), pv[:, 4:5], None, ALU.mult, extra=["pv"])
                ACT(sqt, r2, kkraw, r1, AF.Square)
                pq, rq = PQ()
                mm(pq, rq, CD[:, C_BONES:C_BONES + 128], ["cst"], sqt, [r2])
                ACT(rn, r3, pq, rq, AF.Sqrt)
                TS("dve", rn, r3, rn, r3, 1e-12, None, ALU.max)
                S.dve(lambda e, rn=rn: e.reciprocal(rn, rn), reads=[r3], writes=[r3])
                TT("dve", kk, r4, kkraw, r1, rn, r3, ALU.mult)
                tht, r5 = g("th"); sig, r6 = g("sig"); cs, r7 = g("cs"); E, r8 = g("E"); Ex, r9 = g("Ex")
                gin, r10 = g("gin"); gex, r11 = g("gex"); ginv, r12 = g("ginv")
                ACT(tht[hs, :], r5, u[hs, 3, :], rub(3), AF.Tanh)
                pq, rq = PQ()
                mm(pq, rq, w2t[hs, :], ["w2t"], tht[hs, :], [r5])
                ACT(sig, r6, pq, rq, AF.Sigmoid, bias=pv[:, d:d + 1], extra=["pv"])
                S.dve(lambda e, cs=cs, sig=sig: e.tensor_tensor_scan(cs, ones, sig, 0.0, ALU.mult, ALU.add),
                      reads=[r6, "cst"], writes=[r7])
                totp = tot[:, par:par + 1]
                gCp = gCr[:, par:par + 1]
                if d == 0:
                    Et, rE = cs, r7
                    TT("dve", Ex, r9, cs, r7, sig, r6, ALU.subtract)
                    Ext, rEx = Ex, r9
                else:
                    TS("dve", Ex, r9, cs, r7, -1.0, cs[:, 127:128], ALU.mult, ALU.add)
                    TT("dve", E, r8, Ex, r9, sig, r6, ALU.add)
                    Et, rE, Ext, rEx = E, r8, Ex, r9
                ACT(gin, r10, Et, rE, AF.Exp, scale=-KAPPA)
                ACT(gex, r11, Ext, rEx, AF.Exp, scale=-KAPPA)
                ACT(ginv, r12, Et, rE, AF.Exp, scale=KAPPA)
                ACT(gCp, ("gC", par), cs[:, 127:128], r7, AF.Exp, scale=-KAPPA)
                at_, r13 = g("a"); t1, r14 = g("t1"); kd, r15 = g("kd"); bb, r16 = g("b")
                pq, rq = PQ()
                mm(pq, rq, a2t[hs, :], ["a2t"], u[hs, 4, :], [rub(4)])
                ACT(at_, r13, pq, rq, AF.Sigmoid, bias=pv[:, 2 + d:3 + d], extra=["pv"])
                TS("dve", t1, r14, at_, r13, -1.0, pv[:, 5:6], ALU.add, ALU.mult, extra=["pv"])
                STT("dve", kd, r15, t1, r14, 1.0, kT, rub(1), ALU.add, ALU.mult)
                TT("dve", bb, r16, kk, r4, at_, r13, ALU.mult)
                bt, r17 = g("bt"); al, r18 = g("at"); kt, r19 = g("kt"); rt, r20 = g("rt")
                TT("dve", bt, r17, kk, r4, gex, r11, ALU.mult)
                STT("dve", al, r18, bb, r16, -1.0, ginv, r12, ALU.mult, ALU.mult)
                TT("dve", kt, r19, kd, r15, ginv, r12, ALU.mult)
                TT("dve", rt, r20, rT, rub(0), gin, r10, ALU.mult)
                vtok, r21 = g("vtok"); atok, r22 = g("atok"); ktok, r23 = g("ktok")
                pq, rq = PQ(); tr(pq, rq, vT, rub(2))
                S.act(lambda e, vtok=vtok, pq=pq: e.copy(vtok, pq), reads=[rq], writes=[r21])
                pq, rq = PQ(); tr(pq, rq, al, r18)
                S.act(lambda e, atok=atok, pq=pq: e.copy(atok[:, 0, 0:64], pq[:, 0:64]), reads=[rq], writes=[r22])
                S.dve(lambda e, atok=atok, pq=pq: e.tensor_copy(atok[:, 1, 64:128], pq[:, 64:128]), reads=[rq], writes=[r22])
                pq, rq = PQ(); tr(pq, rq, kt, r19)
                S.act(lambda e, ktok=ktok, pq=pq: e.copy(ktok[:, 0, 0:64], pq[:, 0:64]), reads=[rq], writes=[r23])
                S.dve(lambda e, ktok=ktok, pq=pq: e.tensor_copy(ktok[:, 1, 64:128], pq[:, 64:128]), reads=[rq], writes=[r23])
                zT, r24 = g("zT")
                STT("dve", zT, r24, rT, rub(0), pv[:, 6:7], kd, r15, ALU.mult, ALU.mult, extra=["pv"])
                pqb_, rqb_ = PQ()
                mm(pqb_[:, 0:2], rqb_, zT, [r24], CD[:, C_HSEL:C_HSEL + 2], ["cst"])
                pqb = bsv[:, 2 * par:2 * par + 2]
                rqb = ("bsv", par)
                S.act(lambda e, pqb=pqb, pqb_=pqb_: e.copy(pqb, pqb_[:, 0:2]), reads=[rqb_], writes=[rqb])
                yfb, r25 = g("yfwb")
                ytok, r26 = g("ytok")
                if d == 1:
                    S.dma("act", yfb, yfw[t0:t0 + 128, :], writes=[r25])
                    sgd, sgr = g("sgd"); gsil, gsr = g("gsil"); gtok, gtr = g("gtok")
                    ACT(sgd, sgr, u[:, 5, :], rub(5), AF.Sigmoid)
                    ACT(gsil[0:32, :], gsr, u[0:32, 6, :], rub(6), AF.Sigmoid)
                    pq, rq = PQ()
                    mm(pq, rq, sgd, [sgr], g2at, ["g2at"], start=True, stop=False)
                    mm(pq, rq, gsil[0:32, :], [gsr], g2bt[0:32, :], ["g2bt"], start=False, stop=True)
                    S.act(lambda e, gtok=gtok, pq=pq: e.copy(gtok, pq), reads=[rq], writes=[gtr])
                for h in range(2):
                    hp = slice(h * 64, h * 64 + 64)
                    S.stage("H%d" % h)

                    def gh(nme):
                        return th_[nme][h][par]
                    N_, n1 = gh("N"); A_, n2 = gh("A"); BK, n3 = gh("BK"); RA, n4 = gh("RA"); RK, n5 = gh("RK")
                    pl = pools["H%d" % h]
                    bA, rA = ps[pl[0]], ("pq", pl[0])
                    bB, rB = ps[pl[1]], ("pq", pl[1])
                    A0, A1 = bA[:, 0:128], bA[:, 128:256]
                    B0, B1 = bB[:, 0:128], bB[:, 128:256]
                    mm(A0, rA, al[hp, :], [r18], bt[hp, :], [r17])
                    mm(A1, rA, bt[hp, :], [r17], al[hp, :], [r18])
                    TT("dve", N_, n1, A0, rA, Mst, "cst", ALU.mult)
                    TT("dve", A_, n2, A1, rA, MstT, "cst", ALU.mult)
                    Pc, nP = gh("P"); Qc, nQ = gh("Q")
                    TT("pool", Pc, nP, N_, n1, ident, "cst", ALU.add)
                    TT("pool", Qc, nQ, A_, n2, ident, "cst", ALU.add)
                    Npc, nNp, Apc, nAp = N_, n1, A_, n2
                    alt = [(gh("Np"), gh("Ap"), gh("P2"), gh("Q2")), (gh("Np2"), gh("Ap2"), gh("P"), gh("Q"))]
                    for l in range(1, 7):
                        (Npn, nNpn), (Apn, nApn), (Pn, nPn), (Qn, nQn) = alt[(l - 1) % 2]
                        mm(A0, rA, Apc, [nAp], Npc, [nNp])
                        if l < 6:
                            mm(A1, rA, Npc, [nNp], Apc, [nAp])
                        S.act(lambda e, Npn=Npn, A0=A0: e.copy(Npn, A0), reads=[rA], writes=[nNpn])
                        if l < 6:
                            S.act(lambda e, Apn=Apn, A1=A1: e.copy(Apn, A1), reads=[rA], writes=[nApn])
                        mm(B0, rB, Qc, [nQ], Npn, [nNpn])
                        if l < 6:
                            mm(B1, rB, Pc, [nP], Apn, [nApn])
                        TT("dve", Pn, nPn, B0, rB, Pc, nP, ALU.add)
                        if l < 6:
                            TT("dve", Qn, nQn, B1, rB, Qc, nQ, ALU.add)
                        Npc, nNp, Apc, nAp = Npn, nNpn, Apn, nApn
                        Pc, nP = Pn, nPn
                        if l < 6:
                            Qc, nQ = Qn, nQn
                    mm(A0, rA, kt[hp, :], [r19], bt[hp, :], [r17])
                    mm(A1, rA, al[hp, :], [r18], rt[hp, :], [r20])
                    TT("dve", BK, n3, A0, rA, Mst, "cst", ALU.mult)
                    TT("dve", RA, n4, A1, rA, Min, "cst", ALU.mult)
                    mm(B0, rB, kt[hp, :], [r19], rt[hp, :], [r20])
                    TT("dve", RK, n5, B0, rB, Min, "cst", ALU.mult)
                    S.stage("S")
                    RHS, n6 = gh("RHS"); Us, n7 = gh("Us")
                    vh = vtok[:, hp]
                    pq, rq = PQ()
                    mm(pq[:, 0:64], rq, bt[hp, :], [r17], state[hp, 0:64], ["state"], start=True, stop=False)
                    mm(pq[:, 0:64], rq, BK, [n3], vh, [r21], start=False, stop=True)
                    S.act(lambda e, RHS=RHS, pq=pq: e.copy(RHS[:, 0:64], pq[:, 0:64]), reads=[rq], writes=[n6])
                    pq, rq = PQ()
                    mm(pq[:, 0:64], rq, Pc, [nP], RHS[:, 0:64], [n6])
                    S.act(lambda e, Us=Us, pq=pq: e.copy(Us[:, 0:64], pq[:, 0:64]), reads=[rq], writes=[n7])
                    pq, rq = PQ()
                    mm(pq[:, 0:64], rq, rt[hp, :], [r20], state[hp, 0:64], ["state"], start=True, stop=False)
                    mm(pq[:, 0:64], rq, RA, [n4], Us[:, 0:64], [n7], start=False, stop=False)
                    mm(pq[:, 0:64], rq, RK, [n5], vh, [r21], start=False, stop=True)
                    S.dve(lambda e, ytok=ytok, pq=pq, hp=hp: e.tensor_copy(ytok[:, hp], pq[:, 0:64]), reads=[rq], writes=[r26])
                    if h == 0:
                        pqs, rqs = ps[6][:, 0:128], ("pqs", 0)
                    mm(pqs[:, 0:64], rqs, atok[:, h, :], [r22], Us[:, 0:64], [n7], start=(h == 0), stop=False)
                    mm(pqs[:, 0:64], rqs, ktok[:, h, :], [r23], vh, [r21], start=False, stop=(h == 1))
                S.stage("S")
                TS("dve", state[:, 0:64], "state", state[:, 0:64], "state", gCp, None, ALU.mult, extra=[("gC", par)])
                STT("dve", state[:, 0:64], "state", pqs[:, 0:64], rqs, gCp, state[:, 0:64], "state", ALU.mult, ALU.add,
                    extra=[("gC", par)])

                S.stage("PR")
                qT, kT2, vT2, gT, qsw, ksw = (R_[:, i, :] for i in range(6))
                qr, q1 = g("qr"); kr, q2 = g("kr"); qx, q3 = g("qx"); vr, q4 = g("vr"); krz, q5 = g("krz"); PT, q6 = g("PT")
                if kind == "x":
                    cb, q7 = g("cosb"); sb, q8 = g("sinb")
                    xp = t0 - NCTX
                    S.dma("sp", cb, cosT[:, xp:xp + 128], writes=[q7])
                    S.dma("sp", sb, sinT[:, xp:xp + 128], writes=[q8])
                    TT("pool", qr, q1, qT, rR, cb, q7, ALU.mult)
                    TT("pool", qx, q3, qsw, rR, sb, q8, ALU.mult)
                    TT("pool", qr, q1, qr, q1, qx, q3, ALU.add)
                    TT("pool", kr, q2, kT2, rR, cb, q7, ALU.mult)
                    TT("pool", qx, q3, ksw, rR, sb, q8, ALU.mult)
                    TT("pool", kr, q2, kr, q2, qx, q3, ALU.add)
                    qrr, krr = qr, kr
                    rqr, rkr = q1, q2
                else:
                    qrr, krr, rqr, rkr = qT, kT2, rR, rR
                TT("dve", qx, q3, qrr, rqr, xibc[d], "xibc", ALU.mult)
                pq, rq = PQ(); tr(pq, rq, vT2, rR)
                S.act(lambda e, vr=vr, pq=pq: e.copy(vr, pq), reads=[rq], writes=[q4])
                pq, rq = PQ(); tr(pq, rq, krr, rkr)
                TS("dve", krz, q5, pq, rq, zeta[:, d:d + 1], None, ALU.mult, extra=["zeta"])
                pq, rq = PQ(); mm(pq, rq, krr, [rkr], qrr, [rqr])
                TT("dve", PT, q6, pq, rq, dmT[d], "dmT", ALU.mult)
                if d == 1:
                    gsl, gslr = g("gsl")
                    pq, rq = PQ(); tr(pq, rq, gT, rR)
                    S.act(lambda e, gsl=gsl, pq=pq: e.activation(out=gsl, in_=pq, func=AF.Silu), reads=[rq], writes=[gslr])
                S.stage("S")
                yret, q9 = g("yret")
                pq, rq = PQ()
                mm(pq, rq, PT, [q6], vr, [q4], start=True, stop=False)
                mm(pq, rq, qx, [q3], Sret, ["Sret"], start=False, stop=True)
                S.act(lambda e, yret=yret, pq=pq: e.copy(yret, pq), reads=[rq], writes=[q9])
                pq, rq = PQ()
                mm(pq, rq, krz, [q5], vr, [q4])
                STT("dve", Sret, "Sret", Sret, "Sret", gC[:, d:d + 1], pq, rq, ALU.mult, ALU.add, extra=["gC"])

                if d == 0:
                    S.dve(lambda e, yfb=yfb, ytok=ytok: e.tensor_copy(yfb[:, 0:128], ytok), reads=[r26], writes=[r25])
                    S.dve(lambda e, yfb=yfb, yret=yret: e.tensor_copy(yfb[:, 128:256], yret), reads=[q9], writes=[r25])
                    S.dve(lambda e, yfb=yfb, pqb=pqb: e.tensor_copy(yfb[:, 256:258], pqb[:, 0:2]), reads=[rqb], writes=[r25])
                    S.dma("sp", yfw[t0:t0 + 128, :], yfb, reads=[r25], writes=["yfw"])
                else:
                    o1, o1r = g("o1"); o2, o2r = g("o2")
                    gtot, gtotr = g("gtot")
                    TT("dve", ytok, r26, ytok, r26, yfb[:, 0:128], r25, ALU.add)
                    y3 = ytok.rearrange("p (h v) -> p h v", h=2)
                    smr = ("sm", par)
                    so = par * 32
                    mean = sm[:, so:so + 2]; var = sm[:, so + 2:so + 4]; bsum = sm[:, so + 4:so + 6]
                    S.dve(lambda e, mean=mean, y3=y3: e.tensor_reduce(out=mean, in_=y3, axis=AX.X, op=ALU.add), reads=[r26], writes=[smr])
                    S.dve(lambda e, mean=mean: e.tensor_scalar(mean, mean, 1.0 / 64, None, ALU.mult), reads=[smr], writes=[smr])
                    o13 = o1.rearrange("p (h v) -> p h v", h=2)
                    o23 = o2.rearrange("p (h v) -> p h v", h=2)
                    S.dve(lambda e, o13=o13, y3=y3, mean=mean: e.tensor_tensor(o13, y3, mean.unsqueeze(2).broadcast_to([128, 2, 64]), ALU.subtract),
                          reads=[r26, smr], writes=[o1r])
                    TT("dve", o2, o2r, o1, o1r, o1, o1r, ALU.mult)
                    S.dve(lambda e, var=var, o23=o23: e.tensor_reduce(out=var, in_=o23, axis=AX.X, op=ALU.add), reads=[o2r], writes=[smr])
                    S.act(lambda e, var=var: e.activation(out=var, in_=var, func=AF.Sqrt, scale=1.0 / 64, bias=epsv[:, 1:2]), reads=[smr, "epsv"], writes=[smr])
                    S.dve(lambda e, var=var: e.reciprocal(var, var), reads=[smr], writes=[smr])
                    S.dve(lambda e, o13=o13, var=var: e.tensor_tensor(o13, o13, var.unsqueeze(2).broadcast_to([128, 2, 64]), ALU.mult),
                          reads=[o1r, smr], writes=[o1r])
                    TT("dve", o1, o1r, o1, o1r, lnw, "rowp", ALU.mult)
                    TT("dve", o1, o1r, o1, o1r, lnb, "rowp", ALU.add)
                    S.dve(lambda e, bsum=bsum, pqb=pqb, yfb=yfb: e.tensor_tensor(bsum, pqb[:, 0:2], yfb[:, 256:258], ALU.add),
                          reads=[rqb, r25], writes=[smr])
                    v3 = vtok.rearrange("p (h v) -> p h v", h=2)
                    S.dve(lambda e, o23=o23, v3=v3, bsum=bsum: e.tensor_tensor(o23, v3, bsum.unsqueeze(2).broadcast_to([128, 2, 64]), ALU.mult),
                          reads=[r21, smr], writes=[o2r])
                    TT("dve", o1, o1r, o1, o1r, o2, o2r, ALU.add)
                    TT("dve", gtot, gtotr, o1, o1r, gtok, gtr, ALU.mult)
                    S.dma("sp", out[t0:t0 + 128, 0:128], gtot, reads=[gtotr], writes=["out"])
                    TT("dve", yret, q9, yret, q9, yfb[:, 128:256], r25, ALU.add)
                    ss = sm[:, so + 8:so + 9]
                    S.dve(lambda e, ss=ss: e.memset(ss, 0.0), writes=[smr])
                    ACT(o2, o2r, yret, q9, AF.Square, accum=ss, extra=[smr])
                    S.act(lambda e, ss=ss: e.activation(out=ss, in_=ss, func=AF.Sqrt, scale=1.0 / 128, bias=epsv[:, 0:1]), reads=[smr, o2r, "epsv"], writes=[smr])
                    S.dve(lambda e, ss=ss: e.reciprocal(ss, ss), reads=[smr], writes=[smr])
                    STT("dve", o2, o2r, yret, q9, ss, gnw, "rowp", ALU.mult, ALU.mult, extra=[smr])
                    TT("dve", o2, o2r, o2, o2r, gsl, gslr, ALU.mult)
                    S.dma("sp", out[t0:t0 + 128, 128:256], o2, reads=[o2r], writes=["out"])
                rec = S.end()
                if prevS is None:
                    S.replay(rec["P0"])
                else:
                    S.replay(zipper([rec["P0"], prevS]))
                S.replay(zipper([rec["H0"], rec["H1"], rec["PR"]]))
                prevS = rec["S"]
            S.replay(prevS)
            S.barrier(dummy[:])
        S.emit()
    return nc


D = 2048
NTB = 2080
NE = 32
TILES_B = [(0, 32)] + [(32 + 128 * i, 128) for i in range(16)]
EPS = 1e-6


def build_B(n_exp=NE):
    nc = bass.Bass("TRN2", target_bir_lowering=False)

    def din(name, shape):
        return nc.dram_tensor(name, shape, F32, kind="ExternalInput").ap()

    ymT = din("ymT", [D, NTB])
    xres = din("xres", [NTB, D])
    w_out = din("w_out", [D, D])
    w1 = din("w1", [NE, D, 2048])
    w2 = din("w2", [NE, 1024, D])
    rw = din("rw", [D, 32])
    rb = din("rb", [32])
    vecs = din("vecs", [10, D])
    ident_d = din("ident", [128, 128])
    out = nc.dram_tensor("out", [NTB, D], F32, kind="ExternalOutput").ap()
    outn = nc.dram_tensor("outn", [NTB, D], F32, kind="ExternalOutput").ap()
    xn = nc.dram_tensor("xn", [NTB, D], F32).ap()
    h2Td = nc.dram_tensor("h2Td", [D, NTB], F32).ap()

    with contextlib.ExitStack() as st:
        S = Sched(nc, st)
        UR = st.enter_context(nc.sbuf_tensor("UR", [128, 29440], F32R))
        UF = st.enter_context(nc.sbuf_tensor("UF", [128, 21024], F32))
        gates = st.enter_context(nc.sbuf_tensor("gates", [128, 17, 32], F32))
        small = st.enter_context(nc.sbuf_tensor("small", [128, 512], F32))
        ident = st.enter_context(nc.sbuf_tensor("identt", [128, 128], F32))
        dummy = st.enter_context(nc.sbuf_tensor("dummyb", [128, 8], F32))
        ps = [st.enter_context(nc.psum_tensor("ps%d" % i, [128, 512], F32)) for i in range(8)]

        off = [0]
        offr = [0]

        def carve(n):
            a = UF[:, off[0]:off[0] + n]
            off[0] += n
            assert off[0] <= 21024, off[0]
            return a

        def carver(n):
            a = UR[:, offr[0]:offr[0] + n]
            offr[0] += n
            assert offr[0] <= 29440, offr[0]
            return a

        S.dma("sp", ident[:], ident_d[:, :], writes=["ident"])
        epsb = small[:, 500:501]
        S.dve(lambda e: e.memset(small[:, 500:501], EPS), writes=["epsb"])

        wo = [carver(16 * 512).rearrange("p (c n) -> p c n", c=16) for _ in range(2)]
        ym = [carver(16 * 128).rearrange("p (c t) -> p c t", c=16) for _ in range(2)]
        xg = [carve(512) for _ in range(2)]
        ob = [carve(512) for _ in range(2)]
        bc = {k: carve(2048) for k in ("gt1x", "gt1c", "gscx", "gscc", "sh2x", "sh2c")}
        xt1 = carve(2048)
        xt = [xt1, xt1]
        h2 = carve(2048)
        tmpv = h2
        h2T1 = carve(16 * 128).rearrange("p (c t) -> p c t", c=16)
        h2T = [h2T1, h2T1]
        rws = carve(16 * 32).rearrange("p (c e) -> p c e", c=16)
        rbb = carve(32)

        vidx = {"gt1x": 0, "gt1c": 1, "sh2x": 2, "sh2c": 3, "sc2x": 4, "sc2c": 5, "gt2x": 6, "gt2c": 7, "n2g": 8, "fg": 9}
        for k in ("gt1x", "gt1c", "sh2x", "sh2c"):
            S.dma("sp", bc[k], vecs[vidx[k]].partition_broadcast(128), writes=["bc_" + k])
        S.dma("sp", tmpv, vecs[8].partition_broadcast(128), writes=["h2"])
        for k, sck in (("gscx", "sc2x"), ("gscc", "sc2c")):
            S.dma("sp", bc[k], vecs[vidx[sck]].partition_broadcast(128), writes=["bc_" + k])
            S.dve(lambda e, k=k: e.scalar_tensor_tensor(out=bc[k], in0=bc[k], scalar=1.0, in1=tmpv,
                                                        op0=ALU.add, op1=ALU.mult),
                  reads=["bc_" + k, "h2"], writes=["bc_" + k])
        S.dma("sp", rws, rw.rearrange("(c p) e -> p c e", p=128), writes=["rws"])
        S.dma("sp", rbb, rb.partition_broadcast(128), writes=["rbb"])

        k = 0
        for g in range(4):
            gs = slice(g * 512, (g + 1) * 512)
            wob = wo[g % 2]
            S.dma("pool", wob, w_out[:, gs].rearrange("(c p) n -> p c n", p=128), writes=[("wo", g % 2)])
            for ti, (r0, nt) in enumerate(TILES_B):
                kb = k % 2
                ymb, xgb, obb, psb = ym[kb], xg[kb], ob[kb], ps[kb]
                S.dma("pool", ymb[:, :, :nt], ymT[:, r0:r0 + nt].rearrange("(c p) t -> p c t", p=128),
                      writes=[("ym", kb)])
                S.dma("sp", xgb[:nt], xres[r0:r0 + nt, gs], writes=[("xg", kb)])
                for c in range(16):
                    S.pe(lambda e, c=c, ymb=ymb, wob=wob, psb=psb, nt=nt: e.matmul(
                        psb[:nt, :], ymb[:, c, :nt], wob[:, c, :], start=(c == 0), stop=(c == 15)),
                        reads=[("ym", kb), ("wo", g % 2)], writes=[("ps", kb)])
                gtb = bc["gt1c"] if ti == 0 else bc["gt1x"]
                S.dve(lambda e, obb=obb, psb=psb, gtb=gtb, nt=nt, gs=gs: e.tensor_tensor(
                    obb[:nt], psb[:nt, :], gtb[:nt, gs], ALU.mult),
                    reads=[("ps", kb), "bc_gt1c", "bc_gt1x"], writes=[("ob", kb)])
                S.dve(lambda e, obb=obb, xgb=xgb, nt=nt: e.tensor_tensor(obb[:nt], obb[:nt], xgb[:nt], ALU.add),
                      reads=[("ob", kb), ("xg", kb)], writes=[("ob", kb)])
                S.dma("act", xn[r0:r0 + nt, gs], obb[:nt], reads=[("ob", kb)], writes=[("xn", ti)])
                k += 1

        for ti, (r0, nt) in enumerate(TILES_B):
            kb = 0
            xtb, h2Tb = xt[kb], h2T[kb]
            sfx = "c" if ti == 0 else "x"
            S.dma("sp", xtb[:nt], xn[r0:r0 + nt, :], reads=[("xn", ti)], writes=[("xt", kb)])
            ss = small[:, 0:1]
            rstd = small[:, 1:2]
            S.dve(lambda e: e.memset(small[:, 0:2], 0.0), writes=["ss", "rstd"])
            S.act(lambda e, xtb=xtb, nt=nt: e.activation(out=h2[:nt], in_=xtb[:nt], func=AF.Square,
                                                         accum_out=small[:nt, 0:1]),
                  reads=[("xt", kb), "ss"], writes=["h2", "ss"])
            S.act(lambda e, nt=nt: e.activation(out=small[:nt, 1:2], in_=small[:nt, 0:1], func=AF.Sqrt, scale=1.0 / D, bias=epsb[:nt, 0:1]),
                  reads=["ss", "epsb"], writes=["rstd"])
            S.dve(lambda e, nt=nt: e.reciprocal(small[:nt, 1:2], small[:nt, 1:2]),
                  reads=["rstd"], writes=["rstd"])
            S.dve(lambda e, xtb=xtb, nt=nt, sfx=sfx: e.scalar_tensor_tensor(
                out=h2[:nt], in0=xtb[:nt], scalar=small[:nt, 1:2], in1=bc["gsc" + sfx][:nt],
                op0=ALU.mult, op1=ALU.mult),
                reads=[("xt", kb), "rstd", "bc_gscx", "bc_gscc"], writes=["h2"])
            S.dve(lambda e, nt=nt, sfx=sfx: e.tensor_tensor(h2[:nt], h2[:nt], bc["sh2" + sfx][:nt], ALU.add),
                  reads=["h2", "bc_sh2x", "bc_sh2c"], writes=["h2"])
            for c in range(16):
                q = 2 + c // 4
                S.pe(lambda e, c=c, q=q, nt=nt: e.transpose(
                    ps[q][:, (c % 4) * 128:(c % 4) * 128 + nt], h2[:nt, c * 128:(c + 1) * 128], ident[:nt, :nt]),
                    reads=["h2", "ident"], writes=[("ps", q)])
            for q in range(4):
                src = ps[2 + q][:, :].rearrange("p (c t) -> p c t", c=4)[:, :, :nt]
                dst = h2Tb[:, 4 * q:4 * q + 4, :nt]
                if q % 2 == 0:
                    S.act(lambda e, src=src, dst=dst: e.copy(dst, src), reads=[("ps", 2 + q)], writes=[("h2T", kb)])
                else:
                    S.dve(lambda e, src=src, dst=dst: e.tensor_copy(dst, src), reads=[("ps", 2 + q)],
                          writes=[("h2T", kb)])
            for c in range(16):
                S.pe(lambda e, c=c, h2Tb=h2Tb, nt=nt: e.matmul(
                    ps[6][:nt, 0:32], h2Tb[:, c, :nt], rws[:, c, :], start=(c == 0), stop=(c == 15)),
                    reads=[("h2T", kb), "rws"], writes=[("ps", 6)])
            S.dma("act", h2Td[:, r0:r0 + nt].rearrange("(c p) t -> p c t", p=128), h2Tb[:, :, :nt],
                  reads=[("h2T", kb)], writes=[("h2Td", ti)])
            sc = small[:, 32:64]
            bi = small[:, 64:96]
            eq = small[:, 96:128]
            mk = small[:, 128:160]
            sel = small[:, 160:192]
            ww = small[:, 192:224]
            m1 = small[:, 224:228]
            m2 = small[:, 228:232]
            gsum = small[:, 232:236]
            goh = small[:, 236:240]
            gm = small[:, 240:241]
            wsum = small[:, 241:242]

            def v3(a, nt):
                return a[:nt].rearrange("p (g e) -> p g e", g=4)

            def b3(a, nt):
                return a[:nt].unsqueeze(2).broadcast_to([nt, 4, 8])

            R = ["rt"]
            S.act(lambda e, nt=nt: e.activation(out=sc[:nt], in_=ps[6][:nt, 0:32], func=AF.Sigmoid),
                  reads=[("ps", 6)], writes=R)
            S.dve(lambda e, nt=nt: e.tensor_tensor(bi[:nt], sc[:nt], rbb[:nt], ALU.add), reads=R + ["rbb"], writes=R)
            S.dve(lambda e, nt=nt: e.tensor_reduce(out=m1[:nt], in_=v3(bi, nt), axis=AX.X, op=ALU.max), reads=R, writes=R)
            S.dve(lambda e, nt=nt: e.tensor_tensor(v3(eq, nt), v3(bi, nt), b3(m1, nt), ALU.is_equal), reads=R, writes=R)
            S.dve(lambda e, nt=nt: e.scalar_tensor_tensor(out=mk[:nt], in0=eq[:nt], scalar=-1e9, in1=bi[:nt],
                                                          op0=ALU.mult, op1=ALU.add), reads=R, writes=R)
            S.dve(lambda e, nt=nt: e.tensor_reduce(out=m2[:nt], in_=v3(mk, nt), axis=AX.X, op=ALU.max), reads=R, writes=R)
            S.dve(lambda e, nt=nt: e.tensor_tensor(gsum[:nt], m1[:nt], m2[:nt], ALU.add), reads=R, writes=R)
            S.dve(lambda e, nt=nt: e.tensor_reduce(out=gm[:nt], in_=gsum[:nt], axis=AX.X, op=ALU.max), reads=R, writes=R)
            S.dve(lambda e, nt=nt: e.tensor_scalar(goh[:nt], gsum[:nt], gm[:nt, 0:1], None, ALU.is_equal), reads=R, writes=R)
            S.dve(lambda e, nt=nt: e.tensor_tensor(v3(sel, nt), v3(bi, nt), b3(m2, nt), ALU.is_ge), reads=R, writes=R)
            S.dve(lambda e, nt=nt: e.tensor_tensor(v3(sel, nt), v3(sel, nt), b3(goh, nt), ALU.mult), reads=R, writes=R)
            S.dve(lambda e, nt=nt: e.tensor_tensor(ww[:nt], sc[:nt], sel[:nt], ALU.mult), reads=R, writes=R)
            S.dve(lambda e, nt=nt: e.tensor_reduce(out=wsum[:nt], in_=ww[:nt], axis=AX.X, op=ALU.add), reads=R, writes=R)
            S.dve(lambda e, nt=nt: e.reciprocal(wsum[:nt], wsum[:nt]), reads=R, writes=R)
            S.dve(lambda e, nt=nt, ti=ti: e.tensor_scalar(gates[:nt, ti, :], ww[:nt], wsum[:nt, 0:1], None, ALU.mult),
                  reads=R, writes=["gates"])

        S.barrier(dummy[:])

        off[0] = 0
        offr[0] = 0
        h2Tp = carver(16 * 544).rearrange("p (c t) -> p c t", c=16)
        acc = carve(5 * 2048).rearrange("p (j n) -> p j n", j=5)
        w1g = [carver(16 * 128).rearrange("p (c f) -> p c f", c=16) for _ in range(2)]
        w1u = [carver(16 * 128).rearrange("p (c f) -> p c f", c=16) for _ in range(2)]
        w2g = [carver(8 * 512).rearrange("p (f n) -> p f n", f=8) for _ in range(2)]
        actT = carver(8 * 544).rearrange("p (f t) -> p f t", f=8)
        xt2 = carve(2048)
        bc2 = {k: carve(2048) for k in ("gt2x", "gt2c", "fg")}
        S.dma("sp", bc2["gt2x"], vecs[6].partition_broadcast(128), writes=["bc_gt2x"])
        S.dma("sp", bc2["gt2c"], vecs[7].partition_broadcast(128), writes=["bc_gt2c"])
        S.dma("sp", bc2["fg"], vecs[9].partition_broadcast(128), writes=["bc_fg"])

        pi = 0
        w2c = 0
        yc = 0
        for p in range(4):
            ptiles = list(range(0, 5)) if p == 0 else list(range(4 * p + 1, 4 * p + 5))
            t0 = TILES_B[ptiles[0]][0]
            T = sum(TILES_B[t][1] for t in ptiles)
            chunks = [(0, 512), (512, 32)] if p == 0 else [(0, 512)]
            S.dma("pool", h2Tp[:, :, :T], h2Td[:, t0:t0 + T].rearrange("(c p) t -> p c t", p=128),
                  reads=[("h2Td", t) for t in ptiles], writes=["h2Tp"])
            S.pool(lambda e: e.memset(acc, 0.0), writes=[("acc", j) for j in range(5)])
            for ex in range(n_exp):
                w2bufs = {}
                for fb in range(8):
                    b = pi % 2
                    w1gb, w1ub = w1g[b], w1u[b]
                    S.dma("pool", w1gb, w1[ex, :, fb * 128:(fb + 1) * 128].rearrange("(c p) f -> p c f", p=128),
                          writes=[("w1g", b)])
                    S.dma("pool", w1ub, w1[ex, :, 1024 + fb * 128:1024 + (fb + 1) * 128].rearrange("(c p) f -> p c f", p=128),
                          writes=[("w1u", b)])
                    for ci, (c0, cn) in enumerate(chunks):
                        if ci == 0:
                            psg, psu = ps[(pi % 2) * 2][:, 0:cn], ps[(pi % 2) * 2 + 1][:, 0:cn]
                            rg, ru = ("ps", (pi % 2) * 2), ("ps", (pi % 2) * 2 + 1)
                        else:
                            o = (pi % 2) * 64
                            psg, psu = ps[4][:, o:o + 32], ps[4][:, o + 32:o + 64]
                            rg, ru = ("ps", 4), ("ps", 4)
                        for c in range(16):
                            S.pe(lambda e, c=c, psg=psg, w1gb=w1gb, c0=c0, cn=cn: e.matmul(
                                psg, w1gb[:, c, :], h2Tp[:, c, c0:c0 + cn], start=(c == 0), stop=(c == 15)),
                                reads=[("w1g", b), "h2Tp"], writes=[rg])
                        for c in range(16):
                            S.pe(lambda e, c=c, psu=psu, w1ub=w1ub, c0=c0, cn=cn: e.matmul(
                                psu, w1ub[:, c, :], h2Tp[:, c, c0:c0 + cn], start=(c == 0), stop=(c == 15)),
                                reads=[("w1u", b), "h2Tp"], writes=[ru])
                        dst = actT[:, fb, c0:c0 + cn]
                        S.act(lambda e, dst=dst, psg=psg: e.activation(out=dst, in_=psg, func=AF.Silu),
                              reads=[rg], writes=[("actT", fb, ci)])
                        S.dve(lambda e, dst=dst, psu=psu: e.tensor_tensor(dst, dst, psu, ALU.mult),
                              reads=[ru, ("actT", fb, ci)], writes=[("actT", fb, ci)])
                    pi += 1
                for grp in range(4):
                    wb = w2c % 2
                    w2c += 1
                    w2b = w2g[wb]
                    S.dma("pool", w2b, w2[ex, :, grp * 512:(grp + 1) * 512].rearrange("(f p) n -> p f n", p=128),
                          writes=[("w2g", wb)])
                    tt0 = 0
                    for j, t in enumerate(ptiles):
                        nt = TILES_B[t][1]
                        psy = ps[5 + yc % 2]
                        ry = ("ps", 5 + yc % 2)
                        yc += 1
                        for fb in range(8):
                            S.pe(lambda e, fb=fb, psy=psy, nt=nt, tt0=tt0, w2b=w2b: e.matmul(
                                psy[:nt, :], actT[:, fb, tt0:tt0 + nt], w2b[:, fb, :], start=(fb == 0), stop=(fb == 7)),
                                reads=[("actT", fb, 0), ("actT", fb, 1), ("w2g", wb)], writes=[ry])
                        gsl = slice(grp * 512, (grp + 1) * 512)
                        S.dve(lambda e, psy=psy, nt=nt, j=j, t=t, ex=ex, gsl=gsl: e.scalar_tensor_tensor(
                            out=acc[:nt, j, gsl], in0=psy[:nt, :], scalar=gates[:nt, t, ex:ex + 1],
                            in1=acc[:nt, j, gsl], op0=ALU.mult, op1=ALU.add),
                            reads=[ry, "gates", ("acc", j)], writes=[("acc", j)])
                        tt0 += nt
            for j, t in enumerate(ptiles):
                r0, nt = TILES_B[t]
                sfx = "c" if t == 0 else "x"
                S.dma("sp", xt2[:nt], xn[r0:r0 + nt, :], reads=[("xn", t)], writes=["xt2"])
                S.dve(lambda e, nt=nt, j=j, sfx=sfx: e.tensor_tensor(acc[:nt, j, :], acc[:nt, j, :], bc2["gt2" + sfx][:nt], ALU.mult),
                      reads=[("acc", j), "bc_gt2x", "bc_gt2c"], writes=[("acc", j)])
                S.dve(lambda e, nt=nt, j=j: e.tensor_tensor(acc[:nt, j, :], acc[:nt, j, :], xt2[:nt], ALU.add),
                      reads=[("acc", j), "xt2"], writes=[("acc", j)])
                S.dma("sp", out[r0:r0 + nt, :], acc[:nt, j, :], reads=[("acc", j)], writes=[("out", t)])
                S.dve(lambda e: e.memset(small[:, 0:2], 0.0), writes=["ss", "rstd"])
                S.act(lambda e, nt=nt, j=j: e.activation(out=xt2[:nt], in_=acc[:nt, j, :], func=AF.Square,
                                                         accum_out=small[:nt, 0:1]),
                      reads=[("acc", j), "ss"], writes=["xt2", "ss"])
                S.act(lambda e, nt=nt: e.activation(out=small[:nt, 1:2], in_=small[:nt, 0:1], func=AF.Sqrt, scale=1.0 / D, bias=epsb[:nt, 0:1]),
                      reads=["ss", "epsb"], writes=["rstd"])
                S.dve(lambda e, nt=nt: e.reciprocal(small[:nt, 1:2], small[:nt, 1:2]),
                      reads=["rstd"], writes=["rstd"])
                S.dve(lambda e, nt=nt, j=j: e.scalar_tensor_tensor(
                    out=xt2[:nt], in0=acc[:nt, j, :], scalar=small[:nt, 1:2], in1=bc2["fg"][:nt],
                    op0=ALU.mult, op1=ALU.mult),
                    reads=[("acc", j), "rstd", "bc_fg"], writes=["xt2"])
                S.dma("sp", outn[r0:r0 + nt, :], xt2[:nt], reads=["xt2"], writes=[("outn", t)])
        S.emit()
    return nc


def build_M():
    nc = bass.Bass("TRN2", target_bir_lowering=False)
    aw = nc.dram_tensor("aw", [2, 2048, 1536], F32, kind="ExternalInput").ap()
    ab = nc.dram_tensor("ab", [2, 1536], F32, kind="ExternalInput").ap()
    cc = nc.dram_tensor("cc", [128, 16, 2], F32, kind="ExternalInput").ap()
    mo = nc.dram_tensor("mo", [2, 2, 1536], F32, kind="ExternalOutput").ap()
    with contextlib.ExitStack() as st:
        S = Sched(nc, st)
        cct = st.enter_context(nc.sbuf_tensor("cct", [128, 16, 2], F32))
        sct = st.enter_context(nc.sbuf_tensor("sct", [128, 16, 2], F32))
        awt = [st.enter_context(nc.sbuf_tensor("awt%d" % i, [128, 16, 512], F32)) for i in range(2)]
        bt = [st.enter_context(nc.sbuf_tensor("bt%d" % i, [2, 512], F32)) for i in range(2)]
        ot = [st.enter_context(nc.sbuf_tensor("ot%d" % i, [2, 512], F32)) for i in range(2)]
        ps = [st.enter_context(nc.psum_tensor("psm%d" % i, [128, 512], F32)) for i in range(2)]
        S.dma("sp", cct[:], cc[:, :, :], writes=["cct"])
        S.act(lambda e: e.activation(out=sct[:], in_=cct[:], func=AF.Silu), reads=["cct"], writes=["sct"])
        k = 0
        for l in range(2):
            for g in range(3):
                b = k % 2
                k += 1
                gs = slice(g * 512, (g + 1) * 512)
                S.dma("sp" if b == 0 else "act", awt[b][:], aw[l, :, gs].rearrange("(c p) n -> p c n", p=128), writes=[("awt", b)])
                S.dma("sp", bt[b][:], ab[l, gs].partition_broadcast(2), writes=[("bt", b)])
                for c in range(16):
                    S.pe(lambda e, c=c, b=b: e.matmul(ps[b][0:2, :], sct[:, c, :], awt[b][:, c, :], start=(c == 0), stop=(c == 15)),
                         reads=["sct", ("awt", b)], writes=[("ps", b)])
                S.dve(lambda e, b=b: e.tensor_tensor(ot[b][:], ps[b][0:2, :], bt[b][:], ALU.add),
                      reads=[("ps", b), ("bt", b)], writes=[("ot", b)])
                S.dma("sp", mo[l, :, gs], ot[b][:], reads=[("ot", b)], writes=["mo"])
        S.emit()
    return nc

import numpy as np

RWKV_IN = 3488


def rope_tables():
    half = 64
    inv = (10000.0 ** (-np.arange(0, half, 2, dtype=np.float32) / half)).astype(np.float32)
    t = np.arange(16384)
    posr = (t // 64).astype(np.float32)
    posc = (t % 64).astype(np.float32)
    cosT = np.zeros((128, 16384), np.float32)
    sinT = np.zeros((128, 16384), np.float32)
    for hb, pos in ((0, posr), (64, posc)):
        ang = (pos[None, :] * inv[:, None]).astype(np.float32)
        c, s = np.cos(ang).astype(np.float32), np.sin(ang).astype(np.float32)
        cosT[hb:hb + 32] = c
        cosT[hb + 32:hb + 64] = c
        sinT[hb:hb + 32] = -s
        sinT[hb + 32:hb + 64] = s
    return cosT, sinT


SWAP = np.concatenate([np.arange(32, 64), np.arange(0, 32), np.arange(96, 128), np.arange(64, 96)])


def col_blocks(c):
    oc = np.arange(c * 128, c * 128 + 128)
    blocks = [oc, 1024 + oc, 2048 + oc, np.arange(3072, 3200), np.arange(3200, 3328), np.arange(3328, 3456),
              np.arange(3456, 3488), RWKV_IN + oc, RWKV_IN + 1024 + oc, RWKV_IN + 2048 + oc, RWKV_IN + 3072 + oc,
              RWKV_IN + oc[SWAP], RWKV_IN + 1024 + oc[SWAP]]
    return blocks


def fp(v):
    return np.ascontiguousarray(v.reshape(16, 128).T)


def prep_A(inp, l, c, xT, mod_x, mod_c, tables, cst):
    blocks = col_blocks(c)
    cols = np.concatenate(blocks)
    sh1, sc1 = mod_x[0:2048], mod_x[2048:4096]
    csh1, csc1 = mod_c[0:2048], mod_c[2048:4096]
    mvec = np.stack([fp(sh1), fp(sc1), fp(csh1), fp(csc1), fp(inp["norm1_g"][l])], axis=1).astype(np.float32)
    mu = np.zeros((128, 7, 4), np.float32)
    for b in range(7):
        cb = blocks[b]
        mu[:len(cb), b, :] = inp["shift_mu"][l][:, cb].T
    oc = np.arange(c * 128, c * 128 + 128)
    pvec = np.zeros((128, 8), np.float32)
    pvec[:, 0] = inp["rwkv_w0"][l, 0, oc]
    pvec[:, 1] = inp["rwkv_w0"][l, 1, oc]
    pvec[:, 2] = inp["rwkv_a0"][l, 0, oc]
    pvec[:, 3] = inp["rwkv_a0"][l, 1, oc]
    pvec[:, 4] = inp["rwkv_k_k"][l, oc]
    pvec[:, 5] = inp["rwkv_k_a"][l, oc]
    pvec[:, 6] = inp["rwkv_r_k"][l].reshape(-1)[oc]
    w2s = np.concatenate([inp["rwkv_w2"][l, 0][:, oc], inp["rwkv_w2"][l, 1][:, oc]], 0)
    a2s = np.concatenate([inp["rwkv_a2"][l, 0][:, oc], inp["rwkv_a2"][l, 1][:, oc]], 0)
    g2 = inp["rwkv_g2"][l]
    rowv = np.stack([inp["rwkv_lnx_w"][l, oc], inp["rwkv_lnx_b"][l, oc], inp["ret_gn_w"][l, oc]])
    return {
        "xT": xT, "W": np.ascontiguousarray(inp["w_in"][l][:, cols]), "mvec": mvec, "mu": mu, "pvec": pvec,
        "w2s": np.ascontiguousarray(w2s), "a2s": np.ascontiguousarray(a2s),
        "g2a": np.ascontiguousarray(g2[0:128, oc]), "g2b": np.ascontiguousarray(g2[128:160, oc]),
        "rowv": np.ascontiguousarray(rowv.astype(np.float32)),
        "dec": np.ascontiguousarray(inp["ret_log2_decay"][l, :, c]),
        "cosT": tables[0], "sinT": tables[1], "cst": cst,
    }

def kernel(**inp):
    inp = {k: np.asarray(v) for k, v in inp.items()}
    x = inp["x"][0]
    ctx = inp["ctx"][0]
    cores = list(range(8))
    ncM = build_M()
    cc = np.ascontiguousarray(np.stack([fp(inp["c"][0]), fp(inp["c_ctx"])], axis=2).astype(np.float32))
    ims = []
    for c in cores:
        cs = slice(c * 1536, (c + 1) * 1536)
        ims.append({"aw": np.ascontiguousarray(inp["ada_w"][:, :, cs]),
                    "ab": np.ascontiguousarray(inp["ada_b"][:, cs]), "cc": cc})
    res = run_bass_kernel_spmd(ncM, ims, core_ids=cores)
    mod = np.concatenate([r["mo"] for r in res.results], axis=2)
    del ims
    ncA = build_A()
    ncB = build_B()
    xin = np.concatenate([ctx, x], 0).astype(np.float32)
    tables = rope_tables()
    cst = make_cst()
    ident = np.eye(128, dtype=np.float32)
    rows_of = [np.concatenate([np.arange(32 * c, 32 * c + 32), 256 + np.arange(2048 * c, 2048 * c + 2048)]) for c in cores]
    for l in range(2):
        mod_x, mod_c = mod[l, 0], mod[l, 1]
        xT = np.ascontiguousarray(xin.T)
        imsA = [prep_A(inp, l, c, xT, mod_x, mod_c, tables, cst) for c in cores]
        resA = run_bass_kernel_spmd(ncA, imsA, core_ids=cores)
        del imsA, xT
        ymix = np.empty((NTOK, 2048), np.float32)
        for c in cores:
            o = resA.results[c]["out"]
            ymix[:, c * 128:(c + 1) * 128] = o[:, :128]
            ymix[:, 1024 + c * 128:1024 + (c + 1) * 128] = o[:, 128:]
        del resA
        sh1, sc1, gt1, sh2, sc2, gt2 = np.split(mod_x, 6)
        csh1, csc1, cgt1, csh2, csc2, cgt2 = np.split(mod_c, 6)
        vecs = np.ascontiguousarray(np.stack([gt1, cgt1, sh2, csh2, sc2, csc2, gt2, cgt2,
                                              inp["norm2_g"][l], inp["final_g"]]).astype(np.float32))
        imsB = []
        for c in cores:
            rows = rows_of[c]
            imsB.append({"ymT": np.ascontiguousarray(ymix[rows].T), "xres": np.ascontiguousarray(xin[rows]),
                         "w_out": inp["w_out"][l], "w1": inp["moe_w1"][l], "w2": inp["moe_w2"][l],
                         "rw": inp["router_w"], "rb": inp["router_b"], "vecs": vecs, "ident": ident})
        resB = run_bass_kernel_spmd(ncB, imsB, core_ids=cores)
        del imsB, ymix
        key = "outn" if l == 1 else "out"
        xnew = np.empty_like(xin)
        for c in cores:
            xnew[rows_of[c]] = resB.results[c][key]
        del resB
        xin = xnew
    return np.ascontiguousarray(xin[256:].reshape(1, 16384, 2048).astype(np.float32))
```

```python
import contextlib
import numpy as np
import concourse.bass as bass
import concourse.mybir as mybir
from concourse.bass_utils import run_bass_kernel_spmd

F32 = mybir.dt.float32
F32R = mybir.dt.float32r
AF = mybir.ActivationFunctionType
ALU = mybir.AluOpType
AX = mybir.AxisListType

ENGS = ("pe", "act", "dve", "pool", "sp")
DMAQ = ("sp", "act", "pool")
NDSEM = 6


class _Op:
    __slots__ = ("eng", "fn", "dma", "deps", "sig", "dsem", "dval", "consumed", "idx")

    def __init__(self, eng, fn, dma):
        self.eng = eng
        self.fn = fn
        self.dma = dma
        self.deps = ()
        self.sig = 0
        self.dsem = None
        self.dval = 0
        self.consumed = False
        self.idx = 0


class Sched:
    def __init__(self, nc, stack):
        self.nc = nc
        self.ops = {e: [] for e in ENGS}
        self.res = {}
        self.sem = {e: stack.enter_context(nc.semaphore("sem_" + e)) for e in ENGS}
        self.dsem = {e: [stack.enter_context(nc.semaphore("dsem_%s%d" % (e, i)))
                         for i in range(NDSEM)] for e in DMAQ}
        self.duse = {e: [None] * NDSEM for e in DMAQ}
        self.dcnt = {e: 0 for e in DMAQ}
        self.nops = 0
        self.rec = None
        self.cur = None

    def stage(self, name):
        self.cur = name

    def begin(self, names):
        self.rec = {n: [] for n in names}

    def end(self):
        r = self.rec
        self.rec = None
        return r

    def replay(self, lst):
        for t in lst:
            self.add(t[0], t[1], t[2], t[3], t[4])

    def add(self, eng, fn, reads=(), writes=(), dma=False, hold=False):
        if self.rec is not None:
            self.rec[self.cur].append((eng, fn, tuple(reads), tuple(writes), dma, hold))
            return None
        op = _Op(eng, fn, dma)
        op.idx = self.nops
        self.nops += 1
        deps = {}
        res = self.res
        if "PHASE" not in writes:
            reads = tuple(reads) + ("PHASE",)
        for r in reads:
            st = res.get(r)
            if st is None:
                st = res[r] = [None, {}]
            if st[0] is not None:
                deps[id(st[0])] = st[0]
        for w in writes:
            st = res.get(w)
            if st is None:
                st = res[w] = [None, {}]
            if st[0] is not None:
                deps[id(st[0])] = st[0]
            for rd in st[1].values():
                deps[id(rd)] = rd
        if dma:
            k = self.dcnt[eng] % NDSEM
            self.dcnt[eng] += 1
            prev = self.duse[eng][k]
            if prev is not None:
                deps[id(prev)] = prev
            op.dsem = (eng, k)
            op.dval = (prev.dval if prev is not None else 0) + 16
            self.duse[eng][k] = op
        for r in reads:
            st = res[r]
            key = (("d",) + op.dsem) if dma else eng
            st[1][key] = op
        for w in writes:
            res[w] = [op, {}]
        deps.pop(id(op), None)
        dl = []
        for d in deps.values():
            if (not d.dma) and d.eng == eng and eng == "pe":
                continue
            d.consumed = True
            dl.append(d)
        op.deps = dl
        self.ops[eng].append(op)
        return op

    def pe(self, fn, reads=(), writes=(), hold=False):
        return self.add("pe", fn, reads, writes, hold=hold)

    def act(self, fn, reads=(), writes=()):
        return self.add("act", fn, reads, writes)

    def dve(self, fn, reads=(), writes=()):
        return self.add("dve", fn, reads, writes)

    def pool(self, fn, reads=(), writes=()):
        return self.add("pool", fn, reads, writes)

    def barrier(self, tile):
        return self.add("dve", lambda e: e.memset(tile, 0.0), (), ("PHASE",))

    def dma(self, q, out, in_, reads=(), writes=()):
        return self.add(q, lambda e: e.dma_start(out=out, in_=in_), reads, writes, dma=True)

    def emit(self):
        nc = self.nc
        eobj = {"pe": nc.tensor, "act": nc.scalar, "dve": nc.vector, "pool": nc.gpsimd, "sp": nc.sync}
        for e in ENGS:
            n = 0
            for op in self.ops[e]:
                if op.consumed and not op.dma:
                    n += 1
                    op.sig = n
        sem = self.sem
        dsem = self.dsem
        duse = self.duse
        allops = self.ops

        def run(e, eng):
            known = {}
            for op in allops[e]:
                need = {}
                for d in op.deps:
                    if d.dma:
                        key = ("d",) + d.dsem
                        val = d.dval
                        s = dsem[d.dsem[0]][d.dsem[1]]
                    else:
                        key = d.eng
                        val = d.sig
                        s = sem[d.eng]
                    if known.get(key, 0) >= val:
                        continue
                    if key not in need or need[key][0] < val:
                        need[key] = (val, s)
                for key, (val, s) in need.items():
                    known[key] = val
                    eng.wait_ge(s, val)
                ins = op.fn(eng)
                if op.dma:
                    ins.then_inc(dsem[op.dsem[0]][op.dsem[1]], 16)
                elif op.consumed:
                    ins.then_inc(sem[e], 1)
            if e in DMAQ:
                for k in range(NDSEM):
                    last = duse[e][k]
                    if last is not None and known.get(("d", e, k), 0) < last.dval:
                        eng.wait_ge(dsem[e][k], last.dval)

        with nc.Block() as block:
            @block.sync
            def _(eng):
                run("sp", eng)

            @block.tensor
            def _(eng):
                run("pe", eng)

            @block.scalar
            def _(eng):
                run("act", eng)

            @block.vector
            def _(eng):
                run("dve", eng)

            @block.gpsimd
            def _(eng):
                run("pool", eng)


def zipper(lists):
    lists = [l for l in lists if l]
    out = []
    idx = [0] * len(lists)
    n = [len(l) for l in lists]
    while True:
        best = -1
        bf = 2.0
        for i in range(len(lists)):
            if idx[i] < n[i]:
                f = idx[i] / n[i]
                if f < bf:
                    bf = f
                    best = i
        if best < 0:
            break
        l = lists[best]
        while True:
            t = l[idx[best]]
            out.append(t)
            idx[best] += 1
            if not t[5] or idx[best] >= n[best]:
                break
    return out

import math

NTOK = 16640
NCTX = 256
NCOLB = 13
KAPPA = math.exp(-0.5)
LNX_EPS = 64e-5
RET_SCALE = 128 ** -0.5
C_DIFF, C_NP1, C_N128, C_MLT, C_MLE, C_MGT, C_MGE, C_ID, C_BONES = [128 * i for i in range(9)]
C_HSEL = 128 * 9
C_COLM = C_HSEL + 2
C_COL127 = C_COLM + 1
C_ONES = C_COL127 + 1
CW = C_ONES + 128


def make_cst():
    m = np.arange(128, dtype=np.float32)[:, None]
    n = np.arange(128, dtype=np.float32)[None, :]
    c = np.zeros((128, CW), np.float32)
    c[:, C_DIFF:C_DIFF + 128] = n - m
    c[:, C_NP1:C_NP1 + 128] = n + 1 + 0 * m
    c[:, C_N128:C_N128 + 128] = 128 - n + 0 * m
    c[:, C_MLT:C_MLT + 128] = (m < n)
    c[:, C_MLE:C_MLE + 128] = (m <= n)
    c[:, C_MGT:C_MGT + 128] = (m > n)
    c[:, C_MGE:C_MGE + 128] = (m >= n)
    c[:, C_ID:C_ID + 128] = (m == n)
    c[:, C_BONES:C_BONES + 128] = ((m // 64) == (n // 64))
    c[:, C_HSEL] = (m[:, 0] < 64)
    c[:, C_HSEL + 1] = (m[:, 0] >= 64)
    c[:, C_COLM] = m[:, 0]
    c[:, C_COL127] = 127 - m[:, 0]
    c[:, C_ONES:C_ONES + 128] = 1.0
    return c


def build_A():
    nc = bass.Bass("TRN2", target_bir_lowering=False)

    def din(name, shape):
        return nc.dram_tensor(name, shape, F32, kind="ExternalInput").ap()

    xT = din("xT", [2048, NTOK])
    W = din("W", [2048, 1568])
    mvec = din("mvec", [128, 5, 16])
    mu = din("mu", [128, 7, 4])
    pvec = din("pvec", [128, 8])
    w2s = din("w2s", [128, 128])
    a2s = din("a2s", [128, 128])
    g2a = din("g2a", [128, 128])
    g2b = din("g2b", [32, 128])
    rowv = din("rowv", [3, 128])
    dec = din("dec", [2])
    cosT = din("cosT", [128, 16384])
    sinT = din("sinT", [128, 16384])
    cst_d = din("cst", [128, CW])
    out = nc.dram_tensor("out", [NTOK, 256], F32, kind="ExternalOutput").ap()
    pT = nc.dram_tensor("pT", [NCOLB, 128, NTOK], F32).ap()
    yfw = nc.dram_tensor("yfw", [NTOK, 260], F32).ap()
    rsd = nc.dram_tensor("rsd", [128, NTOK], F32).ap()

    with contextlib.ExitStack() as st:
        S = Sched(nc, st)
        U = st.enter_context(nc.sbuf_tensor("U", [128, 28400], F32))
        UR2 = st.enter_context(nc.sbuf_tensor("UR2", [128, 21008], F32R))
        cst = st.enter_context(nc.sbuf_tensor("cstt", [128, CW], F32))
        pers = st.enter_context(nc.sbuf_tensor("pers", [128, 2048], F32))
        dummy = st.enter_context(nc.sbuf_tensor("dummya", [128, 8], F32))
        ps = [st.enter_context(nc.psum_tensor("ps%d" % i, [128, 512], F32)) for i in range(8)]
        off = [0]

        def carve(n):
            a = U[:, off[0]:off[0] + n]
            off[0] += n
            assert off[0] <= 28400, off[0]
            return a

        poff = [0]

        def pcarve(n):
            a = pers[:, poff[0]:poff[0] + n]
            poff[0] += n
            assert poff[0] <= 2040
            return a

        S.dma("sp", cst[:], cst_d[:, :], writes=["cst"])
        epsv = pers[:, 2040:2042]
        S.dve(lambda e: e.memset(pers[:, 2040:2041], 1e-6), writes=["epsv"])
        S.dve(lambda e: e.memset(pers[:, 2041:2042], LNX_EPS), writes=["epsv"])
        ones = cst[:, C_ONES:C_ONES + 128]
        ident = cst[:, C_ID:C_ID + 128]

        mv = pcarve(80).rearrange("p (v c) -> p v c", v=5)
        S.dma("sp", mv, mvec[:, :, :], writes=["mv"])
        mus = pcarve(28).rearrange("p (b k) -> p b k", b=7)
        S.dma("sp", mus, mu[:, :, :], writes=["mus"])
        pv = pcarve(8)
        S.dma("sp", pv, pvec[:, :], writes=["pv"])
        pvh = pcarve(4)
        S.dve(lambda e: e.tensor_scalar(pvh, pv[:, 0:4], 0.5, None, ALU.mult), reads=["pv"], writes=["pvh"])
        w2t = pcarve(128); a2t = pcarve(128); g2at = pcarve(128); g2bt = pcarve(128)
        S.dma("sp", w2t, w2s[:, :], writes=["w2t"])
        S.dma("sp", a2t, a2s[:, :], writes=["a2t"])
        S.dma("sp", g2at, g2a[:, :], writes=["g2at"])
        S.dma("sp", g2bt[0:32, :], g2b[:, :], writes=["g2bt"])
        lnw = pcarve(128); lnb = pcarve(128); gnw = pcarve(128)
        S.dma("sp", lnw, rowv[0].partition_broadcast(128), writes=["rowp"])
        S.dma("sp", lnb, rowv[1].partition_broadcast(128), writes=["rowp"])
        S.dma("sp", gnw, rowv[2].partition_broadcast(128), writes=["rowp"])
        dect = pcarve(2)
        S.dma("sp", dect, dec.partition_broadcast(128), writes=["dect"])
        sx = pcarve(16); sc_ = pcarve(16)
        c0 = pcarve(7)
        c0c = pcarve(7)
        bias = pcarve(16)
        lg = pcarve(2); nlg = pcarve(2); gC = pcarve(2); zeta = pcarve(2)
        state = pcarve(128)
        Sret = pcarve(128)
        dmT = [pcarve(128), pcarve(128)]
        xibc = [pcarve(128), pcarve(128)]

        S.dve(lambda e: e.scalar_tensor_tensor(out=sx, in0=mv[:, 1, :], scalar=1.0, in1=mv[:, 4, :], op0=ALU.add, op1=ALU.mult),
              reads=["mv"], writes=["sx"])
        S.dve(lambda e: e.scalar_tensor_tensor(out=sc_, in0=mv[:, 3, :], scalar=1.0, in1=mv[:, 4, :], op0=ALU.add, op1=ALU.mult),
              reads=["mv"], writes=["sc_"])
        S.dve(lambda e: e.tensor_reduce(out=c0, in_=mus, axis=AX.X, op=ALU.add), reads=["mus"], writes=["c0"])
        S.dve(lambda e: e.tensor_scalar(c0, c0, -1.0, 1.0, ALU.mult, ALU.add), reads=["c0"], writes=["c0"])
        S.dve(lambda e: e.tensor_reduce(out=c0c, in_=mus[:, :, 0:2], axis=AX.X, op=ALU.add), reads=["mus"], writes=["c0c"])
        S.dve(lambda e: e.tensor_scalar(c0c, c0c, -1.0, 1.0, ALU.mult, ALU.add), reads=["c0c"], writes=["c0c"])
        S.act(lambda e: e.activation(out=lg, in_=dect, func=AF.Exp, scale=-math.log(2.0)), reads=["dect"], writes=["lg"])
        S.act(lambda e: e.activation(out=lg, in_=lg, func=AF.Ln, scale=-1.0, bias=1.0), reads=["lg"], writes=["lg"])
        S.dve(lambda e: e.tensor_scalar(nlg, lg, -1.0, None, ALU.mult), reads=["lg"], writes=["nlg"])
        S.act(lambda e: e.activation(out=gC, in_=lg, func=AF.Exp, scale=128.0), reads=["lg"], writes=["gC"])
        S.act(lambda e: e.activation(out=dmT[0], in_=cst[:, C_DIFF:C_DIFF + 128], func=AF.Exp, scale=lg[:, 0:1]),
              reads=["cst", "lg"], writes=["dmT"])
        S.dve(lambda e: e.scalar_tensor_tensor(out=dmT[0], in0=dmT[0], scalar=RET_SCALE, in1=cst[:, C_MLE:C_MLE + 128],
                                               op0=ALU.mult, op1=ALU.mult), reads=["dmT", "cst"], writes=["dmT"])
        S.act(lambda e: e.activation(out=dmT[1], in_=cst[:, C_DIFF:C_DIFF + 128], func=AF.Exp, scale=nlg[:, 1:2]),
              reads=["cst", "nlg"], writes=["dmT"])
        S.dve(lambda e: e.scalar_tensor_tensor(out=dmT[1], in0=dmT[1], scalar=RET_SCALE, in1=cst[:, C_MGT:C_MGT + 128],
                                               op0=ALU.mult, op1=ALU.mult), reads=["dmT", "cst"], writes=["dmT"])
        S.act(lambda e: e.activation(out=xibc[0], in_=cst[:, C_NP1:C_NP1 + 128], func=AF.Exp, scale=lg[:, 0:1]),
              reads=["cst", "lg"], writes=["xibc"])
        S.act(lambda e: e.activation(out=xibc[1], in_=cst[:, C_N128:C_N128 + 128], func=AF.Exp, scale=lg[:, 1:2]),
              reads=["cst", "lg"], writes=["xibc"])
        S.act(lambda e: e.activation(out=zeta[:, 0:1], in_=cst[:, C_COL127:C_COL127 + 1], func=AF.Exp, scale=lg[:, 0:1]),
              reads=["cst", "lg"], writes=["zeta"])
        S.act(lambda e: e.activation(out=zeta[:, 1:2], in_=cst[:, C_COLM:C_COLM + 1], func=AF.Exp, scale=lg[:, 1:2]),
              reads=["cst", "lg"], writes=["zeta"])
        S.dve(lambda e: e.tensor_scalar(zeta, zeta, RET_SCALE, None, ALU.mult), reads=["zeta"], writes=["zeta"])

        roff = [0]

        def rcarve(n):
            a = UR2[:, roff[0]:roff[0] + n]
            roff[0] += n
            assert roff[0] <= 21008, roff[0]
            return a
        Wh = rcarve(16 * 800).rearrange("p (c n) -> p c n", c=16)
        xb = [rcarve(16 * 256).rearrange("p (c t) -> p c t", c=16) for _ in range(2)]
        sh1r = rcarve(16)
        sq = carve(16 * 256).rearrange("p (c t) -> p c t", c=16)
        rsb = carve(256)
        tmpb = [carve(256) for _ in range(2)]
        pob = [carve(256) for _ in range(2)]
        colw = [128] * 6 + [32] + [128] * 6
        colo = [0]
        for wdt in colw:
            colo.append(colo[-1] + wdt)
        halves = [(list(range(0, 7)), 0, 800), (list(range(7, 13)), 800, 768)]

        blocks = [(0, 256, True)] + [(256 + 256 * i, 256, False) for i in range(64)]
        kk_ = 0
        cnt = 0
        for variant in ("c", "x"):
            vi = 2 if variant == "c" else 0
            sv = sc_ if variant == "c" else sx
            for (hblocks, hoff, hw) in halves:
                S.dma("pool", Wh[:, :, 0:hw], W[:, hoff:hoff + hw].rearrange("(c p) n -> p c n", p=128), writes=["Wp"])
                for b in hblocks:
                    wdt = colw[b]
                    pq = ps[7][:wdt, b:b + 1]
                    lo = colo[b] - hoff
                    for c in range(16):
                        S.pe(lambda e, c=c, pq=pq, lo=lo, wdt=wdt, vi=vi: e.matmul(pq, Wh[:, c, lo:lo + wdt].bitcast(F32), mv[:, vi, c:c + 1],
                                                                                     start=(c == 0), stop=(c == 15)),
                             reads=["Wp", "mv"], writes=["ps7"])
                    S.dve(lambda e, b=b, wdt=wdt: e.tensor_copy(bias[:wdt, b:b + 1], ps[7][:wdt, b:b + 1]), reads=["ps7"], writes=["bias"])
                for c in range(16):
                    S.dve(lambda e, c=c, sv=sv, hw=hw: e.tensor_scalar(Wh[:, c, 0:hw], Wh[:, c, 0:hw], sv[:, c:c + 1], None, ALU.mult),
                          reads=["Wp", "sx", "sc_"], writes=["Wp"])
                for (t0, tn, isctx) in blocks:
                    if isctx != (variant == "c"):
                        continue
                    xbb = xb[kk_ % 2]
                    rx = ("xb", kk_ % 2)
                    kk_ += 1
                    S.dma("pool", xbb, xT[:, t0:t0 + tn].rearrange("(c p) t -> p c t", p=128), writes=[rx])
                    if hoff == 0:
                        S.act(lambda e, xbb=xbb: e.activation(out=sq, in_=xbb, func=AF.Square), reads=[rx], writes=["sq"])
                        for c in range(16):
                            S.pe(lambda e, c=c: e.matmul(ps[6][:, 0:256], ones, sq[:, c, :], start=(c == 0), stop=(c == 15)),
                                 reads=["sq", "cst"], writes=["ps6"])
                        S.act(lambda e: e.activation(out=rsb, in_=ps[6][:, 0:256], func=AF.Sqrt, scale=1.0 / 2048, bias=epsv[:, 0:1]),
                              reads=["ps6", "epsv"], writes=["rsb"])
                        S.dve(lambda e: e.reciprocal(rsb, rsb), reads=["rsb"], writes=["rsb"])
                        S.dma("sp", rsd[:, t0:t0 + tn], rsb, reads=["rsb"], writes=[("rsd", t0)])
                    else:
                        S.dma("sp", rsb, rsd[:, t0:t0 + tn], reads=[("rsd", t0)], writes=["rsb"])
                    for b in hblocks:
                        wdt = colw[b]
                        lo = colo[b] - hoff
                        pb = cnt % 4
                        cnt += 1
                        pq = ps[pb][:wdt, 0:256]
                        for c in range(16):
                            S.pe(lambda e, c=c, pq=pq, lo=lo, wdt=wdt, xbb=xbb: e.matmul(
                                pq, Wh[:, c, lo:lo + wdt], xbb[:, c, :], start=(c == 0), stop=(c == 15)),
                                reads=["Wp", rx], writes=[("psp", pb)])
                        tb, ob_ = tmpb[cnt % 2], pob[cnt % 2]
                        S.dve(lambda e, tb=tb, pq=pq, wdt=wdt: e.tensor_tensor(tb[:wdt], pq, rsb[:wdt], ALU.mult),
                              reads=[("psp", pb), "rsb"], writes=[("tmpb", cnt % 2)])
                        S.act(lambda e, tb=tb, ob_=ob_, wdt=wdt, b=b: e.activation(out=ob_[:wdt], in_=tb[:wdt], func=AF.Identity,
                                                                                   bias=bias[:wdt, b:b + 1]),
                              reads=[("tmpb", cnt % 2), "bias"], writes=[("pob", cnt % 2)])
                        S.dma("sp", pT[b, :wdt, t0:t0 + tn], ob_[:wdt], reads=[("pob", cnt % 2)], writes=["pT"])
        S.barrier(dummy[:])

        off[0] = 0
        NT_ = [0]

        def T(par=2):
            r = []
            for _ in range(par):
                NT_[0] += 1
                r.append((carve(128), "t%d" % NT_[0]))
            return r

        names = ["H", "R", "u", "cosb", "sinb", "kkraw", "sq", "rn", "kk", "th", "sig", "cs", "E", "Ex", "gin", "gex", "ginv",
                 "a", "t1", "kd", "b", "bt", "at", "kt", "rt", "vtok", "atok", "ktok", "zT", "sgd", "gtok",
                 "qr", "kr", "qx", "vr", "krz", "PT", "ytok", "yret", "yfwb", "o1", "o2", "gsil", "gtot", "stmp", "gsl"]
        tl = {}
        for nme in names:
            if nme == "H":
                tl[nme] = []
                for _ in range(2):
                    NT_[0] += 1
                    tl[nme].append((carve(7 * 256).rearrange("p (b t) -> p b t", b=7), "t%d" % NT_[0]))
            elif nme in ("R", "u"):
                nb = 6 if nme == "R" else 7
                tl[nme] = []
                for _ in range(2):
                    NT_[0] += 1
                    tl[nme].append((carve(nb * 128).rearrange("p (b t) -> p b t", b=nb), "t%d" % NT_[0]))
            elif nme in ("atok", "ktok"):
                tl[nme] = []
                for _ in range(2):
                    NT_[0] += 1
                    tl[nme].append((carve(256).rearrange("p (h k) -> p h k", h=2), "t%d" % NT_[0]))
            elif nme == "yfwb":
                tl[nme] = []
                for _ in range(2):
                    NT_[0] += 1
                    tl[nme].append((carve(260), "t%d" % NT_[0]))
            else:
                tl[nme] = T(2)
        hn = ["N", "A", "BK", "RA", "RK", "Np", "Ap", "P", "Q", "Np2", "Ap2", "P2", "Q2", "RHS", "Us"]
        th_ = {nme: [T(2), T(2)] for nme in hn}
        gCr = carve(4)
        tot = carve(4)
        sm = carve(64)
        bsv = carve(4)
        for nme in ("atok", "ktok"):
            for (ap, rn) in tl[nme]:
                S.pool(lambda e, ap=ap: e.memset(ap, 0.0), writes=[rn])

        pools = {"P0": [4], "PR": [4], "H0": [0, 1], "H1": [2, 3], "S": [5, 7]}
        pqc = {k: 0 for k in pools}

        def PQ():
            pl = pools[S.cur]
            i = pl[pqc[S.cur] % len(pl)]
            pqc[S.cur] += 1
            return ps[i][:, 0:128], ("pq", i)

        def mm(o, ro, lhsT, rl, rhs, rr, start=True, stop=True):
            S.pe(lambda e: e.matmul(o, lhsT, rhs, start=start, stop=stop), reads=list(rl) + list(rr), writes=[ro],
                 hold=(not stop))

        def tr(o, ro, in_, ri):
            S.pe(lambda e: e.transpose(o, in_, ident), reads=[ri, "cst"], writes=[ro])

        def TT(eng, o, ro, a, ra, b, rb, op):
            S.add(eng, lambda e: e.tensor_tensor(o, a, b, op), reads=[ra, rb], writes=[ro])

        def TS(eng, o, ro, a, ra, s1, s2, op0, op1=None, extra=()):
            if op1 is None:
                S.add(eng, lambda e: e.tensor_scalar(o, a, s1, None, op0), reads=[ra] + list(extra), writes=[ro])
            else:
                S.add(eng, lambda e: e.tensor_scalar(o, a, s1, s2, op0, op1), reads=[ra] + list(extra), writes=[ro])

        def STT(eng, o, ro, a, ra, sc, b, rb, op0, op1, extra=()):
            S.add(eng, lambda e: e.scalar_tensor_tensor(out=o, in0=a, scalar=sc, in1=b, op0=op0, op1=op1),
                  reads=[ra, rb] + list(extra), writes=[ro])

        def ACT(o, ro, a, ra, func, bias=None, scale=None, extra=(), accum=None):
            kw = {}
            if bias is not None:
                kw["bias"] = bias
            if scale is not None:
                kw["scale"] = scale
            if accum is not None:
                kw["accum_out"] = accum
            S.act(lambda e: e.activation(out=o, in_=a, func=func, **kw), reads=[ra] + list(extra), writes=[ro])

        chunks_f = [(0, "c", 0), (128, "c", 1)] + [(256 + 128 * i, "x", i) for i in range(128)]
        chunks_b = [(128, "c", 1), (0, "c", 0)] + [(256 + 128 * i, "x", i) for i in reversed(range(128))]
        CD = cst

        it = 0
        for d, chunks in ((0, chunks_f), (1, chunks_b)):
            S.dve(lambda e: e.memset(state, 0.0), writes=["state"])
            S.dve(lambda e: e.memset(Sret, 0.0), writes=["Sret"])
            Mst = CD[:, C_MLT:C_MLT + 128] if d == 0 else CD[:, C_MGT:C_MGT + 128]
            MstT = CD[:, C_MGT:C_MGT + 128] if d == 0 else CD[:, C_MLT:C_MLT + 128]
            Min = CD[:, C_MLE:C_MLE + 128] if d == 0 else CD[:, C_MGE:C_MGE + 128]
            prevS = None
            for (t0, kind, ci) in chunks:
                par = it % 2
                it += 1
                S.begin(["P0", "H0", "H1", "PR", "S"])
                S.stage("P0")

                def g(nme):
                    return tl[nme][par]
                H, rH = g("H")
                R_, rR = g("R")
                u, ru = g("u")
                if kind == "x":
                    lo_ok = ci > 0
                    hi_ok = ci < 127
                else:
                    lo_ok = ci > 0
                    hi_ok = ci < 1
                if not (lo_ok and hi_ok):
                    S.pool(lambda e, H=H: e.memset(H, 0.0), writes=[rH])
                a0 = t0 - (64 if lo_ok else 0)
                a1 = t0 + 128 + (64 if hi_ok else 0)
                h0 = 64 - (t0 - a0)
                S.dma("sp", H[:, 0:6, h0:h0 + (a1 - a0)], pT[0:6, :, a0:a1].rearrange("b p t -> p b t"), writes=[rH])
                S.dma("sp", H[0:32, 6, h0:h0 + (a1 - a0)], pT[6, 0:32, a0:a1], writes=[rH])
                S.dma("act", R_, pT[7:13, :, t0:t0 + 128].rearrange("b p t -> p b t"), writes=[rR])
                def rub(b):
                    return (ru, b)
                steps = 5 if kind == "x" else 3
                for step in range(steps):
                    for b in range(7):
                        np_ = colw[b]
                        Hc = H[:np_, b, 64:192]
                        ub = u[:np_, b, :]
                        if step == 0:
                            cc_ = c0 if kind == "x" else c0c
                            S.dve(lambda e, ub=ub, Hc=Hc, b=b, np_=np_, cc_=cc_: e.tensor_scalar(ub, Hc, cc_[:np_, b:b + 1], None, ALU.mult),
                                  reads=[rH, "c0", "c0c"], writes=[rub(b)])
                            continue
                        if kind == "x":
                            u3 = ub.rearrange("p (r c) -> p r c", r=2)
                            H3 = Hc.rearrange("p (r c) -> p r c", r=2)
                            dst, src = [(u3[:, :, 1:64], H3[:, :, 0:63]), (u3[:, :, 0:63], H3[:, :, 1:64]),
                                        (ub, H[:np_, b, 0:128]), (ub, H[:np_, b, 128:256])][step - 1]
                        else:
                            dst, src = [(ub, H[:np_, b, 63:191]), (ub, H[:np_, b, 65:193])][step - 1]
                        mcol = step - 1
                        S.dve(lambda e, dst=dst, src=src, b=b, np_=np_, mcol=mcol: e.scalar_tensor_tensor(
                            out=dst, in0=src, scalar=mus[:np_, b, mcol:mcol + 1], in1=dst, op0=ALU.mult, op1=ALU.add),
                            reads=[rH, rub(b), "mus"], writes=[rub(b)])
                rT, kT, vT = u[:, 0, :], u[:, 1, :], u[:, 2, :]
                hs = slice(d * 64, d * 64 + 64)
                kkraw, r1 = g("kkraw"); sqt, r2 = g("sq"); rn, r3 = g("rn"); kk, r4 = g("kk")
                TS("dve", kkraw, r1, kT, rub(1), pv[:, 4:5], None, ALU.mult, extra=["pv"])
                ACT(sqt, r2, kkraw, r1, AF.Square)
                pq, rq = PQ()
                mm(pq, rq, CD[:, C_BONES:C_BONES + 128], ["cst"], sqt, [r2])
                ACT(rn, r3, pq, rq, AF.Sqrt)
                TS("dve", rn, r3, rn, r3, 1e-12, None, ALU.max)
                S.dve(lambda e, rn=rn: e.reciprocal(rn, rn), reads=[r3], writes=[r3])
                TT("dve", kk, r4, kkraw, r1, rn, r3, ALU.mult)
                tht, r5 = g("th"); sig, r6 = g("sig"); cs, r7 = g("cs"); E, r8 = g("E"); Ex, r9 = g("Ex")
                gin, r10 = g("gin"); gex, r11 = g("gex"); ginv, r12 = g("ginv")
                ACT(tht[hs, :], r5, u[hs, 3, :], rub(3), AF.Tanh)
                pq, rq = PQ()
                mm(pq, rq, w2t[hs, :], ["w2t"], tht[hs, :], [r5])
                ACT(sig, r6, pq, rq, AF.Tanh, bias=pvh[:, d:d + 1], scale=0.5, extra=["pvh"])
                TS("dve", sig, r6, sig, r6, 0.5, 0.5, ALU.mult, ALU.add)
                S.dve(lambda e, cs=cs, sig=sig: e.tensor_tensor_scan(cs, ones, sig, 0.0, ALU.mult, ALU.add),
                      reads=[r6, "cst"], writes=[r7])
                totp = tot[:, par:par + 1]
                gCp = gCr[:, par:par + 1]
                if d == 0:
                    Et, rE = cs, r7
                    TT("dve", Ex, r9, cs, r7, sig, r6, ALU.subtract)
                    Ext, rEx = Ex, r9
                else:
                    TS("dve", Ex, r9, cs, r7, -1.0, cs[:, 127:128], ALU.mult, ALU.add)
                    TT("dve", E, r8, Ex, r9, sig, r6, ALU.add)
                    Et, rE, Ext, rEx = E, r8, Ex, r9
                ACT(gin, r10, Et, rE, AF.Exp, scale=-KAPPA)
                ACT(gex, r11, Ext, rEx, AF.Exp, scale=-KAPPA)
                ACT(ginv, r12, Et, rE, AF.Exp, scale=KAPPA)
                ACT(gCp, ("gC", par), cs[:, 127:128], r7, AF.Exp, scale=-KAPPA)
                at_, r13 = g("a"); t1, r14 = g("t1"); kd, r15 = g("kd"); bb, r16 = g("b")
                pq, rq = PQ()
                mm(pq, rq, a2t[hs, :], ["a2t"], u[hs, 4, :], [rub(4)])
                ACT(at_, r13, pq, rq, AF.Tanh, bias=pvh[:, 2 + d:3 + d], scale=0.5, extra=["pvh"])
                TS("dve", at_, r13, at_, r13, 0.5, 0.5, ALU.mult, ALU.add)
                TS("dve", t1, r14, at_, r13, -1.0, pv[:, 5:6], ALU.add, ALU.mult, extra=["pv"])
                STT("dve", kd, r15, t1, r14, 1.0, kT, rub(1), ALU.add, ALU.mult)
                TT("dve", bb, r16, kk, r4, at_, r13, ALU.mult)
                bt, r17 = g("bt"); al, r18 = g("at"); kt, r19 = g("kt"); rt, r20 = g("rt")
                TT("dve", bt, r17, kk, r4, gex, r11, ALU.mult)
                STT("dve", al, r18, bb, r16, -1.0, ginv, r12, ALU.mult, ALU.mult)
                TT("dve", kt, r19, kd, r15, ginv, r12, ALU.mult)
                TT("dve", rt, r20, rT, rub(0), gin, r10, ALU.mult)
                vtok, r21 = g("vtok"); atok, r22 = g("atok"); ktok, r23 = g("ktok")
                pq, rq = PQ(); tr(pq, rq, vT, rub(2))
                S.act(lambda e, vtok=vtok, pq=pq: e.copy(vtok, pq), reads=[rq], writes=[r21])
                pq, rq = PQ(); tr(pq, rq, al, r18)
                S.act(lambda e, atok=atok, pq=pq: e.copy(atok[:, 0, 0:64], pq[:, 0:64]), reads=[rq], writes=[r22])
                S.dve(lambda e, atok=atok, pq=pq: e.tensor_copy(atok[:, 1, 64:128], pq[:, 64:128]), reads=[rq], writes=[r22])
                pq, rq = PQ(); tr(pq, rq, kt, r19)
                S.act(lambda e, ktok=ktok, pq=pq: e.copy(ktok[:, 0, 0:64], pq[:, 0:64]), reads=[rq], writes=[r23])
                S.dve(lambda e, ktok=ktok, pq=pq: e.tensor_copy(ktok[:, 1, 64:128], pq[:, 64:128]), reads=[rq], writes=[r23])
                zT, r24 = g("zT")
                STT("dve", zT, r24, rT, rub(0), pv[:, 6:7], kd, r15, ALU.mult, ALU.mult, extra=["pv"])
                pqb_, rqb_ = PQ()
                mm(pqb_[:, 0:2], rqb_, zT, [r24], CD[:, C_HSEL:C_HSEL + 2], ["cst"])
                pqb = bsv[:, 2 * par:2 * par + 2]
                rqb = ("bsv", par)
                S.act(lambda e, pqb=pqb, pqb_=pqb_: e.copy(pqb, pqb_[:, 0:2]), reads=[rqb_], writes=[rqb])
                yfb, r25 = g("yfwb")
                ytok, r26 = g("ytok")
                if d == 1:
                    S.dma("act", yfb[:, 0:258], yfw[t0:t0 + 128, 0:258], writes=[r25])
                    sgd, sgr = g("sgd"); gsil, gsr = g("gsil"); gtok, gtr = g("gtok")
                    ACT(sgd, sgr, u[:, 5, :], rub(5), AF.Tanh, scale=0.5)
                    TS("dve", sgd, sgr, sgd, sgr, 0.5, 0.5, ALU.mult, ALU.add)
                    ACT(gsil[0:32, :], gsr, u[0:32, 6, :], rub(6), AF.Tanh, scale=0.5)
                    TS("dve", gsil[0:32, :], gsr, gsil[0:32, :], gsr, 0.5, 0.5, ALU.mult, ALU.add)
                    pq, rq = PQ()
                    mm(pq, rq, sgd, [sgr], g2at, ["g2at"], start=True, stop=False)
                    mm(pq, rq, gsil[0:32, :], [gsr], g2bt[0:32, :], ["g2bt"], start=False, stop=True)
                    S.act(lambda e, gtok=gtok, pq=pq: e.copy(gtok, pq), reads=[rq], writes=[gtr])
                for h in range(2):
                    hp = slice(h * 64, h * 64 + 64)
                    S.stage("H%d" % h)

                    def gh(nme):
                        return th_[nme][h][par]
                    N_, n1 = gh("N"); A_, n2 = gh("A"); BK, n3 = gh("BK"); RA, n4 = gh("RA"); RK, n5 = gh("RK")
                    pl = pools["H%d" % h]
                    bA, rA = ps[pl[0]], ("pq", pl[0])
                    bB, rB = ps[pl[1]], ("pq", pl[1])
                    A0, A1 = bA[:, 0:128], bA[:, 128:256]
                    B0, B1 = bB[:, 0:128], bB[:, 128:256]
                    mm(A0, rA, al[hp, :], [r18], bt[hp, :], [r17])
                    mm(A1, rA, bt[hp, :], [r17], al[hp, :], [r18])
                    TT("dve", N_, n1, A0, rA, Mst, "cst", ALU.mult)
                    TT("dve", A_, n2, A1, rA, MstT, "cst", ALU.mult)
                    Pc, nP = gh("P"); Qc, nQ = gh("Q")
                    TT("pool", Pc, nP, N_, n1, ident, "cst", ALU.add)
                    TT("pool", Qc, nQ, A_, n2, ident, "cst", ALU.add)
                    Npc, nNp, Apc, nAp = N_, n1, A_, n2
                    alt = [(gh("Np"), gh("Ap"), gh("P2"), gh("Q2")), (gh("Np2"), gh("Ap2"), gh("P"), gh("Q"))]
                    for l in range(1, 7):
                        (Npn, nNpn), (Apn, nApn), (Pn, nPn), (Qn, nQn) = alt[(l - 1) % 2]
                        mm(A0, rA, Apc, [nAp], Npc, [nNp])
                        if l < 6:
                            mm(A1, rA, Npc, [nNp], Apc, [nAp])
                        S.act(lambda e, Npn=Npn, A0=A0: e.copy(Npn, A0), reads=[rA], writes=[nNpn])
                        if l < 6:
                            S.act(lambda e, Apn=Apn, A1=A1: e.copy(Apn, A1), reads=[rA], writes=[nApn])
                        mm(B0, rB, Qc, [nQ], Npn, [nNpn])
                        if l < 6:
                            mm(B1, rB, Pc, [nP], Apn, [nApn])
                        TT("dve", Pn, nPn, B0, rB, Pc, nP, ALU.add)
                        if l < 6:
                            TT("dve", Qn, nQn, B1, rB, Qc, nQ, ALU.add)
                        Npc, nNp, Apc, nAp = Npn, nNpn, Apn, nApn
                        Pc, nP = Pn, nPn
                        if l < 6:
                            Qc, nQ = Qn, nQn
                    mm(A0, rA, kt[hp, :], [r19], bt[hp, :], [r17])
                    mm(A1, rA, al[hp, :], [r18], rt[hp, :], [r20])
                    TT("dve", BK, n3, A0, rA, Mst, "cst", ALU.mult)
                    TT("dve", RA, n4, A1, rA, Min, "cst", ALU.mult)
                    mm(B0, rB, kt[hp, :], [r19], rt[hp, :], [r20])
                    TT("dve", RK, n5, B0, rB, Min, "cst", ALU.mult)
                    S.stage("S")
                    RHS, n6 = gh("RHS"); Us, n7 = gh("Us")
                    vh = vtok[:, hp]
                    pq, rq = PQ()
                    mm(pq[:, 0:64], rq, bt[hp, :], [r17], state[hp, 0:64], ["state"], start=True, stop=False)
                    mm(pq[:, 0:64], rq, BK, [n3], vh, [r21], start=False, stop=True)
                    S.act(lambda e, RHS=RHS, pq=pq: e.copy(RHS[:, 0:64], pq[:, 0:64]), reads=[rq], writes=[n6])
                    pq, rq = PQ()
                    mm(pq[:, 0:64], rq, Pc, [nP], RHS[:, 0:64], [n6])
                    S.act(lambda e, Us=Us, pq=pq: e.copy(Us[:, 0:64], pq[:, 0:64]), reads=[rq], writes=[n7])
                    pq, rq = PQ()
                    mm(pq[:, 0:64], rq, rt[hp, :], [r20], state[hp, 0:64], ["state"], start=True, stop=False)
                    mm(pq[:, 0:64], rq, RA, [n4], Us[:, 0:64], [n7], start=False, stop=False)
                    mm(pq[:, 0:64], rq, RK, [n5], vh, [r21], start=False, stop=True)
                    S.dve(lambda e, ytok=ytok, pq=pq, hp=hp: e.tensor_copy(ytok[:, hp], pq[:, 0:64]), reads=[rq], writes=[r26])
                    if h == 0:
                        pqs, rqs = ps[6][:, 0:128], ("pqs", 0)
                    mm(pqs[:, 0:64], rqs, atok[:, h, :], [r22], Us[:, 0:64], [n7], start=(h == 0), stop=False)
                    mm(pqs[:, 0:64], rqs, ktok[:, h, :], [r23], vh, [r21], start=False, stop=(h == 1))
                S.stage("S")
                TS("dve", state[:, 0:64], "state", state[:, 0:64], "state", gCp, None, ALU.mult, extra=[("gC", par)])
                STT("dve", state[:, 0:64], "state", pqs[:, 0:64], rqs, gCp, state[:, 0:64], "state", ALU.mult, ALU.add,
                    extra=[("gC", par)])

                S.stage("PR")
                qT, kT2, vT2, gT, qsw, ksw = (R_[:, i, :] for i in range(6))
                qr, q1 = g("qr"); kr, q2 = g("kr"); qx, q3 = g("qx"); vr, q4 = g("vr"); krz, q5 = g("krz"); PT, q6 = g("PT")
                if kind == "x":
                    cb, q7 = g("cosb"); sb, q8 = g("sinb")
                    xp = t0 - NCTX
                    S.dma("sp", cb, cosT[:, xp:xp + 128], writes=[q7])
                    S.dma("sp", sb, sinT[:, xp:xp + 128], writes=[q8])
                    TT("pool", qr, q1, qT, rR, cb, q7, ALU.mult)
                    TT("pool", qx, q3, qsw, rR, sb, q8, ALU.mult)
                    TT("pool", qr, q1, qr, q1, qx, q3, ALU.add)
                    TT("pool", kr, q2, kT2, rR, cb, q7, ALU.mult)
                    TT("pool", qx, q3, ksw, rR, sb, q8, ALU.mult)
                    TT("pool", kr, q2, kr, q2, qx, q3, ALU.add)
                    qrr, krr = qr, kr
                    rqr, rkr = q1, q2
                else:
                    qrr, krr, rqr, rkr = qT, kT2, rR, rR
                TT("dve", qx, q3, qrr, rqr, xibc[d], "xibc", ALU.mult)
                pq, rq = PQ(); tr(pq, rq, vT2, rR)
                S.act(lambda e, vr=vr, pq=pq: e.copy(vr, pq), reads=[rq], writes=[q4])
                pq, rq = PQ(); tr(pq, rq, krr, rkr)
                TS("dve", krz, q5, pq, rq, zeta[:, d:d + 1], None, ALU.mult, extra=["zeta"])
                pq, rq = PQ(); mm(pq, rq, krr, [rkr], qrr, [rqr])
                TT("dve", PT, q6, pq, rq, dmT[d], "dmT", ALU.mult)
                if d == 1:
                    gsl, gslr = g("gsl")
                    pq, rq = PQ(); tr(pq, rq, gT, rR)
                    S.act(lambda e, gsl=gsl, pq=pq: e.activation(out=gsl, in_=pq, func=AF.Tanh, scale=0.5), reads=[rq], writes=[gslr])
                    STT("dve", gsl, gslr, gsl, gslr, 1.0, pq, rq, ALU.add, ALU.mult)
                    TS("dve", gsl, gslr, gsl, gslr, 0.5, None, ALU.mult)
                S.stage("S")
                yret, q9 = g("yret")
                pq, rq = PQ()
                mm(pq, rq, PT, [q6], vr, [q4], start=True, stop=False)
                mm(pq, rq, qx, [q3], Sret, ["Sret"], start=False, stop=True)
                S.act(lambda e, yret=yret, pq=pq: e.copy(yret, pq), reads=[rq], writes=[q9])
                pq, rq = PQ()
                mm(pq, rq, krz, [q5], vr, [q4])
                STT("dve", Sret, "Sret", Sret, "Sret", gC[:, d:d + 1], pq, rq, ALU.mult, ALU.add, extra=["gC"])

                if d == 0:
                    S.dve(lambda e, yfb=yfb, ytok=ytok: e.tensor_copy(yfb[:, 0:128], ytok), reads=[r26], writes=[r25])
                    S.dve(lambda e, yfb=yfb, yret=yret: e.tensor_copy(yfb[:, 128:256], yret), reads=[q9], writes=[r25])
                    S.dve(lambda e, yfb=yfb, pqb=pqb: e.tensor_copy(yfb[:, 256:258], pqb[:, 0:2]), reads=[rqb], writes=[r25])
                    S.dma("sp", yfw[t0:t0 + 128, 0:258], yfb[:, 0:258], reads=[r25], writes=["yfw"])
                else:
                    o1, o1r = g("o1"); o2, o2r = g("o2")
                    gtot, gtotr = g("gtot")
                    TT("dve", ytok, r26, ytok, r26, yfb[:, 0:128], r25, ALU.add)
                    y3 = ytok.rearrange("p (h v) -> p h v", h=2)
                    smr = ("sm", par)
                    so = par * 32
                    mean = sm[:, so:so + 2]; var = sm[:, so + 2:so + 4]; bsum = sm[:, so + 4:so + 6]
                    S.dve(lambda e, mean=mean, y3=y3: e.tensor_reduce(out=mean, in_=y3, axis=AX.X, op=ALU.add), reads=[r26], writes=[smr])
                    S.dve(lambda e, mean=mean: e.tensor_scalar(mean, mean, 1.0 / 64, None, ALU.mult), reads=[smr], writes=[smr])
                    o13 = o1.rearrange("p (h v) -> p h v", h=2)
                    o23 = o2.rearrange("p (h v) -> p h v", h=2)
                    S.dve(lambda e, o13=o13, y3=y3, mean=mean: e.tensor_tensor(o13, y3, mean.unsqueeze(2).broadcast_to([128, 2, 64]), ALU.subtract),
                          reads=[r26, smr], writes=[o1r])
                    TT("dve", o2, o2r, o1, o1r, o1, o1r, ALU.mult)
                    S.dve(lambda e, var=var, o23=o23: e.tensor_reduce(out=var, in_=o23, axis=AX.X, op=ALU.add), reads=[o2r], writes=[smr])
                    S.act(lambda e, var=var: e.activation(out=var, in_=var, func=AF.Sqrt, scale=1.0 / 64, bias=epsv[:, 1:2]), reads=[smr, "epsv"], writes=[smr])
                    S.dve(lambda e, var=var: e.reciprocal(var, var), reads=[smr], writes=[smr])
                    S.dve(lambda e, o13=o13, var=var: e.tensor_tensor(o13, o13, var.unsqueeze(2).broadcast_to([128, 2, 64]), ALU.mult),
                          reads=[o1r, smr], writes=[o1r])
                    TT("dve", o1, o1r, o1, o1r, lnw, "rowp", ALU.mult)
                    TT("dve", o1, o1r, o1, o1r, lnb, "rowp", ALU.add)
                    S.dve(lambda e, bsum=bsum, pqb=pqb, yfb=yfb: e.tensor_tensor(bsum, pqb[:, 0:2], yfb[:, 256:258], ALU.add),
                          reads=[rqb, r25], writes=[smr])
                    v3 = vtok.rearrange("p (h v) -> p h v", h=2)
                    S.dve(lambda e, o23=o23, v3=v3, bsum=bsum: e.tensor_tensor(o23, v3, bsum.unsqueeze(2).broadcast_to([128, 2, 64]), ALU.mult),
                          reads=[r21, smr], writes=[o2r])
                    TT("dve", o1, o1r, o1, o1r, o2, o2r, ALU.add)
                    TT("dve", gtot, gtotr, o1, o1r, gtok, gtr, ALU.mult)
                    S.dma("sp", out[t0:t0 + 128, 0:128], gtot, reads=[gtotr], writes=["out"])
                    TT("dve", yret, q9, yret, q9, yfb[:, 128:256], r25, ALU.add)
                    ss = sm[:, so + 8:so + 9]
                    S.dve(lambda e, ss=ss: e.memset(ss, 0.0), writes=[smr])
                    ACT(o2, o2r, yret, q9, AF.Square, accum=ss, extra=[smr])
                    S.act(lambda e, ss=ss: e.activation(out=ss, in_=ss, func=AF.Sqrt, scale=1.0 / 128, bias=epsv[:, 0:1]), reads=[smr, o2r, "epsv"], writes=[smr])
                    S.dve(lambda e, ss=ss: e.reciprocal(ss, ss), reads=[smr], writes=[smr])
                    STT("dve", o2, o2r, yret, q9, ss, gnw, "rowp", ALU.mult, ALU.mult, extra=[smr])
                    TT("dve", o2, o2r, o2, o2r, gsl, gslr, ALU.mult)
                    S.dma("sp", out[t0:t0 + 128, 128:256], o2, reads=[o2r], writes=["out"])
                rec = S.end()
                if prevS is None:
                    S.replay(rec["P0"])
                else:
                    S.replay(zipper([rec["P0"], prevS]))
                S.replay(zipper([rec["H0"], rec["H1"], rec["PR"]]))
                prevS = rec["S"]
            S.replay(prevS)
            S.barrier(dummy[:])
        S.emit()
    return nc


D = 2048
NTB = 2080
NE = 32
TILES_B = [(0, 32)] + [(32 + 128 * i, 128) for i in range(16)]
EPS = 1e-6


def build_B(n_exp=NE):
    nc = bass.Bass("TRN2", target_bir_lowering=False)

    def din(name, shape):
        return nc.dram_tensor(name, shape, F32, kind="ExternalInput").ap()

    ymT = din("ymT", [D, NTB])
    xres = din("xres", [NTB, D])
    w_out = din("w_out", [D, D])
    w1 = din("w1", [NE, D, 2048])
    w2 = din("w2", [NE, 1024, D])
    rw = din("rw", [D, 32])
    rb = din("rb", [32])
    vecs = din("vecs", [10, D])
    ident_d = din("ident", [128, 128])
    out = nc.dram_tensor("out", [NTB, D], F32, kind="ExternalOutput").ap()
    outn = nc.dram_tensor("outn", [NTB, D], F32, kind="ExternalOutput").ap()
    xn = nc.dram_tensor("xn", [NTB, D], F32).ap()
    h2Td = nc.dram_tensor("h2Td", [D, NTB], F32).ap()

    with contextlib.ExitStack() as st:
        S = Sched(nc, st)
        UR = st.enter_context(nc.sbuf_tensor("UR", [128, 29440], F32R))
        UF = st.enter_context(nc.sbuf_tensor("UF", [128, 21024], F32))
        gates = st.enter_context(nc.sbuf_tensor("gates", [128, 17, 32], F32))
        small = st.enter_context(nc.sbuf_tensor("small", [128, 512], F32))
        ident = st.enter_context(nc.sbuf_tensor("identt", [128, 128], F32))
        dummy = st.enter_context(nc.sbuf_tensor("dummyb", [128, 8], F32))
        ps = [st.enter_context(nc.psum_tensor("ps%d" % i, [128, 512], F32)) for i in range(8)]

        off = [0]
        offr = [0]

        def carve(n):
            a = UF[:, off[0]:off[0] + n]
            off[0] += n
            assert off[0] <= 21024, off[0]
            return a

        def carver(n):
            a = UR[:, offr[0]:offr[0] + n]
            offr[0] += n
            assert offr[0] <= 29440, offr[0]
            return a

        S.dma("sp", ident[:], ident_d[:, :], writes=["ident"])
        epsb = small[:, 500:501]
        S.dve(lambda e: e.memset(small[:, 500:501], EPS), writes=["epsb"])

        wo = [carver(16 * 512).rearrange("p (c n) -> p c n", c=16) for _ in range(2)]
        ym = [carver(16 * 128).rearrange("p (c t) -> p c t", c=16) for _ in range(2)]
        xg = [carve(512) for _ in range(2)]
        ob = [carve(512) for _ in range(2)]
        bc = {k: carve(2048) for k in ("gt1x", "gt1c", "gscx", "gscc", "sh2x", "sh2c")}
        xt1 = carve(2048)
        xt = [xt1, xt1]
        h2 = carve(2048)
        tmpv = h2
        h2T1 = carve(16 * 128).rearrange("p (c t) -> p c t", c=16)
        h2T = [h2T1, h2T1]
        rws = carve(16 * 32).rearrange("p (c e) -> p c e", c=16)
        rbb = carve(32)

        vidx = {"gt1x": 0, "gt1c": 1, "sh2x": 2, "sh2c": 3, "sc2x": 4, "sc2c": 5, "gt2x": 6, "gt2c": 7, "n2g": 8, "fg": 9}
        for k in ("gt1x", "gt1c", "sh2x", "sh2c"):
            S.dma("sp", bc[k], vecs[vidx[k]].partition_broadcast(128), writes=["bc_" + k])
        S.dma("sp", tmpv, vecs[8].partition_broadcast(128), writes=["h2"])
        for k, sck in (("gscx", "sc2x"), ("gscc", "sc2c")):
            S.dma("sp", bc[k], vecs[vidx[sck]].partition_broadcast(128), writes=["bc_" + k])
            S.dve(lambda e, k=k: e.scalar_tensor_tensor(out=bc[k], in0=bc[k], scalar=1.0, in1=tmpv,
                                                        op0=ALU.add, op1=ALU.mult),
                  reads=["bc_" + k, "h2"], writes=["bc_" + k])
        S.dma("sp", rws, rw.rearrange("(c p) e -> p c e", p=128), writes=["rws"])
        S.dma("sp", rbb, rb.partition_broadcast(128), writes=["rbb"])

        k = 0
        for g in range(4):
            gs = slice(g * 512, (g + 1) * 512)
            wob = wo[g % 2]
            S.dma("pool", wob, w_out[:, gs].rearrange("(c p) n -> p c n", p=128), writes=[("wo", g % 2)])
            for ti, (r0, nt) in enumerate(TILES_B):
                kb = k % 2
                ymb, xgb, obb, psb = ym[kb], xg[kb], ob[kb], ps[kb]
                S.dma("pool", ymb[:, :, :nt], ymT[:, r0:r0 + nt].rearrange("(c p) t -> p c t", p=128),
                      writes=[("ym", kb)])
                S.dma("sp", xgb[:nt], xres[r0:r0 + nt, gs], writes=[("xg", kb)])
                for c in range(16):
                    S.pe(lambda e, c=c, ymb=ymb, wob=wob, psb=psb, nt=nt: e.matmul(
                        psb[:nt, :], ymb[:, c, :nt], wob[:, c, :], start=(c == 0), stop=(c == 15)),
                        reads=[("ym", kb), ("wo", g % 2)], writes=[("ps", kb)])
                gtb = bc["gt1c"] if ti == 0 else bc["gt1x"]
                S.dve(lambda e, obb=obb, psb=psb, gtb=gtb, nt=nt, gs=gs: e.tensor_tensor(
                    obb[:nt], psb[:nt, :], gtb[:nt, gs], ALU.mult),
                    reads=[("ps", kb), "bc_gt1c", "bc_gt1x"], writes=[("ob", kb)])
                S.dve(lambda e, obb=obb, xgb=xgb, nt=nt: e.tensor_tensor(obb[:nt], obb[:nt], xgb[:nt], ALU.add),
                      reads=[("ob", kb), ("xg", kb)], writes=[("ob", kb)])
                S.dma("act", xn[r0:r0 + nt, gs], obb[:nt], reads=[("ob", kb)], writes=[("xn", ti)])
                k += 1

        for ti, (r0, nt) in enumerate(TILES_B):
            kb = 0
            xtb, h2Tb = xt[kb], h2T[kb]
            sfx = "c" if ti == 0 else "x"
            S.dma("sp", xtb[:nt], xn[r0:r0 + nt, :], reads=[("xn", ti)], writes=[("xt", kb)])
            ss = small[:, 0:1]
            rstd = small[:, 1:2]
            S.dve(lambda e: e.memset(small[:, 0:2], 0.0), writes=["ss", "rstd"])
            S.act(lambda e, xtb=xtb, nt=nt: e.activation(out=h2[:nt], in_=xtb[:nt], func=AF.Square,
                                                         accum_out=small[:nt, 0:1]),
                  reads=[("xt", kb), "ss"], writes=["h2", "ss"])
            S.act(lambda e, nt=nt: e.activation(out=small[:nt, 1:2], in_=small[:nt, 0:1], func=AF.Sqrt, scale=1.0 / D, bias=epsb[:nt, 0:1]),
                  reads=["ss", "epsb"], writes=["rstd"])
            S.dve(lambda e, nt=nt: e.reciprocal(small[:nt, 1:2], small[:nt, 1:2]),
                  reads=["rstd"], writes=["rstd"])
            S.dve(lambda e, xtb=xtb, nt=nt, sfx=sfx: e.scalar_tensor_tensor(
                out=h2[:nt], in0=xtb[:nt], scalar=small[:nt, 1:2], in1=bc["gsc" + sfx][:nt],
                op0=ALU.mult, op1=ALU.mult),
                reads=[("xt", kb), "rstd", "bc_gscx", "bc_gscc"], writes=["h2"])
            S.dve(lambda e, nt=nt, sfx=sfx: e.tensor_tensor(h2[:nt], h2[:nt], bc["sh2" + sfx][:nt], ALU.add),
                  reads=["h2", "bc_sh2x", "bc_sh2c"], writes=["h2"])
            for c in range(16):
                q = 2 + c // 4
                S.pe(lambda e, c=c, q=q, nt=nt: e.transpose(
                    ps[q][:, (c % 4) * 128:(c % 4) * 128 + nt], h2[:nt, c * 128:(c + 1) * 128], ident[:nt, :nt]),
                    reads=["h2", "ident"], writes=[("ps", q)])
            for q in range(4):
                src = ps[2 + q][:, :].rearrange("p (c t) -> p c t", c=4)[:, :, :nt]
                dst = h2Tb[:, 4 * q:4 * q + 4, :nt]
                if q % 2 == 0:
                    S.act(lambda e, src=src, dst=dst: e.copy(dst, src), reads=[("ps", 2 + q)], writes=[("h2T", kb)])
                else:
                    S.dve(lambda e, src=src, dst=dst: e.tensor_copy(dst, src), reads=[("ps", 2 + q)],
                          writes=[("h2T", kb)])
            for c in range(16):
                S.pe(lambda e, c=c, h2Tb=h2Tb, nt=nt: e.matmul(
                    ps[6][:nt, 0:32], h2Tb[:, c, :nt], rws[:, c, :], start=(c == 0), stop=(c == 15)),
                    reads=[("h2T", kb), "rws"], writes=[("ps", 6)])
            S.dma("act", h2Td[:, r0:r0 + nt].rearrange("(c p) t -> p c t", p=128), h2Tb[:, :, :nt],
                  reads=[("h2T", kb)], writes=[("h2Td", ti)])
            sc = small[:, 32:64]
            bi = small[:, 64:96]
            eq = small[:, 96:128]
            mk = small[:, 128:160]
            sel = small[:, 160:192]
            ww = small[:, 192:224]
            m1 = small[:, 224:228]
            m2 = small[:, 228:232]
            gsum = small[:, 232:236]
            goh = small[:, 236:240]
            gm = small[:, 240:241]
            wsum = small[:, 241:242]

            def v3(a, nt):
                return a[:nt].rearrange("p (g e) -> p g e", g=4)

            def b3(a, nt):
                return a[:nt].unsqueeze(2).broadcast_to([nt, 4, 8])

            R = ["rt"]
            S.act(lambda e, nt=nt: e.activation(out=sc[:nt], in_=ps[6][:nt, 0:32], func=AF.Sigmoid),
                  reads=[("ps", 6)], writes=R)
            S.dve(lambda e, nt=nt: e.tensor_tensor(bi[:nt], sc[:nt], rbb[:nt], ALU.add), reads=R + ["rbb"], writes=R)
            S.dve(lambda e, nt=nt: e.tensor_reduce(out=m1[:nt], in_=v3(bi, nt), axis=AX.X, op=ALU.max), reads=R, writes=R)
            S.dve(lambda e, nt=nt: e.tensor_tensor(v3(eq, nt), v3(bi, nt), b3(m1, nt), ALU.is_equal), reads=R, writes=R)
            S.dve(lambda e, nt=nt: e.scalar_tensor_tensor(out=mk[:nt], in0=eq[:nt], scalar=-1e9, in1=bi[:nt],
                                                          op0=ALU.mult, op1=ALU.add), reads=R, writes=R)
            S.dve(lambda e, nt=nt: e.tensor_reduce(out=m2[:nt], in_=v3(mk, nt), axis=AX.X, op=ALU.max), reads=R, writes=R)
            S.dve(lambda e, nt=nt: e.tensor_tensor(gsum[:nt], m1[:nt], m2[:nt], ALU.add), reads=R, writes=R)
            S.dve(lambda e, nt=nt: e.tensor_reduce(out=gm[:nt], in_=gsum[:nt], axis=AX.X, op=ALU.max), reads=R, writes=R)
            S.dve(lambda e, nt=nt: e.tensor_scalar(goh[:nt], gsum[:nt], gm[:nt, 0:1], None, ALU.is_equal), reads=R, writes=R)
            S.dve(lambda e, nt=nt: e.tensor_tensor(v3(sel, nt), v3(bi, nt), b3(m2, nt), ALU.is_ge), reads=R, writes=R)
            S.dve(lambda e, nt=nt: e.tensor_tensor(v3(sel, nt), v3(sel, nt), b3(goh, nt), ALU.mult), reads=R, writes=R)
            S.dve(lambda e, nt=nt: e.tensor_tensor(ww[:nt], sc[:nt], sel[:nt], ALU.mult), reads=R, writes=R)
            S.dve(lambda e, nt=nt: e.tensor_reduce(out=wsum[:nt], in_=ww[:nt], axis=AX.X, op=ALU.add), reads=R, writes=R)
            S.dve(lambda e, nt=nt: e.reciprocal(wsum[:nt], wsum[:nt]), reads=R, writes=R)
            S.dve(lambda e, nt=nt, ti=ti: e.tensor_scalar(gates[:nt, ti, :], ww[:nt], wsum[:nt, 0:1], None, ALU.mult),
                  reads=R, writes=["gates"])

        S.barrier(dummy[:])

        off[0] = 0
        offr[0] = 0
        h2Tp = carver(16 * 544).rearrange("p (c t) -> p c t", c=16)
        acc = carve(5 * 2048).rearrange("p (j n) -> p j n", j=5)
        w1g = [carver(16 * 128).rearrange("p (c f) -> p c f", c=16) for _ in range(2)]
        w1u = [carver(16 * 128).rearrange("p (c f) -> p c f", c=16) for _ in range(2)]
        w2g = [carver(8 * 512).rearrange("p (f n) -> p f n", f=8) for _ in range(2)]
        actT = carver(8 * 544).rearrange("p (f t) -> p f t", f=8)
        xt2 = carve(2048)
        bc2 = {k: carve(2048) for k in ("gt2x", "gt2c", "fg")}
        S.dma("sp", bc2["gt2x"], vecs[6].partition_broadcast(128), writes=["bc_gt2x"])
        S.dma("sp", bc2["gt2c"], vecs[7].partition_broadcast(128), writes=["bc_gt2c"])
        S.dma("sp", bc2["fg"], vecs[9].partition_broadcast(128), writes=["bc_fg"])

        pi = 0
        w2c = 0
        yc = 0
        for p in range(4):
            ptiles = list(range(0, 5)) if p == 0 else list(range(4 * p + 1, 4 * p + 5))
            t0 = TILES_B[ptiles[0]][0]
            T = sum(TILES_B[t][1] for t in ptiles)
            chunks = [(0, 512), (512, 32)] if p == 0 else [(0, 512)]
            S.dma("pool", h2Tp[:, :, :T], h2Td[:, t0:t0 + T].rearrange("(c p) t -> p c t", p=128),
                  reads=[("h2Td", t) for t in ptiles], writes=["h2Tp"])
            S.pool(lambda e: e.memset(acc, 0.0), writes=[("acc", j) for j in range(5)])
            for ex in range(n_exp):
                w2bufs = {}
                for fb in range(8):
                    b = pi % 2
                    w1gb, w1ub = w1g[b], w1u[b]
                    S.dma("pool", w1gb, w1[ex, :, fb * 128:(fb + 1) * 128].rearrange("(c p) f -> p c f", p=128),
                          writes=[("w1g", b)])
                    S.dma("pool", w1ub, w1[ex, :, 1024 + fb * 128:1024 + (fb + 1) * 128].rearrange("(c p) f -> p c f", p=128),
                          writes=[("w1u", b)])
                    for ci, (c0, cn) in enumerate(chunks):
                        if ci == 0:
                            psg, psu = ps[(pi % 2) * 2][:, 0:cn], ps[(pi % 2) * 2 + 1][:, 0:cn]
                            rg, ru = ("ps", (pi % 2) * 2), ("ps", (pi % 2) * 2 + 1)
                        else:
                            o = (pi % 2) * 64
                            psg, psu = ps[4][:, o:o + 32], ps[4][:, o + 32:o + 64]
                            rg, ru = ("ps", 4), ("ps", 4)
                        for c in range(16):
                            S.pe(lambda e, c=c, psg=psg, w1gb=w1gb, c0=c0, cn=cn: e.matmul(
                                psg, w1gb[:, c, :], h2Tp[:, c, c0:c0 + cn], start=(c == 0), stop=(c == 15)),
                                reads=[("w1g", b), "h2Tp"], writes=[rg])
                        for c in range(16):
                            S.pe(lambda e, c=c, psu=psu, w1ub=w1ub, c0=c0, cn=cn: e.matmul(
                                psu, w1ub[:, c, :], h2Tp[:, c, c0:c0 + cn], start=(c == 0), stop=(c == 15)),
                                reads=[("w1u", b), "h2Tp"], writes=[ru])
                        dst = actT[:, fb, c0:c0 + cn]
                        S.act(lambda e, dst=dst, psg=psg: e.activation(out=dst, in_=psg, func=AF.Silu),
                              reads=[rg], writes=[("actT", fb, ci)])
                        S.dve(lambda e, dst=dst, psu=psu: e.tensor_tensor(dst, dst, psu, ALU.mult),
                              reads=[ru, ("actT", fb, ci)], writes=[("actT", fb, ci)])
                    pi += 1
                for grp in range(4):
                    wb = w2c % 2
                    w2c += 1
                    w2b = w2g[wb]
                    S.dma("pool", w2b, w2[ex, :, grp * 512:(grp + 1) * 512].rearrange("(f p) n -> p f n", p=128),
                          writes=[("w2g", wb)])
                    tt0 = 0
                    for j, t in enumerate(ptiles):
                        nt = TILES_B[t][1]
                        psy = ps[5 + yc % 2]
                        ry = ("ps", 5 + yc % 2)
                        yc += 1
                        for fb in range(8):
                            S.pe(lambda e, fb=fb, psy=psy, nt=nt, tt0=tt0, w2b=w2b: e.matmul(
                                psy[:nt, :], actT[:, fb, tt0:tt0 + nt], w2b[:, fb, :], start=(fb == 0), stop=(fb == 7)),
                                reads=[("actT", fb, 0), ("actT", fb, 1), ("w2g", wb)], writes=[ry])
                        gsl = slice(grp * 512, (grp + 1) * 512)
                        S.dve(lambda e, psy=psy, nt=nt, j=j, t=t, ex=ex, gsl=gsl: e.scalar_tensor_tensor(
                            out=acc[:nt, j, gsl], in0=psy[:nt, :], scalar=gates[:nt, t, ex:ex + 1],
                            in1=acc[:nt, j, gsl], op0=ALU.mult, op1=ALU.add),
                            reads=[ry, "gates", ("acc", j)], writes=[("acc", j)])
                        tt0 += nt
            for j, t in enumerate(ptiles):
                r0, nt = TILES_B[t]
                sfx = "c" if t == 0 else "x"
                S.dma("sp", xt2[:nt], xn[r0:r0 + nt, :], reads=[("xn", t)], writes=["xt2"])
                S.dve(lambda e, nt=nt, j=j, sfx=sfx: e.tensor_tensor(acc[:nt, j, :], acc[:nt, j, :], bc2["gt2" + sfx][:nt], ALU.mult),
                      reads=[("acc", j), "bc_gt2x", "bc_gt2c"], writes=[("acc", j)])
                S.dve(lambda e, nt=nt, j=j: e.tensor_tensor(acc[:nt, j, :], acc[:nt, j, :], xt2[:nt], ALU.add),
                      reads=[("acc", j), "xt2"], writes=[("acc", j)])
                S.dma("sp", out[r0:r0 + nt, :], acc[:nt, j, :], reads=[("acc", j)], writes=[("out", t)])
                S.dve(lambda e: e.memset(small[:, 0:2], 0.0), writes=["ss", "rstd"])
                S.act(lambda e, nt=nt, j=j: e.activation(out=xt2[:nt], in_=acc[:nt, j, :], func=AF.Square,
                                                         accum_out=small[:nt, 0:1]),
                      reads=[("acc", j), "ss"], writes=["xt2", "ss"])
                S.act(lambda e, nt=nt: e.activation(out=small[:nt, 1:2], in_=small[:nt, 0:1], func=AF.Sqrt, scale=1.0 / D, bias=epsb[:nt, 0:1]),
                      reads=["ss", "epsb"], writes=["rstd"])
                S.dve(lambda e, nt=nt: e.reciprocal(small[:nt, 1:2], small[:nt, 1:2]),
                      reads=["rstd"], writes=["rstd"])
                S.dve(lambda e, nt=nt, j=j: e.scalar_tensor_tensor(
                    out=xt2[:nt], in0=acc[:nt, j, :], scalar=small[:nt, 1:2], in1=bc2["fg"][:nt],
                    op0=ALU.mult, op1=ALU.mult),
                    reads=[("acc", j), "rstd", "bc_fg"], writes=["xt2"])
                S.dma("sp", outn[r0:r0 + nt, :], xt2[:nt], reads=["xt2"], writes=[("outn", t)])
        S.emit()
    return nc


def build_M():
    nc = bass.Bass("TRN2", target_bir_lowering=False)
    aw = nc.dram_tensor("aw", [2, 2048, 1536], F32, kind="ExternalInput").ap()
    ab = nc.dram_tensor("ab", [2, 1536], F32, kind="ExternalInput").ap()
    cc = nc.dram_tensor("cc", [128, 16, 2], F32, kind="ExternalInput").ap()
    mo = nc.dram_tensor("mo", [2, 2, 1536], F32, kind="ExternalOutput").ap()
    with contextlib.ExitStack() as st:
        S = Sched(nc, st)
        cct = st.enter_context(nc.sbuf_tensor("cct", [128, 16, 2], F32))
        sct = st.enter_context(nc.sbuf_tensor("sct", [128, 16, 2], F32))
        awt = [st.enter_context(nc.sbuf_tensor("awt%d" % i, [128, 16, 512], F32)) for i in range(2)]
        bt = [st.enter_context(nc.sbuf_tensor("bt%d" % i, [2, 512], F32)) for i in range(2)]
        ot = [st.enter_context(nc.sbuf_tensor("ot%d" % i, [2, 512], F32)) for i in range(2)]
        ps = [st.enter_context(nc.psum_tensor("psm%d" % i, [128, 512], F32)) for i in range(2)]
        S.dma("sp", cct[:], cc[:, :, :], writes=["cct"])
        S.act(lambda e: e.activation(out=sct[:], in_=cct[:], func=AF.Silu), reads=["cct"], writes=["sct"])
        k = 0
        for l in range(2):
            for g in range(3):
                b = k % 2
                k += 1
                gs = slice(g * 512, (g + 1) * 512)
                S.dma("sp" if b == 0 else "act", awt[b][:], aw[l, :, gs].rearrange("(c p) n -> p c n", p=128), writes=[("awt", b)])
                S.dma("sp", bt[b][:], ab[l, gs].partition_broadcast(2), writes=[("bt", b)])
                for c in range(16):
                    S.pe(lambda e, c=c, b=b: e.matmul(ps[b][0:2, :], sct[:, c, :], awt[b][:, c, :], start=(c == 0), stop=(c == 15)),
                         reads=["sct", ("awt", b)], writes=[("ps", b)])
                S.dve(lambda e, b=b: e.tensor_tensor(ot[b][:], ps[b][0:2, :], bt[b][:], ALU.add),
                      reads=[("ps", b), ("bt", b)], writes=[("ot", b)])
                S.dma("sp", mo[l, :, gs], ot[b][:], reads=[("ot", b)], writes=["mo"])
        S.emit()
    return nc

import numpy as np

RWKV_IN = 3488


def rope_tables():
    half = 64
    inv = (10000.0 ** (-np.arange(0, half, 2, dtype=np.float32) / half)).astype(np.float32)
    t = np.arange(16384)
    posr = (t // 64).astype(np.float32)
    posc = (t % 64).astype(np.float32)
    cosT = np.zeros((128, 16384), np.float32)
    sinT = np.zeros((128, 16384), np.float32)
    for hb, pos in ((0, posr), (64, posc)):
        ang = (pos[None, :] * inv[:, None]).astype(np.float32)
        c, s = np.cos(ang).astype(np.float32), np.sin(ang).astype(np.float32)
        cosT[hb:hb + 32] = c
        cosT[hb + 32:hb + 64] = c
        sinT[hb:hb + 32] = -s
        sinT[hb + 32:hb + 64] = s
    return cosT, sinT


SWAP = np.concatenate([np.arange(32, 64), np.arange(0, 32), np.arange(96, 128), np.arange(64, 96)])


def col_blocks(c):
    oc = np.arange(c * 128, c * 128 + 128)
    blocks = [oc, 1024 + oc, 2048 + oc, np.arange(3072, 3200), np.arange(3200, 3328), np.arange(3328, 3456),
              np.arange(3456, 3488), RWKV_IN + oc, RWKV_IN + 1024 + oc, RWKV_IN + 2048 + oc, RWKV_IN + 3072 + oc,
              RWKV_IN + oc[SWAP], RWKV_IN + 1024 + oc[SWAP]]
    return blocks


def fp(v):
    return np.ascontiguousarray(v.reshape(16, 128).T)


def prep_A(inp, l, c, xT, mod_x, mod_c, tables, cst):
    blocks = col_blocks(c)
    cols = np.concatenate(blocks)
    sh1, sc1 = mod_x[0:2048], mod_x[2048:4096]
    csh1, csc1 = mod_c[0:2048], mod_c[2048:4096]
    mvec = np.stack([fp(sh1), fp(sc1), fp(csh1), fp(csc1), fp(inp["norm1_g"][l])], axis=1).astype(np.float32)
    mu = np.zeros((128, 7, 4), np.float32)
    for b in range(7):
        cb = blocks[b]
        mu[:len(cb), b, :] = inp["shift_mu"][l][:, cb].T
    oc = np.arange(c * 128, c * 128 + 128)
    pvec = np.zeros((128, 8), np.float32)
    pvec[:, 0] = inp["rwkv_w0"][l, 0, oc]
    pvec[:, 1] = inp["rwkv_w0"][l, 1, oc]
    pvec[:, 2] = inp["rwkv_a0"][l, 0, oc]
    pvec[:, 3] = inp["rwkv_a0"][l, 1, oc]
    pvec[:, 4] = inp["rwkv_k_k"][l, oc]
    pvec[:, 5] = inp["rwkv_k_a"][l, oc]
    pvec[:, 6] = inp["rwkv_r_k"][l].reshape(-1)[oc]
    w2s = np.concatenate([inp["rwkv_w2"][l, 0][:, oc], inp["rwkv_w2"][l, 1][:, oc]], 0)
    a2s = np.concatenate([inp["rwkv_a2"][l, 0][:, oc], inp["rwkv_a2"][l, 1][:, oc]], 0)
    g2 = inp["rwkv_g2"][l]
    rowv = np.stack([inp["rwkv_lnx_w"][l, oc], inp["rwkv_lnx_b"][l, oc], inp["ret_gn_w"][l, oc]])
    return {
        "xT": xT, "W": np.ascontiguousarray(inp["w_in"][l][:, cols]), "mvec": mvec, "mu": mu, "pvec": pvec,
        "w2s": np.ascontiguousarray(w2s), "a2s": np.ascontiguousarray(a2s),
        "g2a": np.ascontiguousarray(g2[0:128, oc]), "g2b": np.ascontiguousarray(g2[128:160, oc]),
        "rowv": np.ascontiguousarray(rowv.astype(np.float32)),
        "dec": np.ascontiguousarray(inp["ret_log2_decay"][l, :, c]),
        "cosT": tables[0], "sinT": tables[1], "cst": cst,
    }

def kernel(**inp):
    inp = {k: np.asarray(v) for k, v in inp.items()}
    x = inp["x"][0]
    ctx = inp["ctx"][0]
    cores = list(range(8))
    ncM = build_M()
    cc = np.ascontiguousarray(np.stack([fp(inp["c"][0]), fp(inp["c_ctx"])], axis=2).astype(np.float32))
    ims = []
    for c in cores:
        cs = slice(c * 1536, (c + 1) * 1536)
        ims.append({"aw": np.ascontiguousarray(inp["ada_w"][:, :, cs]),
                    "ab": np.ascontiguousarray(inp["ada_b"][:, cs]), "cc": cc})
    res = run_bass_kernel_spmd(ncM, ims, core_ids=cores)
    mod = np.concatenate([r["mo"] for r in res.results], axis=2)
    del ims
    ncA = build_A()
    ncB = build_B()
    xin = np.concatenate([ctx, x], 0).astype(np.float32)
    tables = rope_tables()
    cst = make_cst()
    ident = np.eye(128, dtype=np.float32)
    rows_of = [np.concatenate([np.arange(32 * c, 32 * c + 32), 256 + np.arange(2048 * c, 2048 * c + 2048)]) for c in cores]
    for l in range(2):
        mod_x, mod_c = mod[l, 0], mod[l, 1]
        xT = np.ascontiguousarray(xin.T)
        imsA = [prep_A(inp, l, c, xT, mod_x, mod_c, tables, cst) for c in cores]
        resA = run_bass_kernel_spmd(ncA, imsA, core_ids=cores)
        del imsA, xT
        ymix = np.empty((NTOK, 2048), np.float32)
        for c in cores:
            o = resA.results[c]["out"]
            ymix[:, c * 128:(c + 1) * 128] = o[:, :128]
            ymix[:, 1024 + c * 128:1024 + (c + 1) * 128] = o[:, 128:]
        del resA
        sh1, sc1, gt1, sh2, sc2, gt2 = np.split(mod_x, 6)
        csh1, csc1, cgt1, csh2, csc2, cgt2 = np.split(mod_c, 6)
        vecs = np.ascontiguousarray(np.stack([gt1, cgt1, sh2, csh2, sc2, csc2, gt2, cgt2,
                                              inp["norm2_g"][l], inp["final_g"]]).astype(np.float32))
        imsB = []
        for c in cores:
            rows = rows_of[c]
            imsB.append({"ymT": np.ascontiguousarray(ymix[rows].T), "xres": np.ascontiguousarray(xin[rows]),
                         "w_out": inp["w_out"][l], "w1": inp["moe_w1"][l], "w2": inp["moe_w2"][l],
                         "rw": inp["router_w"], "rb": inp["router_b"], "vecs": vecs, "ident": ident})
        resB = run_bass_kernel_spmd(ncB, imsB, core_ids=cores)
        del imsB, ymix
        key = "outn" if l == 1 else "out"
        xnew = np.empty_like(xin)
        for c in cores:
            xnew[rows_of[c]] = resB.results[c][key]
        del resB
        xin = xnew
    return np.ascontiguousarray(xin[256:].reshape(1, 16384, 2048).astype(np.float32))
```
